# Optimizing a Trainium2 kernel written in Bass

```python
import math
import jax, jax.numpy as jnp
from jax import lax
import numpy as np

D_MODEL = 2048
BATCH = 16
SEQ = 256
DEPTH = 4
DEC_BATCH = 2
DEC_SEQ = 1024
PAST_LEN = 256

GRID_W = 64
Q_BLOCK = 128
TOKEN_BLOCK = 128
ROPE_BASE = 10000.0
EPS = 1e-6

MLA_HEADS = 8
MLA_NOPE = 128
MLA_ROPE = 64
MLA_V = 128
MLA_Q_RANK = 512
MLA_KV_RANK = 256

POOL_WINDOWS = (2, 4, 8, 16)
POOL_GROUP = 128
POOL_WIDTH = POOL_GROUP * len(POOL_WINDOWS)

DIFF_HEADS = 4
DIFF_QK = 64
DIFF_V = 2 * DIFF_QK

N_BRANCH = 3

PEER_HEADS = 8
PEER_KEYS = 128
PEER_EXPERTS = PEER_KEYS * PEER_KEYS
PEER_TOPK = 16
PEER_QDIM = 256
PEER_HALF = PEER_QDIM // 2

W_Q_A = MLA_Q_RANK
W_KV_A = MLA_KV_RANK + MLA_ROPE
W_DQ = DIFF_HEADS * 2 * DIFF_QK
W_DK = DIFF_HEADS * 2 * DIFF_QK
W_DV = DIFF_HEADS * DIFF_V
W_GATE = N_BRANCH * D_MODEL
IN_WIDTH = W_Q_A + W_KV_A + POOL_WIDTH + W_DQ + W_DK + W_DV + W_GATE
IN_SPLITS = (W_Q_A, W_Q_A + W_KV_A, W_Q_A + W_KV_A + POOL_WIDTH, W_Q_A + W_KV_A + POOL_WIDTH + W_DQ, W_Q_A + W_KV_A + POOL_WIDTH + W_DQ + W_DK, W_Q_A + W_KV_A + POOL_WIDTH + W_DQ + W_DK + W_DV)

kernel_name = 'hybrid_flow_trunk_ctx_and_denoise'


def rmsnorm(x, g):
    xf = x.astype(jnp.float32)
    y = xf * lax.rsqrt(jnp.mean(xf * xf, axis=-1, keepdims=True) + EPS)
    return (y * g.astype(jnp.float32)).astype(x.dtype)


def rope_1d(x, pos):
    half = x.shape[-1] // 2
    inv = ROPE_BASE ** (-jnp.arange(half, dtype=jnp.float32) / half)
    ang = pos.astype(jnp.float32)[:, None] * inv
    cos = jnp.cos(ang)[None, :, None, :]
    sin = jnp.sin(ang)[None, :, None, :]
    xf = x.astype(jnp.float32)
    x1, x2 = xf[..., :half], xf[..., half:]
    return jnp.concatenate([x1 * cos - x2 * sin, x1 * sin + x2 * cos], axis=-1).astype(x.dtype)


def rope_2d(x, pos):
    if pos is None:
        return x
    row, col = pos
    r = x.shape[-1] // 2
    return jnp.concatenate([rope_1d(x[..., :r], row), rope_1d(x[..., r:], col)], axis=-1)


def attention(q, k, v, scale):
    B, Tq, H, dk = q.shape
    blk = min(Q_BLOCK, Tq)
    nb = Tq // blk
    qb = q.reshape(B, nb, blk, H, dk).swapaxes(0, 1)

    def one(qi):
        s = jnp.einsum('bqhd,bkhd->bhqk', qi, k, preferred_element_type=jnp.float32) * scale
        p = jax.nn.softmax(s, axis=-1).astype(v.dtype)
        return jnp.einsum('bhqk,bkhd->bqhd', p, v)

    o = lax.map(one, qb)
    return o.swapaxes(0, 1).reshape(B, Tq, H, v.shape[-1])


def mla_branch(qa, kva, lp, pos, ctx_ckv, ctx_krope):
    B, T, _ = qa.shape
    q = (rmsnorm(qa, lp['g_qnorm']) @ lp['w_qb']).reshape(B, T, MLA_HEADS, MLA_NOPE + MLA_ROPE)
    q_nope, q_rope = q[..., :MLA_NOPE], rope_2d(q[..., MLA_NOPE:], pos)
    ckv = rmsnorm(kva[..., :MLA_KV_RANK], lp['g_kvnorm'])
    krope = kva[..., MLA_KV_RANK:][:, :, None, :]
    krope_rot = rope_2d(krope, pos)
    if ctx_ckv is None:
        ckv_all, krope_all = ckv, krope_rot
    else:
        ckv_all = jnp.concatenate([ctx_ckv, ckv], axis=1)
        krope_all = jnp.concatenate([ctx_krope[:, :, None, :], krope_rot], axis=1)
    kv = (ckv_all @ lp['w_kvb']).reshape(B, ckv_all.shape[1], MLA_HEADS, MLA_NOPE + MLA_V)
    Tk = kv.shape[1]
    k = jnp.concatenate([kv[..., :MLA_NOPE], jnp.broadcast_to(krope_all, (B, Tk, MLA_HEADS, MLA_ROPE))], axis=-1)
    o = attention(jnp.concatenate([q_nope, q_rope], axis=-1), k, kv[..., MLA_NOPE:], (MLA_NOPE + MLA_ROPE) ** -0.5)
    return o.reshape(B, T, MLA_HEADS * MLA_V), ckv, krope[:, :, 0, :]


def multi_scale_pool(z, w_pool, pool_scale):
    B, T, _ = z.shape
    zf = z.astype(jnp.float32)
    csum = jnp.concatenate([jnp.zeros((B, 1, POOL_WIDTH), jnp.float32), jnp.cumsum(zf, axis=1)], axis=1)
    t = jnp.arange(T)
    parts = []
    for gi, w in enumerate(POOL_WINDOWS):
        lo = jnp.maximum(t - w // 2, 0)
        hi = jnp.minimum(t + (w - 1) // 2, T - 1)
        sl = slice(gi * POOL_GROUP, (gi + 1) * POOL_GROUP)
        cg = csum[..., sl]
        mean = (cg[:, hi + 1] - cg[:, lo]) / (hi - lo + 1).astype(jnp.float32)[None, :, None]
        parts.append(mean - zf[..., sl])
    d = jnp.stack(parts, axis=2).astype(z.dtype)
    y = jnp.einsum('btgc,gcd->btgd', d, w_pool).reshape(B, T, POOL_WIDTH)
    return y * pool_scale


def diff_branch(dq, dk, dv, lp, l, pos, ctx_k, ctx_v):
    B, T, _ = dq.shape
    q = rope_2d(dq.reshape(B, T, 2 * DIFF_HEADS, DIFF_QK), pos).reshape(B, T, DIFF_HEADS, 2 * DIFF_QK)
    k_raw = dk.reshape(B, T, DIFF_HEADS, 2 * DIFF_QK)
    k = rope_2d(k_raw.reshape(B, T, 2 * DIFF_HEADS, DIFF_QK), pos).reshape(B, T, DIFF_HEADS, 2 * DIFF_QK)
    v = dv.reshape(B, T, DIFF_HEADS, DIFF_V)
    if ctx_k is None:
        v_all = v
    else:
        k = jnp.concatenate([ctx_k, k], axis=1)
        v_all = jnp.concatenate([ctx_v, v], axis=1)
    lam_init = 0.8 - 0.6 * math.exp(-0.3 * l)
    lv = lp['diff_lambda'].astype(jnp.float32)
    lam = jnp.exp(jnp.sum(lv[0] * lv[1])) - jnp.exp(jnp.sum(lv[2] * lv[3])) + lam_init
    scale = DIFF_QK ** -0.5
    a1 = attention(q[..., :DIFF_QK], k[..., :DIFF_QK], v_all, scale)
    a2 = attention(q[..., DIFF_QK:], k[..., DIFF_QK:], v_all, scale)
    o = a1.astype(jnp.float32) - lam * a2.astype(jnp.float32)
    o = rmsnorm(o, lp['g_diffnorm']) * (1.0 - lam_init)
    return o.reshape(B, T, DIFF_HEADS * DIFF_V).astype(dq.dtype), k_raw, v


def peer_ffn(h, lp):
    B, T, D = h.shape
    n = B * T
    hf = h.reshape(n, D)
    q = (hf @ lp['w_peer_q']).reshape(n, PEER_HEADS, 2, PEER_HALF)
    s = jnp.einsum('nhpc,pkc->nhpk', q, lp['peer_subkeys'], preferred_element_type=jnp.float32)
    s1, i1 = lax.top_k(s[:, :, 0], PEER_TOPK)
    s2, i2 = lax.top_k(s[:, :, 1], PEER_TOPK)
    cand = (s1[..., :, None] + s2[..., None, :]).reshape(n, PEER_HEADS, PEER_TOPK * PEER_TOPK)
    cidx = (i1[..., :, None] * PEER_KEYS + i2[..., None, :]).reshape(n, PEER_HEADS, PEER_TOPK * PEER_TOPK)
    best, sel = lax.top_k(cand, PEER_TOPK)
    idx = jnp.take_along_axis(cidx, sel, axis=-1)
    g = jax.nn.softmax(best, axis=-1)
    nb = n // TOKEN_BLOCK
    u_tab, v_tab = lp['peer_u'], lp['peer_v']

    def one(args):
        ht, it, gt = args
        act = jax.nn.gelu(jnp.einsum('tkd,td->tk', u_tab[it], ht, preferred_element_type=jnp.float32), approximate=False)
        return jnp.einsum('tk,tkd->td', (gt * act).astype(h.dtype), v_tab[it])

    out = lax.map(one, (hf.reshape(nb, TOKEN_BLOCK, D), idx.reshape(nb, TOKEN_BLOCK, PEER_HEADS * PEER_TOPK), g.reshape(nb, TOKEN_BLOCK, PEER_HEADS * PEER_TOPK)))
    return out.reshape(B, T, D)


def trunk_layer(x, cond, l, pos, ctx, lp):
    B, T, _ = x.shape
    mod = (jax.nn.silu(cond) @ lp['w_mod'] + lp['b_mod'])[:, None, :]
    sh1, sc1, ga1, sh2, sc2, ga2 = jnp.split(mod, 6, axis=-1)
    h = rmsnorm(x, lp['g_norm1']) * (1 + sc1) + sh1
    qa, kva, pz, dq, dk, dv, gl = jnp.split(h @ lp['w_in'], IN_SPLITS, axis=-1)
    if ctx is None:
        c_ckv = c_krope = c_k = c_v = None
    else:
        c_ckv, c_krope, c_k, c_v = ctx
    mla_o, ckv, krope = mla_branch(qa, kva, lp, pos, c_ckv, c_krope)
    pool_o = multi_scale_pool(pz, lp['w_pool'], lp['pool_scale'])
    diff_o, k_raw, v_raw = diff_branch(dq, dk, dv, lp, l, pos, c_k, c_v)
    gates = jax.nn.sigmoid(gl.reshape(B, T, N_BRANCH, D_MODEL))
    merged = (gates[:, :, 0] * (mla_o @ lp['w_br_mla'])
              + gates[:, :, 1] * (pool_o @ lp['w_br_pool'])
              + gates[:, :, 2] * (diff_o @ lp['w_br_diff']))
    x = x + ga1 * (merged @ lp['w_out'])
    h2 = rmsnorm(x, lp['g_norm2']) * (1 + sc2) + sh2
    x = x + ga2 * peer_ffn(h2, lp)
    return x, (ckv, krope, k_raw, v_raw)


def setup_inputs(seed: int = 0) -> dict:
    key = jax.random.key(seed)
    ks = jax.random.split(key, 32)
    D = D_MODEL

    def nrm(k, shape, scale=1.0):
        return jax.random.normal(k, shape, jnp.float32) * scale

    def gain(k, shape):
        return 1.0 + 0.02 * jax.random.normal(k, shape, jnp.float32)

    return {
        'x_prompt': nrm(ks[0], (BATCH, SEQ, D)),
        'x_sample': nrm(ks[1], (DEC_BATCH, DEC_SEQ, D)),
        'cache_mla_ckv': nrm(ks[2], (DEC_BATCH, DEPTH, PAST_LEN, MLA_KV_RANK)),
        'cache_mla_krope': nrm(ks[3], (DEC_BATCH, DEPTH, PAST_LEN, MLA_ROPE)),
        'cache_diff_k': nrm(ks[4], (DEC_BATCH, DEPTH, PAST_LEN, DIFF_HEADS, 2 * DIFF_QK)),
        'cache_diff_v': nrm(ks[5], (DEC_BATCH, DEPTH, PAST_LEN, DIFF_HEADS, DIFF_V)),
        'c': nrm(ks[6], (DEC_BATCH, D)),
        'c_ctx': nrm(ks[7], (D,)),
        'w_mod': nrm(ks[8], (DEPTH, D, 6 * D), 0.5 * D ** -0.5),
        'b_mod': nrm(ks[9], (DEPTH, 6 * D), 0.02),
        'g_norm1': gain(ks[10], (DEPTH, D)),
        'w_in': nrm(ks[11], (DEPTH, D, IN_WIDTH), D ** -0.5),
        'g_qnorm': gain(ks[12], (DEPTH, MLA_Q_RANK)),
        'w_qb': nrm(ks[13], (DEPTH, MLA_Q_RANK, MLA_HEADS * (MLA_NOPE + MLA_ROPE)), MLA_Q_RANK ** -0.5),
        'g_kvnorm': gain(ks[14], (DEPTH, MLA_KV_RANK)),
        'w_kvb': nrm(ks[15], (DEPTH, MLA_KV_RANK, MLA_HEADS * (MLA_NOPE + MLA_V)), MLA_KV_RANK ** -0.5),
        'w_pool': nrm(ks[16], (DEPTH, len(POOL_WINDOWS), POOL_GROUP, POOL_GROUP), POOL_GROUP ** -0.5),
        'pool_scale': gain(ks[17], (DEPTH, POOL_WIDTH)),
        'diff_lambda': nrm(ks[18], (DEPTH, 4, DIFF_QK), 0.1),
        'g_diffnorm': gain(ks[19], (DEPTH, DIFF_V)),
        'w_br_mla': nrm(ks[20], (DEPTH, MLA_HEADS * MLA_V, D), (MLA_HEADS * MLA_V) ** -0.5),
        'w_br_pool': nrm(ks[21], (DEPTH, POOL_WIDTH, D), POOL_WIDTH ** -0.5),
        'w_br_diff': nrm(ks[22], (DEPTH, DIFF_HEADS * DIFF_V, D), (DIFF_HEADS * DIFF_V) ** -0.5),
        'w_out': nrm(ks[23], (DEPTH, D, D), D ** -0.5),
        'g_norm2': gain(ks[24], (DEPTH, D)),
        'w_peer_q': nrm(ks[25], (DEPTH, D, PEER_HEADS * PEER_QDIM), D ** -0.5),
        'peer_subkeys': nrm(ks[26], (DEPTH, 2, PEER_KEYS, PEER_HALF), PEER_HALF ** -0.5),
        'peer_u': nrm(ks[27], (DEPTH, PEER_EXPERTS, D), D ** -0.5),
        'peer_v': nrm(ks[28], (DEPTH, PEER_EXPERTS, D), PEER_HEADS ** -0.5),
        'g_final': gain(ks[29], (D,)),
    }


def reference(x_prompt, x_sample, cache_mla_ckv, cache_mla_krope, cache_diff_k, cache_diff_v, c, c_ctx,
              w_mod, b_mod, g_norm1, w_in, g_qnorm, w_qb, g_kvnorm, w_kvb, w_pool, pool_scale,
              diff_lambda, g_diffnorm, w_br_mla, w_br_pool, w_br_diff, w_out, g_norm2,
              w_peer_q, peer_subkeys, peer_u, peer_v, g_final):
    rows = x_sample.shape[1] // GRID_W
    pos = (jnp.repeat(jnp.arange(rows), GRID_W), jnp.tile(jnp.arange(GRID_W), rows))
    xp, xs = x_prompt, x_sample
    st_ckv, st_krope, st_k, st_v = [], [], [], []
    for l in range(DEPTH):
        lp = {
            'w_mod': w_mod[l], 'b_mod': b_mod[l], 'g_norm1': g_norm1[l], 'w_in': w_in[l],
            'g_qnorm': g_qnorm[l], 'w_qb': w_qb[l], 'g_kvnorm': g_kvnorm[l], 'w_kvb': w_kvb[l],
            'w_pool': w_pool[l], 'pool_scale': pool_scale[l], 'diff_lambda': diff_lambda[l],
            'g_diffnorm': g_diffnorm[l], 'w_br_mla': w_br_mla[l], 'w_br_pool': w_br_pool[l],
            'w_br_diff': w_br_diff[l], 'w_out': w_out[l], 'g_norm2': g_norm2[l],
            'w_peer_q': w_peer_q[l], 'peer_subkeys': peer_subkeys[l], 'peer_u': peer_u[l], 'peer_v': peer_v[l],
        }
        xp, st = trunk_layer(xp, c_ctx[None, :], l, None, None, lp)
        st_ckv.append(st[0])
        st_krope.append(st[1])
        st_k.append(st[2])
        st_v.append(st[3])
        ctx = (cache_mla_ckv[:, l], cache_mla_krope[:, l], cache_diff_k[:, l], cache_diff_v[:, l])
        xs, _ = trunk_layer(xs, c, l, pos, ctx, lp)
    y_prompt = rmsnorm(xp, g_final)
    y_sample = rmsnorm(xs, g_final)
    state_mla_ckv = jnp.stack(st_ckv, axis=1)
    state_mla_krope = jnp.stack(st_krope, axis=1)
    state_diff_k = jnp.stack(st_k, axis=1)
    state_diff_v = jnp.stack(st_v, axis=1)
    return (y_prompt, y_sample, state_mla_ckv, state_mla_krope, state_diff_k, state_diff_v)
```

```python
import contextlib
import math

import numpy as np
import concourse.bass as bass
import concourse.mybir as mybir
from concourse.bass_utils import run_bass_kernel_spmd

F32 = mybir.dt.float32
BF16 = mybir.dt.bfloat16
I32 = mybir.dt.int32
U32 = mybir.dt.uint32
AF = mybir.ActivationFunctionType
ALU = mybir.AluOpType
AX = mybir.AxisListType

D = 2048
T = 1024
TK = 1280
PAST = 256
NDS = 40
EPS = 1e-6
BIG = 30000.0

C_QA, C_KVA, C_PZ, C_DQ, C_DK, C_DV, C_GL = 0, 512, 832, 1344, 1856, 2368, 2880


class Buf:
    __slots__ = ("name", "w", "r")

    def __init__(self, name):
        self.name = name
        self.w = None
        self.r = {}


class Ctx:
    def __init__(self, nc, es):
        self.nc = nc
        self.es = es
        self.eng = {}
        for name, h in (("pe", nc.tensor), ("dve", nc.vector), ("act", nc.scalar),
                        ("pool", nc.gpsimd), ("sp", nc.sync)):
            self.eng[name] = {"h": h, "sem": es.enter_context(nc.semaphore("e_" + name)), "cnt": 0,
                              "known": {}, "name": name}
        self.dsems = [es.enter_context(nc.semaphore("d%d" % i)) for i in range(NDS)]
        self.dcnt = [0] * NDS
        self.dnext = 0
        self.ntile = 0
        self.out_events = []

    def sb(self, name, shape, dtype):
        self.ntile += 1
        return self.es.enter_context(self.nc.sbuf_tensor("%s_%d" % (name, self.ntile), list(shape), dtype))

    def ps(self, name, shape, dtype):
        self.ntile += 1
        return self.es.enter_context(self.nc.psum_tensor("%s_%d" % (name, self.ntile), list(shape), dtype))

    def _wait(self, e, ev):
        sem, val = ev
        k = id(sem)
        if e["known"].get(k, 0) >= val:
            return
        e["h"].wait_ge(sem, val)
        e["known"][k] = val

    def _collect(self, reads, writes):
        evs = []
        for b in reads:
            if b.w is not None:
                evs.append(b.w)
        for b in writes:
            if b.w is not None:
                evs.append(b.w)
            evs.extend(b.r.values())
        return evs

    def op(self, ename, fn, reads=(), writes=()):
        e = self.eng[ename]
        for ev in self._collect(reads, writes):
            self._wait(e, ev)
        inst = fn(e["h"])
        e["cnt"] += 1
        inst.then_inc(e["sem"], 1)
        ev = (e["sem"], e["cnt"])
        e["known"][id(e["sem"])] = e["cnt"] - 1
        for b in reads:
            b.r[ename] = ev
        for b in writes:
            b.w = ev
            b.r = {}
        return ev

    def dma(self, qname, out, in_, reads=(), writes=(), indirect=None, is_output=False):
        e = self.eng[qname]
        for ev in self._collect(reads, writes):
            self._wait(e, ev)
        slot = self.dnext
        self.dnext = (slot + 1) % NDS
        if self.dcnt[slot] > 0:
            self._wait(e, (self.dsems[slot], 16 * self.dcnt[slot]))
        self.dcnt[slot] += 1
        if indirect is not None:
            inst = e["h"].indirect_dma_start(out=out, out_offset=None, in_=in_,
                                             in_offset=bass.IndirectOffsetOnAxis(ap=indirect, axis=0))
        else:
            inst = e["h"].dma_start(out=out, in_=in_)
        inst.then_inc(self.dsems[slot], 16)
        ev = (self.dsems[slot], 16 * self.dcnt[slot])
        for b in reads:
            b.r[("d", slot)] = ev
        for b in writes:
            b.w = ev
            b.r = {}
        if is_output:
            self.out_events.append(ev)
        return ev

    def fence(self):
        evs = [(e["sem"], e["cnt"]) for e in self.eng.values() if e["cnt"] > 0]
        evs += [(self.dsems[s], 16 * self.dcnt[s]) for s in range(NDS) if self.dcnt[s] > 0]
        for e in self.eng.values():
            for ev in evs:
                if ev[0] is e["sem"]:
                    continue
                self._wait(e, ev)

    def finish(self):
        e = self.eng["sp"]
        for ev in self.out_events:
            self._wait(e, ev)
        evs = [(x["sem"], x["cnt"]) for x in self.eng.values() if x["cnt"] > 0 and x is not e]
        evs += [(self.dsems[s], 16 * self.dcnt[s]) for s in range(NDS) if self.dcnt[s] > 0]
        for ev in evs:
            self._wait(e, ev)


class PsumRing:
    def __init__(self, ctx, n=8):
        self.tiles = [ctx.ps("bank", [128, 512], F32) for _ in range(n)]
        self.bufs = [Buf("bank%d" % i) for i in range(n)]
        self.i = 0
        self.n = n
        self.held = set()

    def next(self):
        while self.i in self.held:
            self.i = (self.i + 1) % self.n
        i = self.i
        self.i = (i + 1) % self.n
        return self.tiles[i], self.bufs[i]

    def next_n(self, k):
        return [self.next() for _ in range(k)]

    def hold(self, k):
        out = []
        for _ in range(k):
            t, b = self.next()
            self.held.add(self.tiles.index(t))
            out.append((t, b))
        return out

    def release(self, banks):
        for t, b in banks:
            self.held.discard(self.tiles.index(t))


def build_program(L=4, stop_after=None):
    nc = bass.Bass("TRN2", target_bir_lowering=False)
    es = contextlib.ExitStack()
    cx = Ctx(nc, es)
    dbg = stop_after is not None

    in_names = []

    def din(name, shape, dtype=F32):
        in_names.append(name)
        return nc.dram_tensor(name, list(shape), dtype, kind="ExternalInput").ap()

    def dout(name, shape, dtype=F32):
        return nc.dram_tensor(name, list(shape), dtype, kind="ExternalOutput").ap()

    x_in = din("x", [T, D])
    gvec = din("gvec", [32, 128])
    ctx_ckv = din("ctx_ckv", [L, PAST, 256])
    ctx_krope = din("ctx_krope", [L, PAST, 64])
    ctx_dk = din("ctx_dk", [L, PAST, 512])
    ctx_dv = din("ctx_dv", [L, PAST, 512])
    ident_d = din("ident", [128, 128])
    perm_d = din("perm64", [64, 64])
    ropec_d = din("ropec", [64, T])
    ropes_d = din("ropes", [64, T])
    qaug_d = din("qaug", [4, T])
    kaug_d = din("kaug", [4, TK])
    flag_d = din("flag", [128, 1])
    invcnt_d = din("invcnt", [4, T])
    sel4_d = din("sel4", [4, 512])
    iota16_d = din("iota16", [128, 16])
    w_mod = din("w_mod", [L, D, 6 * D])
    vecs = din("vecs", [L, 144, 128])
    w_in = din("w_in", [L, D, 9024])
    w_qb = din("w_qb", [L, 512, 1536])
    w_kvb = din("w_kvb", [L, 256, 2048])
    w_pool = din("w_pool", [L, 4, 128, 128])
    dlam = din("diff_lambda", [L, 256])
    w_br_mla = din("w_br_mla", [L, 1024, D])
    w_br_pool = din("w_br_pool", [L, 512, D])
    w_br_diff = din("w_br_diff", [L, 512, D])
    w_out = din("w_out", [L, D, D])
    w_peer_q = din("w_peer_q", [L, D, D])
    subkeys = din("peer_subkeys", [L, 2, 128, 128])
    need_peer = stop_after is None or stop_after.startswith("peer") or stop_after == "layer"
    if need_peer:
        peer_u = din("peer_u", [L, 16384, D])
        peer_v = din("peer_v", [L, 16384, D])

    y_out = dout("y", [T, D])
    o_ckv = dout("o_ckv", [L, T, 256])
    o_krope = dout("o_krope", [L, T, 64])
    o_dk = dout("o_dk", [L, T, 512])
    o_dv = dout("o_dv", [L, T, 512])
    if dbg:
        dbg_out = dout("dbg", [128, 16 * T])

    xs = nc.dram_tensor("xs", [16, 128, T], F32, kind="Internal").ap()

    ident_f = cx.sb("ident_f", [128, 128], F32); b_ident = Buf("ident")
    ident_b = cx.sb("ident_b", [128, 128], BF16)
    ones_f = cx.sb("ones_f", [128, 128], F32)
    ones_b = cx.sb("ones_b", [128, 128], BF16)
    perm_f = cx.sb("perm", [64, 64], F32)
    ropec = cx.sb("ropec", [64, T], F32)
    ropes = cx.sb("ropes", [64, T], F32)
    flag = cx.sb("flag", [128, 1], F32)
    invcnt = cx.sb("invcnt", [4, T], F32)
    sel4 = cx.sb("sel4", [4, 512], F32)
    iota16 = cx.sb("iota16", [128, 16], F32)
    b_const = Buf("consts")
    hT = cx.sb("hT", [128, 16, T], BF16)
    b_hT = [Buf("hT%d" % j) for j in range(16)]
    NW = 2
    wbuf = [cx.sb("wbuf", [128, 8192], BF16) for _ in range(NW)]
    b_wbuf = [Buf("wbuf%d" % i) for i in range(NW)]
    wsm = [cx.sb("wsm", [128, 4096], BF16) for _ in range(2)]
    b_wsm = [Buf("wsm%d" % i) for i in range(2)]
    scr = [cx.sb("scr", [128, T], F32) for _ in range(3)]
    b_scr = [Buf("scr%d" % i) for i in range(3)]
    scr_i = [0]
    rs = cx.sb("rs", [128, T], F32); b_rs = Buf("rs")
    modT = cx.sb("modT", [128, 96], F32); b_modT = Buf("modT")
    vecT = cx.sb("vecT", [128, 144], F32); b_vecT = Buf("vecT")
    gvT = cx.sb("gvT", [128, 32], F32); b_gvT = Buf("gvT")
    scondT = cx.sb("scondT", [128, 16], BF16); b_scond = Buf("scond")
    drv = cx.sb("drv", [128, 64], F32); b_drv = Buf("drv")
    lamt = cx.sb("lamt", [128, 264], F32); b_lam = Buf("lam")
    stg = [cx.sb("stg", [128, 128], F32) for _ in range(4)]
    b_stg = [Buf("stg%d" % i) for i in range(4)]
    stg_i = [0]
    ARENA = 86 * 1024
    arena = cx.sb("arena", [128, ARENA // 2], BF16)

    pr = PsumRing(cx, 8)

    def next_scr():
        i = scr_i[0]
        scr_i[0] = (i + 1) % 3
        return scr[i], b_scr[i]

    def next_stg():
        i = stg_i[0]
        stg_i[0] = (i + 1) % 4
        return stg[i], b_stg[i]

    class Arena:
        def __init__(self):
            self.off = 0

        def reset(self):
            self.off = 0

        def take(self, shape, dtype):
            n = 1
            for s in shape[1:]:
                n *= s
            nbytes = n * (4 if dtype in (F32, I32, U32) else 2)
            nbytes = (nbytes + 63) // 64 * 64
            assert self.off + nbytes <= ARENA, ("arena overflow", self.off, nbytes)
            a = arena[:, self.off // 2:(self.off + nbytes) // 2]
            self.off += nbytes
            if dtype != BF16:
                a = a.bitcast(dtype)
            a = a[:, 0:n]
            if len(shape) == 3:
                a = a.rearrange("p (a b) -> p a b", a=shape[1])
            elif len(shape) == 4:
                a = a.rearrange("p (a b c) -> p a b c", a=shape[1], b=shape[2])
            return a

    ar = Arena()

    for t_sb, t_d in ((ident_f, ident_d), (perm_f, perm_d), (ropec, ropec_d), (ropes, ropes_d), (flag, flag_d),
                      (invcnt, invcnt_d), (sel4, sel4_d), (iota16, iota16_d)):
        cx.dma("sp", t_sb[:], t_d, writes=[b_const])
    cx.dma("pool", ident_b[:], ident_d, writes=[b_const])
    cx.op("dve", lambda e: e.memset(ones_f[:], 1.0), writes=[b_const])
    cx.op("dve", lambda e: e.memset(ones_b[:], 1.0), writes=[b_const])

    def transpose_f32(dst_fn, src_ap, p_in, f_in, reads, n_consume=1):
        pt, pb = pr.next()
        cx.op("pe", lambda e: e.transpose(pt[0:f_in, 0:p_in], src_ap, ident_f[0:p_in, 0:p_in]),
              reads=list(reads) + [b_const], writes=[pb])
        dst_fn(pt[0:f_in, 0:p_in], pb)

    for tc in range(8):
        xt, xb = next_scr()
        for hh in range(2):
            cx.dma("sp", xt[:, 0:T], x_in[tc * 128:(tc + 1) * 128, hh * T:(hh + 1) * T], writes=[xb])
            for jj in range(8):
                j = hh * 8 + jj
                st, sbf = next_stg()

                def ev(ps_ap, pb, st=st, sbf=sbf):
                    cx.op("act", lambda e: e.activation(out=st[:, :], in_=ps_ap, func=AF.Copy),
                          reads=[pb], writes=[sbf])
                transpose_f32(ev, xt[:, jj * 128:(jj + 1) * 128], 128, 128, [xb])
                cx.dma("sp", xs[j, :, tc * 128:(tc + 1) * 128], st[:, :], reads=[sbf], writes=[])
    b_xs = [Buf("xs%d" % j) for j in range(16)]
    cx.fence()

    st, sbf = next_stg()
    cx.dma("sp", st[0:32, :], gvec, writes=[sbf])
    transpose_f32(lambda ps_ap, pb: cx.op("act", lambda e: e.activation(out=gvT[:, :], in_=ps_ap, func=AF.Copy),
                                          reads=[pb], writes=[b_gvT]), st[0:32, :], 32, 128, [sbf])
    cx.op("act", lambda e: e.activation(out=scondT[:, :], in_=gvT[:, 0:16], func=AF.Silu),
          reads=[b_gvT], writes=[b_scond])

    w_i = [0]

    def load_w(dram_ap, kc, ncols, small=False):
        if small:
            i = w_i[0] % 2
            tile, buf = wsm[i], b_wsm[i]
            assert kc * ncols <= 4096
        else:
            i = w_i[0] % NW
            tile, buf = wbuf[i], b_wbuf[i]
            assert kc * ncols <= 8192
        w_i[0] += 1
        view = tile[:, 0:kc * ncols].rearrange("p (k n) -> p k n", k=kc)
        rows = dram_ap.shape[0]
        if rows >= 128:
            cx.dma("pool", view, dram_ap.rearrange("(k p) n -> p k n", p=128), writes=[buf])
        else:
            cx.dma("pool", view[0:rows, 0, :], dram_ap, writes=[buf])
        return view, buf

    def proj_fm(w_dram, K, cols, rhs_fn, rhs_bufs, consume, small=False, group=512, halves=(0, 1)):
        kc_n = max(1, K // 128)
        gi = 0
        while gi < len(cols):
            c_start = cols[gi][0]
            ge = gi
            while ge < len(cols) and cols[ge][0] + cols[ge][1] - c_start <= group:
                ge += 1
            width = cols[ge - 1][0] + cols[ge - 1][1] - c_start
            wv, wb = load_w(w_dram[:, c_start:c_start + width], kc_n, width, small=small)
            for ci in range(gi, ge):
                c0, M = cols[ci]
                for half in halves:
                    pt, pb = pr.next()
                    for kc in range(kc_n):
                        kp = min(128, K - kc * 128)
                        cx.op("pe", lambda e, kc=kc, kp=kp: e.matmul(
                            pt[0:M, :], wv[0:kp, kc, c0 - c_start:c0 - c_start + M], rhs_fn(kc, half),
                            start=(kc == 0), stop=(kc == kc_n - 1)),
                            reads=[wb] + list(rhs_bufs(kc)), writes=[pb])
                    consume(ci, half, pt[0:M, :], pb)
            gi = ge

    def rstd_from_psum(ps_list, nfeat, out_ap_fn, out_buf):
        for half, (pt, pb) in enumerate(ps_list):
            cx.op("act", lambda e: e.activation(out=out_ap_fn(half), in_=pt[:, :], func=AF.Sqrt,
                                                scale=1.0 / nfeat, bias=eps_t[:, 0:1]),
                  reads=[pb, b_const], writes=[out_buf])
        for half in range(len(ps_list)):
            cx.op("dve", lambda e: e.reciprocal(out=out_ap_fn(half), in_=out_ap_fn(half)),
                  reads=[out_buf], writes=[out_buf])

    eps_t = cx.sb("eps", [128, 1], F32)
    cx.op("dve", lambda e: e.memset(eps_t[:], EPS), writes=[b_const])

    def norm_x(l, which, final=False):
        acc = pr.next_n(2)
        for j in range(16):
            xt, xb = next_scr()
            cx.dma("sp", xt[:, :], xs[j], reads=[b_xs[j]], writes=[xb])
            sq, sqb = next_scr()
            cx.op("act", lambda e: e.activation(out=sq[:, :], in_=xt[:, :], func=AF.Square), reads=[xb], writes=[sqb])
            for half in range(2):
                pt, pb = acc[half]
                cx.op("pe", lambda e: e.matmul(pt[:, :], ones_f[:, :], sq[:, half * 512:(half + 1) * 512],
                                               start=(j == 0), stop=(j == 15)), reads=[sqb, b_const], writes=[pb])
        rstd_from_psum(acc, D, lambda half: rs[:, half * 512:(half + 1) * 512], b_rs)

    def apply_norm(l, which):
        gs = drv[:, (0 if which == 1 else 16):]
        sh = modT[:, (0 if which == 1 else 48):]
        for j in range(16):
            xt, xb = next_scr()
            cx.dma("sp", xt[:, :], xs[j], reads=[b_xs[j]], writes=[xb])
            cx.op("dve", lambda e: e.tensor_tensor(out=xt[:, :], in0=xt[:, :], in1=rs[:, :], op=ALU.mult),
                  reads=[xb, b_rs], writes=[xb])
            cx.op("dve", lambda e: e.tensor_scalar(out=hT[:, j, :], in0=xt[:, :], scalar1=gs[:, j:j + 1],
                                                   scalar2=sh[:, j:j + 1], op0=ALU.mult, op1=ALU.add),
                  reads=[xb, b_drv, b_modT], writes=[b_hT[j]])

    def dump(ap_2d, bufs, ncols):
        cx.dma("sp", dbg_out[0:ap_2d.shape[0], 0:ncols], ap_2d, reads=bufs, writes=[], is_output=True)

    for l in range(L):
        st, sbf = next_stg()
        cx.dma("sp", st[:, :], vecs[l, 0:128, :], writes=[sbf])
        transpose_f32(lambda ps_ap, pb: cx.op("act", lambda e: e.activation(out=vecT[:, 0:128], in_=ps_ap, func=AF.Copy),
                                              reads=[pb], writes=[b_vecT]), st[:, :], 128, 128, [sbf])
        st, sbf = next_stg()
        cx.dma("sp", st[0:16, :], vecs[l, 128:144, :], writes=[sbf])
        transpose_f32(lambda ps_ap, pb: cx.op("act", lambda e: e.activation(out=vecT[:, 128:144], in_=ps_ap, func=AF.Copy),
                                              reads=[pb], writes=[b_vecT]), st[0:16, :], 16, 128, [sbf])
        mt, mb = pr.next()
        for g in range(24):
            wv, wb = load_w(w_mod[l, :, g * 512:(g + 1) * 512], 16, 512)
            for cc in range(4):
                m = g * 4 + cc
                for kc in range(16):
                    cx.op("pe", lambda e: e.matmul(mt[:, m:m + 1], wv[:, kc, cc * 128:(cc + 1) * 128],
                                                   scondT[:, kc:kc + 1], start=(kc == 0), stop=(kc == 15)),
                          reads=[wb, b_scond], writes=[mb])
        cx.op("dve", lambda e: e.tensor_tensor(out=modT[:, :], in0=mt[:, 0:96], in1=vecT[:, 0:96], op=ALU.add),
              reads=[mb, b_vecT], writes=[b_modT])
        cx.op("dve", lambda e: e.scalar_tensor_tensor(out=drv[:, 0:16], in0=modT[:, 16:32], scalar=1.0,
                                                      in1=vecT[:, 96:112], op0=ALU.add, op1=ALU.mult),
              reads=[b_modT, b_vecT], writes=[b_drv])
        cx.op("dve", lambda e: e.scalar_tensor_tensor(out=drv[:, 16:32], in0=modT[:, 64:80], scalar=1.0,
                                                      in1=vecT[:, 112:128], op0=ALU.add, op1=ALU.mult),
              reads=[b_modT, b_vecT], writes=[b_drv])
        if stop_after == "mod":
            dump(modT[:, :], [b_modT], 96)
            break
        norm_x(l, 1)
        apply_norm(l, 1)
        if stop_after == "norm1":
            for j in range(16):
                xt, xb = next_scr()
                cx.op("act", lambda e: e.activation(out=xt[:, :], in_=hT[:, j, :], func=AF.Copy),
                      reads=[b_hT[j]], writes=[xb])
                cx.dma("sp", dbg_out[:, j * T:(j + 1) * T], xt[:, :], reads=[xb], is_output=True)
            break
        hrhs = lambda kc, half: hT[:, kc, half * 512:(half + 1) * 512]
        hbufs = lambda kc: [b_hT[kc]]
        lam_init = 0.8 - 0.6 * math.exp(-0.3 * l)
        ar.reset()
        mla_oT = ar.take([128, 8, T], BF16); b_mla_o = Buf("mla_o")
        pool_oT = ar.take([128, 4, T], BF16); b_pool_o = Buf("pool_o")
        diff_oT = ar.take([128, 4, T], BF16); b_diff_o = Buf("diff_o")
        mark = ar.off

        def mm_fm(wv, wb, coff, M, K, rhs_fn, rhs_bufs, half, N=512):
            kc_n = max(1, K // 128)
            pt, pb = pr.next()
            for kc in range(kc_n):
                kp = min(128, K - kc * 128)
                cx.op("pe", lambda e: e.matmul(pt[0:M, 0:N], wv[0:kp, kc, coff:coff + M], rhs_fn(kc, half),
                                               start=(kc == 0), stop=(kc == kc_n - 1)),
                      reads=[wb] + list(rhs_bufs(kc)), writes=[pb])
            return pt, pb

        def rope_unit(raw, rawb, dst_fn, dstb):
            for half in range(2):
                sl = slice(half * 512, (half + 1) * 512)
                pt, pb = pr.next()
                cx.op("pe", lambda e: e.matmul(pt[0:64, :], perm_f[:, :], raw[0:64, sl], start=True, stop=True),
                      reads=[rawb, b_const], writes=[pb])
                t1, t1b = next_scr()
                cx.op("dve", lambda e: e.tensor_tensor(out=t1[0:64, 0:512], in0=pt[0:64, :], in1=ropes[:, sl], op=ALU.mult),
                      reads=[pb, b_const], writes=[t1b])
                cx.op("dve", lambda e: e.tensor_tensor(out=t1[0:64, 512:1024], in0=raw[0:64, sl], in1=ropec[:, sl], op=ALU.mult),
                      reads=[rawb, b_const], writes=[t1b])
                cx.op("dve", lambda e: e.tensor_tensor(out=dst_fn(sl), in0=t1[0:64, 0:512], in1=t1[0:64, 512:1024], op=ALU.add),
                      reads=[t1b], writes=[dstb])

        def out_transposes(raw, rawb, nfeat, out_dram_fn, extra=None):
            for tc in range(8):
                st, sbf = next_stg()

                def ev(ps_ap, pb, st=st, sbf=sbf, tc=tc):
                    cx.op("act", lambda e: e.activation(out=st[:, 0:nfeat], in_=ps_ap, func=AF.Copy),
                          reads=[pb], writes=[sbf])
                    if extra is not None:
                        extra(tc, st[:, 0:nfeat], sbf)
                transpose_f32(ev, raw[0:nfeat, tc * 128:(tc + 1) * 128], nfeat, 128, [rawb])
                import os
                if os.environ.get("SKIP_ODMA"):
                    continue
                cx.dma("sp", out_dram_fn(tc), st[:, 0:nfeat], reads=[sbf], is_output=True)

        def attention_core(kparts, qparts, v_fn, v_bufs, et, etb, qh, scale):
            for kc in range(10):
                pt, pb = pr.next()
                n = len(kparts)
                for i in range(n):
                    kf, kb, K = kparts[i]
                    qa_, qb_ = qparts[i]
                    cx.op("pe", lambda e: e.matmul(pt[:, :], kf(kc), qa_, start=(i == 0), stop=(i == n - 1)),
                          reads=list(kb) + list(qb_), writes=[pb])
                cx.op("act", lambda e: e.activation(out=et[:, kc, :], in_=pt[:, :], func=AF.Exp, scale=scale),
                      reads=[pb], writes=[etb])
            (o_ps, o_pb), (d_ps, d_pb) = pr.next_n(2)
            for kc in range(10):
                cx.op("pe", lambda e: e.matmul(o_ps[:, :], v_fn(kc), et[:, kc, :], start=(kc == 0), stop=(kc == 9)),
                      reads=[etb] + list(v_bufs), writes=[o_pb])
            for kc in range(10):
                cx.op("pe", lambda e: e.matmul(d_ps[:, :], ones_b[:, :], et[:, kc, :], start=(kc == 0), stop=(kc == 9)),
                      reads=[etb, b_const], writes=[d_pb])
            return o_ps, o_pb, d_ps, d_pb

        vdiff = ar.take([128, 10, 512], BF16); b_vdiff = Buf("vdiff")
        ET = [ar.take([128, 10, 512], BF16) for _ in range(2)]; b_ET = [Buf("ET0"), Buf("ET1")]
        qu = [ar.take([128, T], BF16) for _ in range(2)]; b_qu = [Buf("qu0"), Buf("qu1")]
        ku = [ar.take([128, TK], BF16) for _ in range(2)]; b_ku = [Buf("ku0"), Buf("ku1")]
        asb = [ar.take([128, 512], F32) for _ in range(3)]; b_asb = [Buf("as0"), Buf("as1"), Buf("as2")]
        ctxk = ar.take([128, 2, 512], F32); b_ctxk = Buf("ctxk")
        for s in range(2):
            cx.dma("pool", qu[s][64:68, :], qaug_d, writes=[b_qu[s]])
            cx.dma("pool", ku[s][64:68, :], kaug_d, writes=[b_ku[s]])
        cx.dma("sp", ctxk, ctx_dk[l].rearrange("(c p) f -> p c f", p=128), writes=[b_ctxk])
        cx.dma("pool", vdiff[:, 0:2, :], ctx_dv[l].rearrange("(c p) f -> p c f", p=128), writes=[b_vdiff])
        cx.dma("sp", lamt[:, 0:256], dlam[l].partition_broadcast(128), writes=[b_lam])
        for i in range(2):
            cx.op("dve", lambda e: e.scalar_tensor_tensor(out=lamt[:, 128 * i:128 * i + 64], in0=lamt[:, 128 * i:128 * i + 64],
                                                          scalar=1.0, in1=lamt[:, 128 * i + 64:128 * i + 128],
                                                          op0=ALU.mult, op1=ALU.mult, accum_out=lamt[:, 256 + i:257 + i]),
                  reads=[b_lam], writes=[b_lam])
        cx.op("act", lambda e: e.activation(out=lamt[:, 258:260], in_=lamt[:, 256:258], func=AF.Exp), reads=[b_lam], writes=[b_lam])
        cx.op("dve", lambda e: e.tensor_tensor(out=lamt[:, 260:261], in0=lamt[:, 259:260], in1=lamt[:, 258:259], op=ALU.subtract),
              reads=[b_lam], writes=[b_lam])
        cx.op("dve", lambda e: e.tensor_scalar(out=lamt[:, 260:261], in0=lamt[:, 260:261], scalar1=-lam_init, scalar2=None, op0=ALU.add),
              reads=[b_lam], writes=[b_lam])
        cx.op("dve", lambda e: e.tensor_scalar(out=drv[:, 32:33], in0=vecT[:, 138:139], scalar1=(1.0 - lam_init), scalar2=None, op0=ALU.mult),
              reads=[b_vecT], writes=[b_drv])
        if stop_after == "diffA":
            dump(lamt[:, 0:264], [b_lam], 264)
            break
        wv, wb = load_w(w_in[l, :, C_DV:C_DV + 512], 16, 512)
        for c in range(4):
            raw, rawb = next_scr()
            for half in range(2):
                pt, pb = mm_fm(wv, wb, c * 128, 128, D, hrhs, hbufs, half)
                cx.op("act", lambda e: e.activation(out=raw[:, half * 512:(half + 1) * 512], in_=pt[:, :], func=AF.Copy),
                      reads=[pb], writes=[rawb])

            def extra(tc, ps_ap, pb, c=c):
                import os
                if os.environ.get("SKIP_EXTRA"):
                    return
                cx.op("dve", lambda e: e.tensor_copy(out=vdiff[:, 2 + tc, c * 128:(c + 1) * 128], in_=ps_ap),
                      reads=[pb], writes=[b_vdiff])
            out_transposes(raw, rawb, 128, lambda tc, c=c: o_dv[l, tc * 128:(tc + 1) * 128, c * 128:(c + 1) * 128], extra)
        if stop_after == "diffB":
            dump(lamt[:, 0:264], [b_lam], 264)
            break
        wq_v, wq_b = load_w(w_in[l, :, C_DQ:C_DQ + 512], 16, 512)
        wk_v, wk_b = load_w(w_in[l, :, C_DK:C_DK + 512], 16, 512)
        dscale = 64.0 ** -0.5
        etc = 0
        for h in range(4):
            for s in range(2):
                u = 2 * h + s
                raw, rawb = next_scr()
                for half in range(2):
                    pt, pb = mm_fm(wk_v, wk_b, u * 64, 64, D, hrhs, hbufs, half)
                    cx.op("act", lambda e: e.activation(out=raw[0:64, half * 512:(half + 1) * 512], in_=pt[0:64, :], func=AF.Copy),
                          reads=[pb], writes=[rawb])
                out_transposes(raw, rawb, 64, lambda tc, u=u: o_dk[l, tc * 128:(tc + 1) * 128, u * 64:(u + 1) * 64])
                rope_unit(raw, rawb, lambda sl, s=s: ku[s][0:64, PAST + sl.start:PAST + sl.stop], b_ku[s])
                for tc in range(2):
                    transpose_f32(lambda ps_ap, pb, s=s, tc=tc: cx.op(
                        "dve", lambda e: e.tensor_copy(out=ku[s][0:64, tc * 128:(tc + 1) * 128], in_=ps_ap),
                        reads=[pb], writes=[b_ku[s]]), ctxk[:, tc, u * 64:(u + 1) * 64], 128, 64, [b_ctxk])
                raw, rawb = next_scr()
                for half in range(2):
                    pt, pb = mm_fm(wq_v, wq_b, u * 64, 64, D, hrhs, hbufs, half)
                    cx.op("act", lambda e: e.activation(out=raw[0:64, half * 512:(half + 1) * 512], in_=pt[0:64, :], func=AF.Copy),
                          reads=[pb], writes=[rawb])
                rope_unit(raw, rawb, lambda sl, s=s: qu[s][0:64, sl], b_qu[s])
            for qh in range(2):
                qs = slice(qh * 512, (qh + 1) * 512)
                for s in range(2):
                    et, etb = ET[etc % 2], b_ET[etc % 2]
                    etc += 1
                    o_ps, o_pb, d_ps, d_pb = attention_core(
                        [(lambda kc, s=s: ku[s][0:68, kc * 128:(kc + 1) * 128], [b_ku[s]], 68)],
                        [(qu[s][0:68, qs], [b_qu[s]])],
                        lambda kc: vdiff[:, kc, h * 128:(h + 1) * 128], [b_vdiff], et, etb, qh, dscale)
                    cx.op("dve", lambda e: e.reciprocal(out=asb[2][:, :], in_=d_ps[:, :]), reads=[d_pb], writes=[b_asb[2]])
                    cx.op("dve", lambda e: e.tensor_tensor(out=asb[s][:, :], in0=o_ps[:, :], in1=asb[2][:, :], op=ALU.mult),
                          reads=[o_pb, b_asb[2]], writes=[b_asb[s]])
                cx.op("dve", lambda e: e.scalar_tensor_tensor(out=asb[0][:, :], in0=asb[1][:, :], scalar=lamt[:, 260:261],
                                                              in1=asb[0][:, :], op0=ALU.mult, op1=ALU.add),
                      reads=[b_asb[0], b_asb[1], b_lam], writes=[b_asb[0]])
                cx.op("act", lambda e: e.activation(out=asb[1][:, :], in_=asb[0][:, :], func=AF.Square),
                      reads=[b_asb[0]], writes=[b_asb[1]])
                pt, pb = pr.next()
                cx.op("pe", lambda e: e.matmul(pt[:, :], ones_f[:, :], asb[1][:, :], start=True, stop=True),
                      reads=[b_asb[1], b_const], writes=[pb])
                rstd_from_psum([(pt, pb)], 128, lambda half: asb[2][:, :], b_asb[2])
                cx.op("dve", lambda e: e.tensor_tensor(out=asb[0][:, :], in0=asb[0][:, :], in1=asb[2][:, :], op=ALU.mult),
                      reads=[b_asb[0], b_asb[2]], writes=[b_asb[0]])
                cx.op("dve", lambda e: e.tensor_scalar(out=diff_oT[:, h, qs], in0=asb[0][:, :], scalar1=drv[:, 32:33], scalar2=None, op0=ALU.mult),
                      reads=[b_asb[0], b_drv], writes=[b_diff_o])
        if stop_after == "diff":
            for j in range(4):
                xt, xb = next_scr()
                cx.op("act", lambda e: e.activation(out=xt[:, :], in_=diff_oT[:, j, :], func=AF.Copy), reads=[b_diff_o], writes=[xb])
                cx.dma("sp", dbg_out[:, j * T:(j + 1) * T], xt[:, :], reads=[xb], is_output=True)
            break
        cx.fence()
        ar.off = mark
        zp = ar.take([128, 4, 272], F32); b_zp = Buf("zp")
        pa = ar.take([128, 4, 272], F32); b_pa = Buf("pa")
        pb2 = ar.take([128, 4, 272], F32); b_pb2 = Buf("pb2")
        dTt = ar.take([128, T], BF16); b_dT = Buf("dT")
        wpl = ar.take([128, 4, 128], BF16); b_wpl = Buf("wpl")
        cx.dma("pool", wpl, w_pool[l].rearrange("g c d -> c g d"), writes=[b_wpl])
        cx.op("dve", lambda e: e.memset(zp[:, :, :], 0.0), writes=[b_zp])
        wv, wb = load_w(w_in[l, :, C_PZ:C_PZ + 512], 16, 512)
        for g in range(4):
            for half in range(2):
                pt, pb = mm_fm(wv, wb, g * 128, 128, D, hrhs, hbufs, half)
                cx.op("act", lambda e: e.activation(out=zp[:, 2 * half:2 * half + 2, 8:264],
                                                    in_=pt[:, :].rearrange("p (a b) -> p a b", a=2), func=AF.Copy),
                      reads=[pb], writes=[b_zp])
            cx.op("dve", lambda e: e.tensor_scalar(out=zp[:, 1:4, 0:8], in0=zp[:, 0:3, 256:264], scalar1=flag[:, 0:1], scalar2=None, op0=ALU.mult),
                  reads=[b_zp, b_const], writes=[b_zp])
            cx.op("dve", lambda e: e.tensor_scalar(out=zp[:, 0:3, 264:272], in0=zp[:, 1:4, 8:16], scalar1=flag[:, 0:1], scalar2=None, op0=ALU.mult),
                  reads=[b_zp, b_const], writes=[b_zp])
            cx.op("dve", lambda e: e.tensor_tensor(out=pa[:, :, 1:272], in0=zp[:, :, 1:272], in1=zp[:, :, 0:271], op=ALU.add),
                  reads=[b_zp], writes=[b_pa])
            win, winb = pa, b_pa
            if g >= 1:
                cx.op("dve", lambda e: e.tensor_tensor(out=pb2[:, :, 2:271], in0=pa[:, :, 3:272], in1=pa[:, :, 1:270], op=ALU.add),
                      reads=[b_pa], writes=[b_pb2])
                win, winb = pb2, b_pb2
            if g >= 2:
                cx.op("dve", lambda e: e.tensor_tensor(out=pa[:, :, 4:269], in0=pb2[:, :, 6:271], in1=pb2[:, :, 2:267], op=ALU.add),
                      reads=[b_pb2], writes=[b_pa])
                win, winb = pa, b_pa
            if g >= 3:
                cx.op("dve", lambda e: e.tensor_tensor(out=pb2[:, :, 8:264], in0=pa[:, :, 12:268], in1=pa[:, :, 4:260], op=ALU.add),
                      reads=[b_pa], writes=[b_pb2])
                win, winb = pb2, b_pb2
            for half in range(2):
                pt, pb = pr.next()
                cx.op("pe", lambda e: e.matmul(pt[:, :], sel4[:, g * 128:(g + 1) * 128], invcnt[:, half * 512:(half + 1) * 512],
                                               start=True, stop=True), reads=[b_const], writes=[pb])
                other = pa if win is pb2 else pb2
                otherb = b_pa if win is pb2 else b_pb2
                cx.op("dve", lambda e: e.tensor_tensor(out=other[:, 2 * half:2 * half + 2, 8:264], in0=win[:, 2 * half:2 * half + 2, 8:264],
                                                       in1=pt[:, :].rearrange("p (a b) -> p a b", a=2), op=ALU.mult),
                      reads=[winb, pb], writes=[otherb])
                cx.op("dve", lambda e: e.tensor_tensor(out=dTt[:, half * 512:(half + 1) * 512].rearrange("p (a b) -> p a b", a=2),
                                                       in0=other[:, 2 * half:2 * half + 2, 8:264], in1=zp[:, 2 * half:2 * half + 2, 8:264], op=ALU.subtract),
                      reads=[otherb, b_zp], writes=[b_dT])
            for half in range(2):
                pt, pb = pr.next()
                cx.op("pe", lambda e: e.matmul(pt[:, :], wpl[:, g, :], dTt[:, half * 512:(half + 1) * 512], start=True, stop=True),
                      reads=[b_wpl, b_dT], writes=[pb])
                cx.op("dve", lambda e: e.tensor_scalar(out=pool_oT[:, g, half * 512:(half + 1) * 512], in0=pt[:, :],
                                                       scalar1=vecT[:, 134 + g:135 + g], scalar2=None, op0=ALU.mult),
                      reads=[pb, b_vecT], writes=[b_pool_o])
        if stop_after == "pool":
            for j in range(4):
                xt, xb = next_scr()
                cx.op("act", lambda e: e.activation(out=xt[:, :], in_=pool_oT[:, j, :], func=AF.Copy), reads=[b_pool_o], writes=[xb])
                cx.dma("sp", dbg_out[:, j * T:(j + 1) * T], xt[:, :], reads=[xb], is_output=True)
            break
        cx.fence()
        ar.off = mark

        ckvT = ar.take([128, 2, TK], BF16); b_ckvT = Buf("ckvT")
        krT = ar.take([128, TK], BF16); b_krT = Buf("krT")
        qanT = ar.take([128, 4, T], BF16); b_qan = Buf("qanT")
        ET = [ar.take([128, 10, 512], BF16) for _ in range(2)]; b_ET = [Buf("ET0"), Buf("ET1")]
        qn = ar.take([128, T], BF16); b_qn = Buf("qn")
        qr = ar.take([128, T], BF16); b_qr = Buf("qr")
        kn = ar.take([128, TK], BF16); b_kn = Buf("kn")
        vm = ar.take([128, 10, 128], BF16); b_vm = Buf("vm")
        cst = ar.take([128, 2, 320], F32); b_cst = Buf("cst")
        rsm = ar.take([128, 512], F32); b_rsm = Buf("rsm")
        cx.dma("pool", qr[64:68, :], qaug_d, writes=[b_qr])
        cx.dma("pool", krT[64:68, :], kaug_d, writes=[b_krT])
        cx.dma("sp", cst[:, :, 0:256], ctx_ckv[l].rearrange("(c p) f -> p c f", p=128), writes=[b_cst])
        cx.dma("sp", cst[:, :, 256:320], ctx_krope[l].rearrange("(c p) f -> p c f", p=128), writes=[b_cst])
        for tc in range(2):
            for c in range(2):
                transpose_f32(lambda ps_ap, pb, tc=tc, c=c: cx.op(
                    "act", lambda e: e.activation(out=ckvT[:, c, tc * 128:(tc + 1) * 128], in_=ps_ap, func=AF.Copy),
                    reads=[pb], writes=[b_ckvT]), cst[:, tc, c * 128:(c + 1) * 128], 128, 128, [b_cst])
            transpose_f32(lambda ps_ap, pb, tc=tc: cx.op(
                "act", lambda e: e.activation(out=krT[0:64, tc * 128:(tc + 1) * 128], in_=ps_ap, func=AF.Copy),
                reads=[pb], writes=[b_krT]), cst[:, tc, 256:320], 128, 64, [b_cst])
        wv, wb = load_w(w_in[l, :, 0:512], 16, 512)
        wv2, wb2 = load_w(w_in[l, :, 512:832], 16, 320)
        for (wv_, wb_, nch, gcol, nfeat, is_q) in ((wv, wb, 4, 128, 512, True), (wv2, wb2, 2, 132, 256, False)):
            acc = pr.hold(2)
            for c in range(nch):
                for half in range(2):
                    pt, pb = mm_fm(wv_, wb_, c * 128, 128, D, hrhs, hbufs, half)
                    sq, sqb = next_scr()
                    cx.op("act", lambda e: e.activation(out=sq[:, 0:512], in_=pt[:, :], func=AF.Square), reads=[pb], writes=[sqb])
                    cx.op("pe", lambda e: e.matmul(acc[half][0][:, :], ones_f[:, :], sq[:, 0:512], start=(c == 0), stop=(c == nch - 1)),
                          reads=[sqb, b_const], writes=[acc[half][1]])
            rstd_from_psum(acc, nfeat, lambda half: rs[:, half * 512:(half + 1) * 512], b_rs)
            pr.release(acc)
            for c in range(nch):
                raw, rawb = next_scr()
                for half in range(2):
                    sl = slice(half * 512, (half + 1) * 512)
                    pt, pb = mm_fm(wv_, wb_, c * 128, 128, D, hrhs, hbufs, half)
                    cx.op("dve", lambda e: e.tensor_tensor(out=raw[:, sl], in0=pt[:, :], in1=rs[:, sl], op=ALU.mult),
                          reads=[pb, b_rs], writes=[rawb])
                    if is_q:
                        cx.op("dve", lambda e: e.tensor_scalar(out=qanT[:, c, sl], in0=raw[:, sl], scalar1=vecT[:, gcol + c:gcol + c + 1],
                                                               scalar2=None, op0=ALU.mult), reads=[rawb, b_vecT], writes=[b_qan])
                    else:
                        cx.op("dve", lambda e: e.tensor_scalar(out=raw[:, sl], in0=raw[:, sl], scalar1=vecT[:, gcol + c:gcol + c + 1],
                                                               scalar2=None, op0=ALU.mult), reads=[rawb, b_vecT], writes=[rawb])
                        cx.op("act", lambda e: e.activation(out=ckvT[:, c, PAST + half * 512:PAST + (half + 1) * 512], in_=raw[:, sl], func=AF.Copy),
                              reads=[rawb], writes=[b_ckvT])
                if not is_q:
                    out_transposes(raw, rawb, 128, lambda tc, c=c: o_ckv[l, tc * 128:(tc + 1) * 128, c * 128:(c + 1) * 128])
        raw, rawb = next_scr()
        for half in range(2):
            pt, pb = mm_fm(wv2, wb2, 256, 64, D, hrhs, hbufs, half)
            cx.op("act", lambda e: e.activation(out=raw[0:64, half * 512:(half + 1) * 512], in_=pt[0:64, :], func=AF.Copy),
                  reads=[pb], writes=[rawb])
        out_transposes(raw, rawb, 64, lambda tc: o_krope[l, tc * 128:(tc + 1) * 128, :])
        rope_unit(raw, rawb, lambda sl: krT[0:64, PAST + sl.start:PAST + sl.stop], b_krT)
        wqb_v, wqb_b = load_w(w_qb[l], 4, 1536)
        wkv_v, wkv_b = load_w(w_kvb[l], 2, 2048, small=True)
        qan_rhs = lambda kc, half: qanT[:, kc, half * 512:(half + 1) * 512]
        qan_bufs = lambda kc: [b_qan]
        mscale = 192.0 ** -0.5
        etc = 0
        for h in range(8):
            for half in range(2):
                pt, pb = mm_fm(wqb_v, wqb_b, h * 192, 128, 512, qan_rhs, qan_bufs, half)
                cx.op("act", lambda e: e.activation(out=qn[:, half * 512:(half + 1) * 512], in_=pt[:, :], func=AF.Copy),
                      reads=[pb], writes=[b_qn])
            raw, rawb = next_scr()
            for half in range(2):
                pt, pb = mm_fm(wqb_v, wqb_b, h * 192 + 128, 64, 512, qan_rhs, qan_bufs, half)
                cx.op("act", lambda e: e.activation(out=raw[0:64, half * 512:(half + 1) * 512], in_=pt[0:64, :], func=AF.Copy),
                      reads=[pb], writes=[rawb])
            rope_unit(raw, rawb, lambda sl: qr[0:64, sl], b_qr)
            for (c0, n) in ((0, 512), (512, 512), (1024, 256)):
                pt, pb = pr.next()
                for kc in range(2):
                    cx.op("pe", lambda e: e.matmul(pt[:, 0:n], wkv_v[:, kc, h * 256:h * 256 + 128], ckvT[:, kc, c0:c0 + n],
                                                   start=(kc == 0), stop=(kc == 1)), reads=[wkv_b, b_ckvT], writes=[pb])
                cx.op("act", lambda e: e.activation(out=kn[:, c0:c0 + n], in_=pt[:, 0:n], func=AF.Copy), reads=[pb], writes=[b_kn])
            for g4 in range(3):
                pt, pb = pr.next()
                nb = 4 if g4 < 2 else 2
                for bi in range(nb):
                    tkc = g4 * 4 + bi
                    for kc in range(2):
                        cx.op("pe", lambda e: e.matmul(pt[:, bi * 128:(bi + 1) * 128], ckvT[:, kc, tkc * 128:(tkc + 1) * 128],
                                                       wkv_v[:, kc, h * 256 + 128:h * 256 + 256], start=(kc == 0), stop=(kc == 1)),
                              reads=[wkv_b, b_ckvT], writes=[pb])
                cx.op("act", lambda e: e.activation(out=vm[:, g4 * 4:g4 * 4 + nb, :], in_=pt[:, 0:nb * 128].rearrange("p (a b) -> p a b", a=nb),
                                                    func=AF.Copy), reads=[pb], writes=[b_vm])
            for qh in range(2):
                qs = slice(qh * 512, (qh + 1) * 512)
                et, etb = ET[etc % 2], b_ET[etc % 2]
                etc += 1
                o_ps, o_pb, d_ps, d_pb = attention_core(
                    [(lambda kc: kn[:, kc * 128:(kc + 1) * 128], [b_kn], 128),
                     (lambda kc: krT[0:68, kc * 128:(kc + 1) * 128], [b_krT], 68)],
                    [(qn[:, qs], [b_qn]), (qr[0:68, qs], [b_qr])],
                    lambda kc: vm[:, kc, :], [b_vm], et, etb, qh, mscale)
                cx.op("dve", lambda e: e.reciprocal(out=rsm[:, :], in_=d_ps[:, :]), reads=[d_pb], writes=[b_rsm])
                cx.op("dve", lambda e: e.tensor_tensor(out=mla_oT[:, h, qs], in0=o_ps[:, :], in1=rsm[:, :], op=ALU.mult),
                      reads=[o_pb, b_rsm], writes=[b_mla_o])
        if stop_after == "mla":
            for j in range(8):
                xt, xb = next_scr()
                cx.op("act", lambda e: e.activation(out=xt[:, :], in_=mla_oT[:, j, :], func=AF.Copy), reads=[b_mla_o], writes=[xb])
                cx.dma("sp", dbg_out[:, j * T:(j + 1) * T], xt[:, :], reads=[xb], is_output=True)
            break
        cx.fence()
        ar.off = mark
        mergedT = ar.take([128, 16, T], BF16); b_merged = [Buf("mg%d" % j) for j in range(16)]
        accg = ar.take([128, 8, 512], F32); b_accg = [Buf("accg%d" % i) for i in range(8)]
        sgt = [ar.take([128, 512], F32) for _ in range(2)]; b_sgt = [Buf("sg0"), Buf("sg1")]
        branches = ((w_br_mla[l], 1024, mla_oT, b_mla_o), (w_br_pool[l], 512, pool_oT, b_pool_o),
                    (w_br_diff[l], 512, diff_oT, b_diff_o))
        sgc = 0
        for jg in range(4):
            for b in range(3):
                gv, gb = load_w(w_in[l, :, C_GL + b * 2048 + jg * 512:C_GL + b * 2048 + (jg + 1) * 512], 16, 512)
                wbr, Kb, brT, brb = branches[b]
                bv, bb = load_w(wbr[:, jg * 512:(jg + 1) * 512], Kb // 128, 512, small=True)
                for jj in range(4):
                    j = jg * 4 + jj
                    for half in range(2):
                        sl = slice(half * 512, (half + 1) * 512)
                        ai = jj * 2 + half
                        pg, pgb = mm_fm(gv, gb, jj * 128, 128, D, hrhs, hbufs, half)
                        s_t, s_b = sgt[sgc % 2], b_sgt[sgc % 2]
                        sgc += 1
                        cx.op("act", lambda e: e.activation(out=s_t[:, :], in_=pg[:, :], func=AF.Sigmoid), reads=[pgb], writes=[s_b])
                        pbr, pbrb = mm_fm(bv, bb, jj * 128, 128, Kb, lambda kc, hf: brT[:, kc, hf * 512:(hf + 1) * 512],
                                          lambda kc: [brb], half)
                        if b == 0:
                            cx.op("dve", lambda e: e.tensor_tensor(out=accg[:, ai, :], in0=s_t[:, :], in1=pbr[:, :], op=ALU.mult),
                                  reads=[s_b, pbrb], writes=[b_accg[ai]])
                        else:
                            cx.op("dve", lambda e: e.tensor_tensor(out=s_t[:, :], in0=s_t[:, :], in1=pbr[:, :], op=ALU.mult),
                                  reads=[s_b, pbrb], writes=[s_b])
                            if b == 1:
                                cx.op("dve", lambda e: e.tensor_tensor(out=accg[:, ai, :], in0=accg[:, ai, :], in1=s_t[:, :], op=ALU.add),
                                      reads=[s_b, b_accg[ai]], writes=[b_accg[ai]])
                            else:
                                cx.op("dve", lambda e: e.tensor_tensor(out=mergedT[:, j, sl], in0=accg[:, ai, :], in1=s_t[:, :], op=ALU.add),
                                      reads=[s_b, b_accg[ai]], writes=[b_merged[j]])
        if stop_after == "merge":
            for j in range(16):
                xt, xb = next_scr()
                cx.op("act", lambda e: e.activation(out=xt[:, :], in_=mergedT[:, j, :], func=AF.Copy), reads=[b_merged[j]], writes=[xb])
                cx.dma("sp", dbg_out[:, j * T:(j + 1) * T], xt[:, :], reads=[xb], is_output=True)
            break
        for jg in range(4):
            wv, wb = load_w(w_out[l, :, jg * 512:(jg + 1) * 512], 16, 512)
            for jj in range(4):
                j = jg * 4 + jj
                xt, xb = next_scr()
                cx.dma("sp", xt[:, :], xs[j], reads=[b_xs[j]], writes=[xb])
                for half in range(2):
                    sl = slice(half * 512, (half + 1) * 512)
                    pt, pb = mm_fm(wv, wb, jj * 128, 128, D, lambda kc, hf: mergedT[:, kc, hf * 512:(hf + 1) * 512],
                                   lambda kc: [b_merged[kc]], half)
                    cx.op("dve", lambda e: e.scalar_tensor_tensor(out=xt[:, sl], in0=pt[:, :], scalar=modT[:, 32 + j:33 + j],
                                                                  in1=xt[:, sl], op0=ALU.mult, op1=ALU.add),
                          reads=[pb, b_modT, xb], writes=[xb])
                cx.dma("sp", xs[j], xt[:, :], reads=[xb], writes=[b_xs[j]])
        norm_x(l, 2)
        apply_norm(l, 2)
        cx.fence()
        ar.reset()
        if stop_after == "attn":
            for j in range(16):
                xt, xb = next_scr()
                cx.dma("sp", xt[:, :], xs[j], reads=[b_xs[j]], writes=[xb])
                cx.dma("sp", dbg_out[:, j * T:(j + 1) * T], xt[:, :], reads=[xb], is_output=True)
            break
        skT = ar.take([128, 2, 128], BF16); b_skT = Buf("skT")
        skst = ar.take([128, 2, 128], F32); b_skst = Buf("skst")
        qT = ar.take([128, 16, 512], BF16); b_qT = Buf("qT")
        s1 = ar.take([128, 16, 128], F32); b_s1 = Buf("s1")
        s2 = ar.take([128, 16, 128], F32); b_s2 = Buf("s2")
        vtop = ar.take([128, 16, 16], F32); b_vtop = Buf("vtop")
        ix = ar.take([128, 16, 16], U32); b_ix = Buf("ix")
        ixf = ar.take([128, 16, 16], F32); b_ixf = Buf("ixf")
        best = ar.take([128, 8, 16], F32); b_best = Buf("best")
        sel = ar.take([128, 8, 16], U32); b_sel = Buf("sel")
        ru = ar.take([128, 2, 8, 16], U32); b_ru = Buf("ru")
        rf = ar.take([128, 2, 8, 16], F32); b_rf = Buf("rf")
        ab = ar.take([128, 2, 8, 16], F32); b_ab = Buf("ab")
        idxf = ar.take([128, 128], F32); b_idxf = Buf("idxf")
        idxi = ar.take([128, 128], I32); b_idxi = Buf("idxi")
        gw = ar.take([128, 8, 16], F32); b_gw = Buf("gw")
        sm = ar.take([128, 32], F32); b_sm = Buf("sm")
        h2tok = ar.take([128, 2048], BF16); b_h2tok = Buf("h2tok")
        junk = ar.take([128, 2048], BF16); b_junk = Buf("junk")
        actv = ar.take([128, 128], F32); b_actv = Buf("actv")
        wgt = ar.take([128, 128], F32); b_wgt = Buf("wgt")
        NG = 4
        gbuf = [ar.take([128, 2048], BF16) for _ in range(NG)]; b_gbuf = [Buf("g%d" % i) for i in range(NG)]
        diag = [ar.take([128, 128], BF16) for _ in range(2)]; b_diag = [Buf("dg0"), Buf("dg1")]
        otok = [ar.take([128, 512], F32) for _ in range(2)]; b_otok = [Buf("ot0"), Buf("ot1")]
        xupd = [ar.take([128, 4, 128], F32) for _ in range(2)]; b_xupd = [Buf("xu0"), Buf("xu1")]
        pu_flat = peer_u.rearrange("l e d -> (l e) d")
        pv_flat = peer_v.rearrange("l e d -> (l e) d")
        cx.dma("sp", skst, subkeys[l].rearrange("p k c -> k p c"), writes=[b_skst])
        for p in range(2):
            transpose_f32(lambda ps_ap, pb, p=p: cx.op("act", lambda e: e.activation(out=skT[:, p, :], in_=ps_ap, func=AF.Copy),
                                                       reads=[pb], writes=[b_skT]), skst[:, p, :], 128, 128, [b_skst])
        gcnt = 0
        for th in range(2):
            for jg in range(4):
                wv, wb = load_w(w_peer_q[l, :, jg * 512:(jg + 1) * 512], 16, 512)
                for jj in range(4):
                    pt, pb = mm_fm(wv, wb, jj * 128, 128, D, hrhs, hbufs, th)
                    cx.op("act", lambda e: e.activation(out=qT[:, jg * 4 + jj, :], in_=pt[:, :], func=AF.Copy), reads=[pb], writes=[b_qT])
            for tb4 in range(4):
                tb = th * 4 + tb4
                tsl = slice(tb4 * 128, (tb4 + 1) * 128)
                banks = pr.hold(4)
                for hp in range(16):
                    bt, bbf = banks[hp // 4]
                    cx.op("pe", lambda e: e.matmul(bt[:, (hp % 4) * 128:(hp % 4 + 1) * 128], qT[:, hp, tsl], skT[:, hp % 2, :],
                                                   start=True, stop=True), reads=[b_qT, b_skT], writes=[bbf])
                for q4 in range(4):
                    bt, bbf = banks[q4]
                    cx.op("act", lambda e: e.activation(out=s1[:, q4 * 4:(q4 + 1) * 4, :], in_=bt[:, :].rearrange("p (a b) -> p a b", a=4),
                                                        func=AF.Copy), reads=[bbf], writes=[b_s1])
                pr.release(banks)
                for hp in range(16):
                    cx.op("dve", lambda e: e.max(out=vtop[:, hp, 0:8], in_=s1[:, hp, :]), reads=[b_s1], writes=[b_vtop])
                    cx.op("dve", lambda e: e.max_index(out=ix[:, hp, 0:8], in_max=vtop[:, hp, 0:8], in_values=s1[:, hp, :]),
                          reads=[b_s1, b_vtop], writes=[b_ix])
                    cx.op("dve", lambda e: e.match_replace(out=s2[:, hp, :], in_to_replace=vtop[:, hp, 0:8], in_values=s1[:, hp, :],
                                                           imm_value=-1e30), reads=[b_s1, b_vtop], writes=[b_s2])
                    cx.op("dve", lambda e: e.max(out=vtop[:, hp, 8:16], in_=s2[:, hp, :]), reads=[b_s2], writes=[b_vtop])
                    cx.op("dve", lambda e: e.max_index(out=ix[:, hp, 8:16], in_max=vtop[:, hp, 8:16], in_values=s2[:, hp, :]),
                          reads=[b_s2, b_vtop], writes=[b_ix])
                cand = s1.rearrange("p (h a) (b c) -> p h a b c", a=2, b=8)
                cand4 = s1.rearrange("p (h a) (b c) -> p h (a b) c", a=2, b=8)
                cand2_4 = s2.rearrange("p (h a) (b c) -> p h (a b) c", a=2, b=8)
                candf = s1.rearrange("p (h a) k -> p h (a k)", a=2)
                cand2f = s2.rearrange("p (h a) k -> p h (a k)", a=2)
                vt4 = vtop.rearrange("p (h a) k -> p h a k", a=2)
                cx.op("dve", lambda e: e.tensor_tensor(out=cand4, in0=vt4[:, :, 0, :].unsqueeze(3).to_broadcast([128, 8, 16, 16]),
                                                       in1=vt4[:, :, 1, :].unsqueeze(2).to_broadcast([128, 8, 16, 16]), op=ALU.add),
                      reads=[b_vtop], writes=[b_s1])
                for h in range(8):
                    cx.op("dve", lambda e: e.max(out=best[:, h, 0:8], in_=candf[:, h, :]), reads=[b_s1], writes=[b_best])
                    cx.op("dve", lambda e: e.max_index(out=sel[:, h, 0:8], in_max=best[:, h, 0:8], in_values=candf[:, h, :]),
                          reads=[b_s1, b_best], writes=[b_sel])
                    cx.op("dve", lambda e: e.match_replace(out=cand2f[:, h, :], in_to_replace=best[:, h, 0:8], in_values=candf[:, h, :],
                                                           imm_value=-1e30), reads=[b_s1, b_best], writes=[b_s2])
                    cx.op("dve", lambda e: e.max(out=best[:, h, 8:16], in_=cand2f[:, h, :]), reads=[b_s2], writes=[b_best])
                    cx.op("dve", lambda e: e.max_index(out=sel[:, h, 8:16], in_max=best[:, h, 8:16], in_values=cand2f[:, h, :]),
                          reads=[b_s2, b_best], writes=[b_sel])
                cx.op("dve", lambda e: e.tensor_single_scalar(out=ru[:, 0], in_=sel, scalar=4, op=ALU.logical_shift_right),
                      reads=[b_sel], writes=[b_ru])
                cx.op("dve", lambda e: e.tensor_single_scalar(out=ru[:, 1], in_=sel, scalar=15, op=ALU.bitwise_and),
                      reads=[b_sel], writes=[b_ru])
                cx.op("dve", lambda e: e.tensor_copy(out=rf, in_=ru), reads=[b_ru], writes=[b_rf])
                cx.op("dve", lambda e: e.tensor_copy(out=ixf, in_=ix), reads=[b_ix], writes=[b_ixf])
                ixf4 = ixf.rearrange("p (h a) k -> p h a k", a=2)
                for a in range(2):
                    cx.op("dve", lambda e: e.tensor_tensor(out=cand4, in0=iota16[:, :].unsqueeze(1).unsqueeze(1).to_broadcast([128, 8, 16, 16]),
                                                           in1=rf[:, a].unsqueeze(3).to_broadcast([128, 8, 16, 16]), op=ALU.is_equal),
                          reads=[b_rf, b_const], writes=[b_s1])
                    cx.op("dve", lambda e: e.tensor_tensor(out=cand2_4, in0=cand4,
                                                           in1=ixf4[:, :, a, :].unsqueeze(2).to_broadcast([128, 8, 16, 16]), op=ALU.mult),
                          reads=[b_s1, b_ixf], writes=[b_s2])
                    cx.op("dve", lambda e: e.tensor_reduce(out=ab[:, a], in_=cand2_4, axis=AX.X, op=ALU.add),
                          reads=[b_s2], writes=[b_ab])
                cx.op("dve", lambda e: e.scalar_tensor_tensor(out=idxf.rearrange("p (h k) -> p h k", h=8), in0=ab[:, 0], scalar=128.0,
                                                              in1=ab[:, 1], op0=ALU.mult, op1=ALU.add), reads=[b_ab], writes=[b_idxf])
                cx.op("dve", lambda e: e.tensor_scalar(out=idxf, in0=idxf, scalar1=float(l * 16384), scalar2=None, op0=ALU.add),
                      reads=[b_idxf], writes=[b_idxf])
                cx.op("dve", lambda e: e.tensor_copy(out=idxi, in_=idxf), reads=[b_idxf], writes=[b_idxi])
                cx.op("dve", lambda e: e.tensor_scalar(out=sm[:, 0:8], in0=best[:, :, 0], scalar1=-1.0, scalar2=None, op0=ALU.mult),
                      reads=[b_best], writes=[b_sm])
                for h in range(8):
                    cx.op("act", lambda e: e.activation(out=gw[:, h, :], in_=best[:, h, :], func=AF.Exp, bias=sm[:, h:h + 1], scale=1.0,
                                                        accum_out=sm[:, 8 + h:9 + h]), reads=[b_best, b_sm], writes=[b_gw, b_sm])
                cx.op("dve", lambda e: e.reciprocal(out=sm[:, 16:24], in_=sm[:, 8:16]), reads=[b_sm], writes=[b_sm])
                cx.op("dve", lambda e: e.tensor_tensor(out=gw, in0=gw, in1=sm[:, 16:24].unsqueeze(2).to_broadcast([128, 8, 16]), op=ALU.mult),
                      reads=[b_gw, b_sm], writes=[b_gw])
                for j8 in range(2):
                    pt, pb = pr.next()
                    ptb = pt[:, :].bitcast(BF16)
                    for jj in range(8):
                        j = j8 * 8 + jj
                        cx.op("pe", lambda e: e.transpose(ptb[:, jj * 128:(jj + 1) * 128], hT[:, j, tb * 128:(tb + 1) * 128], ident_b[:, :]),
                              reads=[b_hT[j], b_const], writes=[pb])
                    cx.op("act", lambda e: e.activation(out=h2tok[:, j8 * 1024:(j8 + 1) * 1024], in_=ptb, func=AF.Copy),
                          reads=[pb], writes=[b_h2tok])
                for kk in range(128):
                    gi = gcnt % NG
                    gcnt += 1
                    cx.dma("pool", gbuf[gi][:, :], pu_flat, indirect=idxi[:, kk:kk + 1], reads=[b_idxi], writes=[b_gbuf[gi]])
                    cx.op("dve", lambda e: e.scalar_tensor_tensor(out=junk[:, :], in0=gbuf[gi][:, :], scalar=1.0, in1=h2tok[:, :],
                                                                  op0=ALU.mult, op1=ALU.mult, accum_out=actv[:, kk:kk + 1]),
                          reads=[b_gbuf[gi], b_h2tok], writes=[b_junk, b_actv])
                cx.op("act", lambda e: e.activation(out=wgt[:, :], in_=actv[:, :], func=AF.Gelu), reads=[b_actv], writes=[b_wgt])
                cx.op("dve", lambda e: e.tensor_tensor(out=wgt[:, :], in0=wgt[:, :], in1=gw.rearrange("p h k -> p (h k)"), op=ALU.mult),
                      reads=[b_wgt, b_gw], writes=[b_wgt])
                accb = pr.hold(4)
                for kk in range(128):
                    gi = gcnt % NG
                    gcnt += 1
                    cx.dma("pool", gbuf[gi][:, :], pv_flat, indirect=idxi[:, kk:kk + 1], reads=[b_idxi], writes=[b_gbuf[gi]])
                    dg, dgb = diag[kk % 2], b_diag[kk % 2]
                    cx.op("dve", lambda e: e.tensor_scalar(out=dg[:, :], in0=ident_b[:, :], scalar1=wgt[:, kk:kk + 1], scalar2=None, op0=ALU.mult),
                          reads=[b_wgt, b_const], writes=[dgb])
                    for dq in range(4):
                        cx.op("pe", lambda e: e.matmul(accb[dq][0][:, :], dg[:, :], gbuf[gi][:, dq * 512:(dq + 1) * 512],
                                                       start=(kk == 0), stop=(kk == 127)), reads=[dgb, b_gbuf[gi]], writes=[accb[dq][1]])
                for dq in range(4):
                    ot, otb = otok[dq % 2], b_otok[dq % 2]
                    xu, xub = xupd[dq % 2], b_xupd[dq % 2]
                    cx.op("act", lambda e: e.activation(out=ot[:, :], in_=accb[dq][0][:, :], func=AF.Copy), reads=[accb[dq][1]], writes=[otb])
                    cx.dma("sp", xu, xs[dq * 4:(dq + 1) * 4, :, tb * 128:(tb + 1) * 128].rearrange("j p t -> p j t"),
                           reads=[b_xs[dq * 4 + i] for i in range(4)], writes=[xub])
                    for jj in range(4):
                        j = dq * 4 + jj
                        transpose_f32(lambda ps_ap, pb, jj=jj, j=j: cx.op(
                            "dve", lambda e: e.scalar_tensor_tensor(out=xu[:, jj, :], in0=ps_ap, scalar=modT[:, 80 + j:81 + j],
                                                                    in1=xu[:, jj, :], op0=ALU.mult, op1=ALU.add),
                            reads=[pb, b_modT, xub], writes=[xub]), ot[:, jj * 128:(jj + 1) * 128], 128, 128, [otb])
                    cx.dma("sp", xs[dq * 4:(dq + 1) * 4, :, tb * 128:(tb + 1) * 128].rearrange("j p t -> p j t"), xu,
                           reads=[xub], writes=[b_xs[dq * 4 + i] for i in range(4)])
                pr.release(accb)
        cx.fence()
        if stop_after == "layer":
            for j in range(16):
                xt, xb = next_scr()
                cx.dma("sp", xt[:, :], xs[j], reads=[b_xs[j]], writes=[xb])
                cx.dma("sp", dbg_out[:, j * T:(j + 1) * T], xt[:, :], reads=[xb], is_output=True)
            break
    else:
        norm_x(L, 1)
        for j in range(16):
            xt, xb = next_scr()
            cx.dma("sp", xt[:, :], xs[j], reads=[b_xs[j]], writes=[xb])
            cx.op("dve", lambda e: e.tensor_tensor(out=xt[:, :], in0=xt[:, :], in1=rs[:, :], op=ALU.mult), reads=[xb, b_rs], writes=[xb])
            cx.op("dve", lambda e: e.tensor_scalar(out=xt[:, :], in0=xt[:, :], scalar1=gvT[:, 16 + j:17 + j], scalar2=None, op0=ALU.mult),
                  reads=[xb, b_gvT], writes=[xb])
            for tc in range(8):
                st, sbf = next_stg()
                transpose_f32(lambda ps_ap, pb, st=st, sbf=sbf: cx.op("act", lambda e: e.activation(out=st[:, :], in_=ps_ap, func=AF.Copy),
                                                                      reads=[pb], writes=[sbf]), xt[:, tc * 128:(tc + 1) * 128], 128, 128, [xb])
                cx.dma("sp", y_out[tc * 128:(tc + 1) * 128, j * 128:(j + 1) * 128], st[:, :], reads=[sbf], is_output=True)

    cx.finish()
    es.close()
    return nc, in_names


def _rope_tables(sample):
    f = np.arange(64)
    axis = f // 32
    jj = f % 16
    hf = (f % 32) // 16
    t = np.arange(T)
    if sample:
        pos = np.where(axis[:, None] == 0, (t // 64)[None, :], (t % 64)[None, :]).astype(np.float32)
        inv = (10000.0 ** (-(jj.astype(np.float32)) / 16.0)).astype(np.float32)
        ang = pos * inv[:, None]
        c = np.cos(ang).astype(np.float32)
        s = np.sin(ang).astype(np.float32)
        s = np.where(hf[:, None] == 0, -s, s).astype(np.float32)
    else:
        c = np.ones((64, T), np.float32)
        s = np.zeros((64, T), np.float32)
    partner = np.where(hf == 0, f + 16, f - 16)
    perm = np.zeros((64, 64), np.float32)
    perm[partner, f] = 1.0
    return c, s, perm


def _core_consts(sample):
    c, s, perm = _rope_tables(sample)
    t = np.arange(T)
    qaug = np.zeros((4, T), np.float32)
    kaug = np.zeros((4, TK), np.float32)
    seglen = T if sample else 256
    if not sample:
        seg = t // 256
        for sp in range(4):
            qaug[sp] = -(seg == sp).astype(np.float32)
            kaug[sp, :PAST] = BIG
            kaug[sp, PAST:] = BIG * (seg != sp).astype(np.float32)
    invcnt = np.zeros((4, T), np.float32)
    tt = t % seglen
    for gi, w in enumerate((2, 4, 8, 16)):
        lo = np.maximum(tt - w // 2, 0)
        hi = np.minimum(tt + (w - 1) // 2, seglen - 1)
        invcnt[gi] = 1.0 / (hi - lo + 1).astype(np.float32)
    sel4 = np.zeros((4, 4, 128), np.float32)
    for g in range(4):
        sel4[g, g, :] = 1.0
    return {
        "ident": np.eye(128, dtype=np.float32), "perm64": perm, "ropec": c, "ropes": s, "qaug": qaug, "kaug": kaug,
        "flag": np.full((128, 1), 1.0 if sample else 0.0, np.float32), "invcnt": invcnt,
        "sel4": sel4.reshape(4, 512), "iota16": np.tile(np.arange(16, dtype=np.float32), (128, 1)),
    }


def make_in_maps(inp, L=4, cores=range(8), names=None):
    f = lambda a: np.ascontiguousarray(np.asarray(a, dtype=np.float32))
    vecs = np.zeros((L, 144, 128), np.float32)
    for l in range(L):
        vecs[l, 0:96] = f(inp["b_mod"])[l].reshape(96, 128)
        vecs[l, 96:112] = f(inp["g_norm1"])[l].reshape(16, 128)
        vecs[l, 112:128] = f(inp["g_norm2"])[l].reshape(16, 128)
        vecs[l, 128:132] = f(inp["g_qnorm"])[l].reshape(4, 128)
        vecs[l, 132:134] = f(inp["g_kvnorm"])[l].reshape(2, 128)
        vecs[l, 134:138] = f(inp["pool_scale"])[l].reshape(4, 128)
        vecs[l, 138] = f(inp["g_diffnorm"])[l]
    shared = {
        "w_mod": f(inp["w_mod"])[:L], "vecs": vecs, "w_in": f(inp["w_in"])[:L], "w_qb": f(inp["w_qb"])[:L],
        "w_kvb": f(inp["w_kvb"])[:L], "w_pool": f(inp["w_pool"])[:L],
        "diff_lambda": f(inp["diff_lambda"])[:L].reshape(L, 256),
        "w_br_mla": f(inp["w_br_mla"])[:L], "w_br_pool": f(inp["w_br_pool"])[:L], "w_br_diff": f(inp["w_br_diff"])[:L],
        "w_out": f(inp["w_out"])[:L], "w_peer_q": f(inp["w_peer_q"])[:L], "peer_subkeys": f(inp["peer_subkeys"])[:L],
    }
    if names is None or "peer_u" in names:
        shared["peer_u"] = f(inp["peer_u"])[:L]
        shared["peer_v"] = f(inp["peer_v"])[:L]
    xp = f(inp["x_prompt"]); xsm = f(inp["x_sample"])
    cs = {True: _core_consts(True), False: _core_consts(False)}
    maps = []
    for c in cores:
        sample = c >= 4
        m = dict(shared)
        m.update(cs[sample])
        gv = np.zeros((32, 128), np.float32)
        gv[16:32] = f(inp["g_final"]).reshape(16, 128)
        if not sample:
            m["x"] = xp[4 * c:4 * c + 4].reshape(T, D)
            gv[0:16] = f(inp["c_ctx"]).reshape(16, 128)
            m["ctx_ckv"] = np.zeros((L, PAST, 256), np.float32)
            m["ctx_krope"] = np.zeros((L, PAST, 64), np.float32)
            m["ctx_dk"] = np.zeros((L, PAST, 512), np.float32)
            m["ctx_dv"] = np.zeros((L, PAST, 512), np.float32)
        else:
            b = (c - 4) % 2
            m["x"] = xsm[b]
            gv[0:16] = f(inp["c"])[b].reshape(16, 128)
            m["ctx_ckv"] = f(inp["cache_mla_ckv"])[b, :L]
            m["ctx_krope"] = f(inp["cache_mla_krope"])[b, :L]
            m["ctx_dk"] = f(inp["cache_diff_k"])[b, :L].reshape(L, PAST, 512)
            m["ctx_dv"] = f(inp["cache_diff_v"])[b, :L].reshape(L, PAST, 512)
        m["gvec"] = gv
        if names is not None:
            m = {k: v for k, v in m.items() if k in names}
        maps.append(m)
    return maps


def kernel(**inp):
    L = 4
    nc, names = build_program(L)
    maps = make_in_maps(inp, L, names=names)
    res = run_bass_kernel_spmd(nc, maps, core_ids=list(range(8)))
    r = res.results
    y_prompt = np.zeros((16, 256, D), np.float32)
    st_ckv = np.zeros((16, L, 256, 256), np.float32)
    st_krope = np.zeros((16, L, 256, 64), np.float32)
    st_k = np.zeros((16, L, 256, 4, 128), np.float32)
    st_v = np.zeros((16, L, 256, 4, 128), np.float32)
    for c in range(4):
        y_prompt[4 * c:4 * c + 4] = r[c]["y"].reshape(4, 256, D)
        for l in range(L):
            st_ckv[4 * c:4 * c + 4, l] = r[c]["o_ckv"][l].reshape(4, 256, 256)
            st_krope[4 * c:4 * c + 4, l] = r[c]["o_krope"][l].reshape(4, 256, 64)
            st_k[4 * c:4 * c + 4, l] = r[c]["o_dk"][l].reshape(4, 256, 4, 128)
            st_v[4 * c:4 * c + 4, l] = r[c]["o_dv"][l].reshape(4, 256, 4, 128)
    y_sample = np.stack([r[4]["y"], r[5]["y"]], axis=0)
    return (y_prompt, y_sample, st_ckv, st_krope, st_k, st_v)
```

```python
import contextlib
import math

import numpy as np
import concourse.bass as bass
import concourse.mybir as mybir
from concourse.bass_utils import run_bass_kernel_spmd

F32 = mybir.dt.float32
BF16 = mybir.dt.bfloat16
I32 = mybir.dt.int32
U32 = mybir.dt.uint32
AF = mybir.ActivationFunctionType
ALU = mybir.AluOpType
AX = mybir.AxisListType

D = 2048
T = 1024
TK = 1280
PAST = 256
NDS = 40
EPS = 1e-6
BIG = 30000.0

C_QA, C_KVA, C_PZ, C_DQ, C_DK, C_DV, C_GL = 0, 512, 832, 1344, 1856, 2368, 2880


class Buf:
    __slots__ = ("name", "w", "r")

    def __init__(self, name):
        self.name = name
        self.w = None
        self.r = {}


class Ctx:
    def __init__(self, nc, es):
        self.nc = nc
        self.es = es
        self.eng = {}
        for name, h in (("pe", nc.tensor), ("dve", nc.vector), ("act", nc.scalar),
                        ("pool", nc.gpsimd), ("sp", nc.sync)):
            self.eng[name] = {"h": h, "sem": es.enter_context(nc.semaphore("e_" + name)), "cnt": 0,
                              "known": {}, "name": name}
        self.dsems = [es.enter_context(nc.semaphore("d%d" % i)) for i in range(NDS)]
        self.dcnt = [0] * NDS
        self.dnext = 0
        self.ntile = 0
        self.out_events = []

    def sb(self, name, shape, dtype):
        self.ntile += 1
        return self.es.enter_context(self.nc.sbuf_tensor("%s_%d" % (name, self.ntile), list(shape), dtype))

    def ps(self, name, shape, dtype):
        self.ntile += 1
        return self.es.enter_context(self.nc.psum_tensor("%s_%d" % (name, self.ntile), list(shape), dtype))

    def _wait(self, e, ev):
        sem, val = ev
        k = id(sem)
        if e["known"].get(k, 0) >= val:
            return
        e["h"].wait_ge(sem, val)
        e["known"][k] = val

    def _collect(self, reads, writes):
        evs = []
        for b in reads:
            if b.w is not None:
                evs.append(b.w)
        for b in writes:
            if b.w is not None:
                evs.append(b.w)
            evs.extend(b.r.values())
        return evs

    def op(self, ename, fn, reads=(), writes=()):
        e = self.eng[ename]
        for ev in self._collect(reads, writes):
            self._wait(e, ev)
        inst = fn(e["h"])
        e["cnt"] += 1
        inst.then_inc(e["sem"], 1)
        ev = (e["sem"], e["cnt"])
        e["known"][id(e["sem"])] = e["cnt"] if ename == "pe" else e["cnt"] - 1
        for b in reads:
            b.r[ename] = ev
        for b in writes:
            b.w = ev
            b.r = {}
        return ev

    def dma(self, qname, out, in_, reads=(), writes=(), indirect=None, is_output=False):
        e = self.eng[qname]
        for ev in self._collect(reads, writes):
            self._wait(e, ev)
        slot = self.dnext
        self.dnext = (slot + 1) % NDS
        if self.dcnt[slot] > 0:
            self._wait(e, (self.dsems[slot], 16 * self.dcnt[slot]))
        self.dcnt[slot] += 1
        if indirect is not None:
            inst = e["h"].indirect_dma_start(out=out, out_offset=None, in_=in_,
                                             in_offset=bass.IndirectOffsetOnAxis(ap=indirect, axis=0))
        else:
            inst = e["h"].dma_start(out=out, in_=in_)
        inst.then_inc(self.dsems[slot], 16)
        ev = (self.dsems[slot], 16 * self.dcnt[slot])
        for b in reads:
            b.r[("d", slot)] = ev
        for b in writes:
            b.w = ev
            b.r = {}
        if is_output:
            self.out_events.append(ev)
        return ev

    def fence(self):
        evs = [(e["sem"], e["cnt"]) for e in self.eng.values() if e["cnt"] > 0]
        evs += [(self.dsems[s], 16 * self.dcnt[s]) for s in range(NDS) if self.dcnt[s] > 0]
        for e in self.eng.values():
            for ev in evs:
                if ev[0] is e["sem"]:
                    continue
                self._wait(e, ev)

    def finish(self):
        e = self.eng["sp"]
        for ev in self.out_events:
            self._wait(e, ev)
        evs = [(x["sem"], x["cnt"]) for x in self.eng.values() if x["cnt"] > 0 and x is not e]
        evs += [(self.dsems[s], 16 * self.dcnt[s]) for s in range(NDS) if self.dcnt[s] > 0]
        for ev in evs:
            self._wait(e, ev)


class PsumRing:
    def __init__(self, ctx, n=8):
        self.tiles = [ctx.ps("bank", [128, 512], F32) for _ in range(n)]
        self.bufs = [Buf("bank%d" % i) for i in range(n)]
        self.i = 0
        self.n = n
        self.held = set()

    def next(self):
        while self.i in self.held:
            self.i = (self.i + 1) % self.n
        i = self.i
        self.i = (i + 1) % self.n
        return self.tiles[i], self.bufs[i]

    def next_n(self, k):
        return [self.next() for _ in range(k)]

    def hold(self, k):
        out = []
        for _ in range(k):
            t, b = self.next()
            self.held.add(self.tiles.index(t))
            out.append((t, b))
        return out

    def release(self, banks):
        for t, b in banks:
            self.held.discard(self.tiles.index(t))


def build_program(L=4, stop_after=None):
    nc = bass.Bass("TRN2", target_bir_lowering=False)
    es = contextlib.ExitStack()
    cx = Ctx(nc, es)
    dbg = stop_after is not None

    in_names = []

    def din(name, shape, dtype=F32):
        in_names.append(name)
        return nc.dram_tensor(name, list(shape), dtype, kind="ExternalInput").ap()

    def dout(name, shape, dtype=F32):
        return nc.dram_tensor(name, list(shape), dtype, kind="ExternalOutput").ap()

    x_in = din("x", [T, D])
    gvec = din("gvec", [32, 128])
    ctx_ckv = din("ctx_ckv", [L, PAST, 256])
    ctx_krope = din("ctx_krope", [L, PAST, 64])
    ctx_dk = din("ctx_dk", [L, PAST, 512])
    ctx_dv = din("ctx_dv", [L, PAST, 512])
    ident_d = din("ident", [128, 128])
    perm_d = din("perm64", [64, 64])
    ropec_d = din("ropec", [64, T])
    ropes_d = din("ropes", [64, T])
    qaug_d = din("qaug", [4, T])
    kaug_d = din("kaug", [4, TK])
    flag_d = din("flag", [128, 1])
    invcnt_d = din("invcnt", [4, T])
    sel4_d = din("sel4", [4, 512])
    iota16_d = din("iota16", [128, 16])
    w_mod = din("w_mod", [L, D, 6 * D])
    vecs = din("vecs", [L, 144, 128])
    w_in = din("w_in", [L, D, 9024])
    w_qb = din("w_qb", [L, 512, 1536])
    w_kvb = din("w_kvb", [L, 256, 2048])
    w_pool = din("w_pool", [L, 4, 128, 128])
    dlam = din("diff_lambda", [L, 256])
    w_br_mla = din("w_br_mla", [L, 1024, D])
    w_br_pool = din("w_br_pool", [L, 512, D])
    w_br_diff = din("w_br_diff", [L, 512, D])
    w_out = din("w_out", [L, D, D])
    w_peer_q = din("w_peer_q", [L, D, D])
    subkeys = din("peer_subkeys", [L, 2, 128, 128])
    need_peer = stop_after is None or stop_after.startswith("peer") or stop_after == "layer"
    if need_peer:
        peer_u = din("peer_u", [L, 16384, D])
        peer_v = din("peer_v", [L, 16384, D])

    y_out = dout("y", [T, D])
    o_ckv = dout("o_ckv", [L, T, 256])
    o_krope = dout("o_krope", [L, T, 64])
    o_dk = dout("o_dk", [L, T, 512])
    o_dv = dout("o_dv", [L, T, 512])
    if dbg:
        dbg_out = dout("dbg", [128, 16 * T])

    xs = nc.dram_tensor("xs", [16, 128, T], F32, kind="Internal").ap()
    if need_peer:
        pu16 = nc.dram_tensor("pu16", [L * 16384, D], BF16, kind="Internal").ap()
        pv16 = nc.dram_tensor("pv16", [L * 16384, D], BF16, kind="Internal").ap()
        pu_flat = peer_u.rearrange("l e d -> (l e) d")
        pv_flat = peer_v.rearrange("l e d -> (l e) d")
    b_tab = [[Buf("tab%d_%d" % (l, i)) for i in range(16)] for l in range(L)]
    conv_pos = [0] * L

    def conv_step(l, n):
        if not need_peer:
            return
        for _ in range(n):
            i = conv_pos[l]
            if i >= 16:
                return
            conv_pos[l] += 1
            src, dst = (pu_flat, pu16) if i < 8 else (pv_flat, pv16)
            r0 = l * 16384 + (i % 8) * 2048
            cx.dma("pool", dst[r0:r0 + 2048, :].rearrange("(p r) d -> p r d", p=128),
                   src[r0:r0 + 2048, :].rearrange("(p r) d -> p r d", p=128), writes=[b_tab[l][i]])

    ident_f = cx.sb("ident_f", [128, 128], F32); b_ident = Buf("ident")
    ident_b = cx.sb("ident_b", [128, 128], BF16)
    ones_f = cx.sb("ones_f", [128, 128], F32)
    ones_b = cx.sb("ones_b", [128, 128], BF16)
    perm_f = cx.sb("perm", [64, 64], F32)
    ropec = cx.sb("ropec", [64, T], F32)
    ropes = cx.sb("ropes", [64, T], F32)
    flag = cx.sb("flag", [128, 1], F32)
    invcnt = cx.sb("invcnt", [4, T], F32)
    sel4 = cx.sb("sel4", [4, 512], F32)
    iota16 = cx.sb("iota16", [128, 16], F32)
    b_const = Buf("consts")
    hT = cx.sb("hT", [128, 16, T], BF16)
    b_hT = [Buf("hT%d" % j) for j in range(16)]
    NW = 2
    wbuf = [cx.sb("wbuf", [128, 8192], BF16) for _ in range(NW)]
    b_wbuf = [Buf("wbuf%d" % i) for i in range(NW)]
    wsm = [cx.sb("wsm", [128, 4096], BF16) for _ in range(2)]
    b_wsm = [Buf("wsm%d" % i) for i in range(2)]
    scr = [cx.sb("scr", [128, T], F32) for _ in range(3)]
    b_scr = [Buf("scr%d" % i) for i in range(3)]
    scr_i = [0]
    rs = cx.sb("rs", [128, T], F32); b_rs = Buf("rs")
    modT = cx.sb("modT", [128, 96], F32); b_modT = Buf("modT")
    vecT = cx.sb("vecT", [128, 144], F32); b_vecT = Buf("vecT")
    gvT = cx.sb("gvT", [128, 32], F32); b_gvT = Buf("gvT")
    scondT = cx.sb("scondT", [128, 16], BF16); b_scond = Buf("scond")
    drv = cx.sb("drv", [128, 64], F32); b_drv = Buf("drv")
    lamt = cx.sb("lamt", [128, 264], F32); b_lam = Buf("lam")
    stg = [cx.sb("stg", [128, 128], F32) for _ in range(4)]
    b_stg = [Buf("stg%d" % i) for i in range(4)]
    stg_i = [0]
    ARENA = 86 * 1024
    arena = cx.sb("arena", [128, ARENA // 2], BF16)

    pr = PsumRing(cx, 8)

    def next_scr():
        i = scr_i[0]
        scr_i[0] = (i + 1) % 3
        return scr[i], b_scr[i]

    def next_stg():
        i = stg_i[0]
        stg_i[0] = (i + 1) % 4
        return stg[i], b_stg[i]

    class Arena:
        def __init__(self):
            self.off = 0

        def reset(self):
            self.off = 0

        def take(self, shape, dtype):
            n = 1
            for s in shape[1:]:
                n *= s
            nbytes = n * (4 if dtype in (F32, I32, U32) else 2)
            nbytes = (nbytes + 63) // 64 * 64
            assert self.off + nbytes <= ARENA, ("arena overflow", self.off, nbytes)
            a = arena[:, self.off // 2:(self.off + nbytes) // 2]
            self.off += nbytes
            if dtype != BF16:
                a = a.bitcast(dtype)
            a = a[:, 0:n]
            if len(shape) == 3:
                a = a.rearrange("p (a b) -> p a b", a=shape[1])
            elif len(shape) == 4:
                a = a.rearrange("p (a b c) -> p a b c", a=shape[1], b=shape[2])
            return a

    ar = Arena()

    for t_sb, t_d in ((ident_f, ident_d), (perm_f, perm_d), (ropec, ropec_d), (ropes, ropes_d), (flag, flag_d),
                      (invcnt, invcnt_d), (sel4, sel4_d), (iota16, iota16_d)):
        cx.dma("sp", t_sb[:], t_d, writes=[b_const])
    cx.dma("pool", ident_b[:], ident_d, writes=[b_const])
    cx.op("dve", lambda e: e.memset(ones_f[:], 1.0), writes=[b_const])
    cx.op("dve", lambda e: e.memset(ones_b[:], 1.0), writes=[b_const])

    def transpose_f32(dst_fn, src_ap, p_in, f_in, reads, n_consume=1):
        pt, pb = pr.next()
        cx.op("pe", lambda e: e.transpose(pt[0:f_in, 0:p_in], src_ap, ident_f[0:p_in, 0:p_in]),
              reads=list(reads) + [b_const], writes=[pb])
        dst_fn(pt[0:f_in, 0:p_in], pb)

    for tc in range(8):
        xt, xb = next_scr()
        for hh in range(2):
            cx.dma("sp", xt[:, 0:T], x_in[tc * 128:(tc + 1) * 128, hh * T:(hh + 1) * T], writes=[xb])
            for jj in range(8):
                j = hh * 8 + jj
                st, sbf = next_stg()

                def ev(ps_ap, pb, st=st, sbf=sbf):
                    cx.op("act", lambda e: e.activation(out=st[:, :], in_=ps_ap, func=AF.Copy),
                          reads=[pb], writes=[sbf])
                transpose_f32(ev, xt[:, jj * 128:(jj + 1) * 128], 128, 128, [xb])
                cx.dma("sp", xs[j, :, tc * 128:(tc + 1) * 128], st[:, :], reads=[sbf], writes=[])
    b_xs = [Buf("xs%d" % j) for j in range(16)]
    cx.fence()

    st, sbf = next_stg()
    cx.dma("sp", st[0:32, :], gvec, writes=[sbf])
    transpose_f32(lambda ps_ap, pb: cx.op("act", lambda e: e.activation(out=gvT[:, :], in_=ps_ap, func=AF.Copy),
                                          reads=[pb], writes=[b_gvT]), st[0:32, :], 32, 128, [sbf])
    cx.op("act", lambda e: e.activation(out=scondT[:, :], in_=gvT[:, 0:16], func=AF.Silu),
          reads=[b_gvT], writes=[b_scond])

    w_i = [0]

    def load_w(dram_ap, kc, ncols, small=False):
        if small:
            i = w_i[0] % 2
            tile, buf = wsm[i], b_wsm[i]
            assert kc * ncols <= 4096
        else:
            i = w_i[0] % NW
            tile, buf = wbuf[i], b_wbuf[i]
            assert kc * ncols <= 8192
        w_i[0] += 1
        view = tile[:, 0:kc * ncols].rearrange("p (k n) -> p k n", k=kc)
        rows = dram_ap.shape[0]
        if rows >= 128:
            cx.dma("pool", view, dram_ap.rearrange("(k p) n -> p k n", p=128), writes=[buf])
        else:
            cx.dma("pool", view[0:rows, 0, :], dram_ap, writes=[buf])
        return view, buf

    def proj_fm(w_dram, K, cols, rhs_fn, rhs_bufs, consume, small=False, group=512, halves=(0, 1)):
        kc_n = max(1, K // 128)
        gi = 0
        while gi < len(cols):
            c_start = cols[gi][0]
            ge = gi
            while ge < len(cols) and cols[ge][0] + cols[ge][1] - c_start <= group:
                ge += 1
            width = cols[ge - 1][0] + cols[ge - 1][1] - c_start
            wv, wb = load_w(w_dram[:, c_start:c_start + width], kc_n, width, small=small)
            for ci in range(gi, ge):
                c0, M = cols[ci]
                for half in halves:
                    pt, pb = pr.next()
                    for kc in range(kc_n):
                        kp = min(128, K - kc * 128)
                        cx.op("pe", lambda e, kc=kc, kp=kp: e.matmul(
                            pt[0:M, :], wv[0:kp, kc, c0 - c_start:c0 - c_start + M], rhs_fn(kc, half),
                            start=(kc == 0), stop=(kc == kc_n - 1)),
                            reads=[wb] + list(rhs_bufs(kc)), writes=[pb])
                    consume(ci, half, pt[0:M, :], pb)
            gi = ge

    def rstd_from_psum(ps_list, nfeat, out_ap_fn, out_buf):
        for half, (pt, pb) in enumerate(ps_list):
            cx.op("act", lambda e: e.activation(out=out_ap_fn(half), in_=pt[:, :], func=AF.Sqrt,
                                                scale=1.0 / nfeat, bias=eps_t[:, 0:1]),
                  reads=[pb, b_const], writes=[out_buf])
        for half in range(len(ps_list)):
            cx.op("dve", lambda e: e.reciprocal(out=out_ap_fn(half), in_=out_ap_fn(half)),
                  reads=[out_buf], writes=[out_buf])

    eps_t = cx.sb("eps", [128, 1], F32)
    cx.op("dve", lambda e: e.memset(eps_t[:], EPS), writes=[b_const])

    def norm_x(l, which, final=False):
        acc = pr.next_n(2)
        for j in range(16):
            xt, xb = next_scr()
            cx.dma("sp", xt[:, :], xs[j], reads=[b_xs[j]], writes=[xb])
            sq, sqb = next_scr()
            cx.op("act", lambda e: e.activation(out=sq[:, :], in_=xt[:, :], func=AF.Square), reads=[xb], writes=[sqb])
            for half in range(2):
                pt, pb = acc[half]
                cx.op("pe", lambda e: e.matmul(pt[:, :], ones_f[:, :], sq[:, half * 512:(half + 1) * 512],
                                               start=(j == 0), stop=(j == 15)), reads=[sqb, b_const], writes=[pb])
        rstd_from_psum(acc, D, lambda half: rs[:, half * 512:(half + 1) * 512], b_rs)

    def apply_norm(l, which):
        gs = drv[:, (0 if which == 1 else 16):]
        sh = modT[:, (0 if which == 1 else 48):]
        for j in range(16):
            xt, xb = next_scr()
            cx.dma("sp", xt[:, :], xs[j], reads=[b_xs[j]], writes=[xb])
            cx.op("dve", lambda e: e.tensor_tensor(out=xt[:, :], in0=xt[:, :], in1=rs[:, :], op=ALU.mult),
                  reads=[xb, b_rs], writes=[xb])
            cx.op("dve", lambda e: e.tensor_scalar(out=hT[:, j, :], in0=xt[:, :], scalar1=gs[:, j:j + 1],
                                                   scalar2=sh[:, j:j + 1], op0=ALU.mult, op1=ALU.add),
                  reads=[xb, b_drv, b_modT], writes=[b_hT[j]])

    def dump(ap_2d, bufs, ncols):
        cx.dma("sp", dbg_out[0:ap_2d.shape[0], 0:ncols], ap_2d, reads=bufs, writes=[], is_output=True)

    for l in range(L):
        st, sbf = next_stg()
        cx.dma("sp", st[:, :], vecs[l, 0:128, :], writes=[sbf])
        transpose_f32(lambda ps_ap, pb: cx.op("act", lambda e: e.activation(out=vecT[:, 0:128], in_=ps_ap, func=AF.Copy),
                                              reads=[pb], writes=[b_vecT]), st[:, :], 128, 128, [sbf])
        st, sbf = next_stg()
        cx.dma("sp", st[0:16, :], vecs[l, 128:144, :], writes=[sbf])
        transpose_f32(lambda ps_ap, pb: cx.op("act", lambda e: e.activation(out=vecT[:, 128:144], in_=ps_ap, func=AF.Copy),
                                              reads=[pb], writes=[b_vecT]), st[0:16, :], 16, 128, [sbf])
        mt, mb = pr.next()
        for g in range(24):
            wv, wb = load_w(w_mod[l, :, g * 512:(g + 1) * 512], 16, 512)
            for cc in range(4):
                m = g * 4 + cc
                for kc in range(16):
                    cx.op("pe", lambda e: e.matmul(mt[:, m:m + 1], wv[:, kc, cc * 128:(cc + 1) * 128],
                                                   scondT[:, kc:kc + 1], start=(kc == 0), stop=(kc == 15)),
                          reads=[wb, b_scond], writes=[mb])
        cx.op("dve", lambda e: e.tensor_tensor(out=modT[:, :], in0=mt[:, 0:96], in1=vecT[:, 0:96], op=ALU.add),
              reads=[mb, b_vecT], writes=[b_modT])
        cx.op("dve", lambda e: e.scalar_tensor_tensor(out=drv[:, 0:16], in0=modT[:, 16:32], scalar=1.0,
                                                      in1=vecT[:, 96:112], op0=ALU.add, op1=ALU.mult),
              reads=[b_modT, b_vecT], writes=[b_drv])
        cx.op("dve", lambda e: e.scalar_tensor_tensor(out=drv[:, 16:32], in0=modT[:, 64:80], scalar=1.0,
                                                      in1=vecT[:, 112:128], op0=ALU.add, op1=ALU.mult),
              reads=[b_modT, b_vecT], writes=[b_drv])
        if stop_after == "mod":
            dump(modT[:, :], [b_modT], 96)
            break
        norm_x(l, 1)
        apply_norm(l, 1)
        conv_step(l, 4)
        if stop_after == "norm1":
            for j in range(16):
                xt, xb = next_scr()
                cx.op("act", lambda e: e.activation(out=xt[:, :], in_=hT[:, j, :], func=AF.Copy),
                      reads=[b_hT[j]], writes=[xb])
                cx.dma("sp", dbg_out[:, j * T:(j + 1) * T], xt[:, :], reads=[xb], is_output=True)
            break
        hrhs = lambda kc, half: hT[:, kc, half * 512:(half + 1) * 512]
        hbufs = lambda kc: [b_hT[kc]]
        lam_init = 0.8 - 0.6 * math.exp(-0.3 * l)
        ar.reset()
        mla_oT = ar.take([128, 8, T], BF16); b_mla_o = Buf("mla_o")
        pool_oT = ar.take([128, 4, T], BF16); b_pool_o = Buf("pool_o")
        diff_oT = ar.take([128, 4, T], BF16); b_diff_o = Buf("diff_o")
        mark = ar.off

        def mm_fm(wv, wb, coff, M, K, rhs_fn, rhs_bufs, half, N=512):
            kc_n = max(1, K // 128)
            pt, pb = pr.next()
            for kc in range(kc_n):
                kp = min(128, K - kc * 128)
                cx.op("pe", lambda e: e.matmul(pt[0:M, 0:N], wv[0:kp, kc, coff:coff + M], rhs_fn(kc, half),
                                               start=(kc == 0), stop=(kc == kc_n - 1)),
                      reads=[wb] + list(rhs_bufs(kc)), writes=[pb])
            return pt, pb

        def rope_unit(raw, rawb, dst_fn, dstb):
            for half in range(2):
                sl = slice(half * 512, (half + 1) * 512)
                pt, pb = pr.next()
                cx.op("pe", lambda e: e.matmul(pt[0:64, :], perm_f[:, :], raw[0:64, sl], start=True, stop=True),
                      reads=[rawb, b_const], writes=[pb])
                t1, t1b = next_scr()
                cx.op("dve", lambda e: e.tensor_tensor(out=t1[0:64, 0:512], in0=pt[0:64, :], in1=ropes[:, sl], op=ALU.mult),
                      reads=[pb, b_const], writes=[t1b])
                cx.op("dve", lambda e: e.tensor_tensor(out=t1[0:64, 512:1024], in0=raw[0:64, sl], in1=ropec[:, sl], op=ALU.mult),
                      reads=[rawb, b_const], writes=[t1b])
                cx.op("dve", lambda e: e.tensor_tensor(out=dst_fn(sl), in0=t1[0:64, 0:512], in1=t1[0:64, 512:1024], op=ALU.add),
                      reads=[t1b], writes=[dstb])

        def out_transposes(raw, rawb, nfeat, out_dram_fn, extra=None):
            for tc in range(8):
                st, sbf = next_stg()

                def ev(ps_ap, pb, st=st, sbf=sbf, tc=tc):
                    cx.op("act", lambda e: e.activation(out=st[:, 0:nfeat], in_=ps_ap, func=AF.Copy),
                          reads=[pb], writes=[sbf])
                    if extra is not None:
                        extra(tc, st[:, 0:nfeat], sbf)
                transpose_f32(ev, raw[0:nfeat, tc * 128:(tc + 1) * 128], nfeat, 128, [rawb])
                import os
                if os.environ.get("SKIP_ODMA"):
                    continue
                cx.dma("sp", out_dram_fn(tc), st[:, 0:nfeat], reads=[sbf], is_output=True)

        def attention_core(kparts, qparts, v_fn, v_bufs, et, etb, qh, scale):
            for kc in range(10):
                pt, pb = pr.next()
                n = len(kparts)
                for i in range(n):
                    kf, kb, K = kparts[i]
                    qa_, qb_ = qparts[i]
                    cx.op("pe", lambda e: e.matmul(pt[:, :], kf(kc), qa_, start=(i == 0), stop=(i == n - 1)),
                          reads=list(kb) + list(qb_), writes=[pb])
                cx.op("act", lambda e: e.activation(out=et[:, kc, :], in_=pt[:, :], func=AF.Exp, scale=scale),
                      reads=[pb], writes=[etb])
            (o_ps, o_pb), (d_ps, d_pb) = pr.next_n(2)
            for kc in range(10):
                cx.op("pe", lambda e: e.matmul(o_ps[:, :], v_fn(kc), et[:, kc, :], start=(kc == 0), stop=(kc == 9)),
                      reads=[etb] + list(v_bufs), writes=[o_pb])
            for kc in range(10):
                cx.op("pe", lambda e: e.matmul(d_ps[:, :], ones_b[:, :], et[:, kc, :], start=(kc == 0), stop=(kc == 9)),
                      reads=[etb, b_const], writes=[d_pb])
            return o_ps, o_pb, d_ps, d_pb

        vdiff = ar.take([128, 10, 512], BF16); b_vdiff = Buf("vdiff")
        ET = [ar.take([128, 10, 512], BF16) for _ in range(2)]; b_ET = [Buf("ET0"), Buf("ET1")]
        qu = [ar.take([128, T], BF16) for _ in range(2)]; b_qu = [Buf("qu0"), Buf("qu1")]
        ku = [ar.take([128, TK], BF16) for _ in range(2)]; b_ku = [Buf("ku0"), Buf("ku1")]
        asb = [ar.take([128, 512], F32) for _ in range(3)]; b_asb = [Buf("as0"), Buf("as1"), Buf("as2")]
        ctxk = ar.take([128, 2, 512], F32); b_ctxk = Buf("ctxk")
        for s in range(2):
            cx.dma("pool", qu[s][64:68, :], qaug_d, writes=[b_qu[s]])
            cx.dma("pool", ku[s][64:68, :], kaug_d, writes=[b_ku[s]])
        cx.dma("sp", ctxk, ctx_dk[l].rearrange("(c p) f -> p c f", p=128), writes=[b_ctxk])
        cx.dma("pool", vdiff[:, 0:2, :], ctx_dv[l].rearrange("(c p) f -> p c f", p=128), writes=[b_vdiff])
        cx.dma("sp", lamt[:, 0:256], dlam[l].partition_broadcast(128), writes=[b_lam])
        for i in range(2):
            cx.op("dve", lambda e: e.scalar_tensor_tensor(out=lamt[:, 128 * i:128 * i + 64], in0=lamt[:, 128 * i:128 * i + 64],
                                                          scalar=1.0, in1=lamt[:, 128 * i + 64:128 * i + 128],
                                                          op0=ALU.mult, op1=ALU.mult, accum_out=lamt[:, 256 + i:257 + i]),
                  reads=[b_lam], writes=[b_lam])
        cx.op("act", lambda e: e.activation(out=lamt[:, 258:260], in_=lamt[:, 256:258], func=AF.Exp), reads=[b_lam], writes=[b_lam])
        cx.op("dve", lambda e: e.tensor_tensor(out=lamt[:, 260:261], in0=lamt[:, 259:260], in1=lamt[:, 258:259], op=ALU.subtract),
              reads=[b_lam], writes=[b_lam])
        cx.op("dve", lambda e: e.tensor_scalar(out=lamt[:, 260:261], in0=lamt[:, 260:261], scalar1=-lam_init, scalar2=None, op0=ALU.add),
              reads=[b_lam], writes=[b_lam])
        cx.op("dve", lambda e: e.tensor_scalar(out=drv[:, 32:33], in0=vecT[:, 138:139], scalar1=(1.0 - lam_init), scalar2=None, op0=ALU.mult),
              reads=[b_vecT], writes=[b_drv])
        if stop_after == "diffA":
            dump(lamt[:, 0:264], [b_lam], 264)
            break
        wv, wb = load_w(w_in[l, :, C_DV:C_DV + 512], 16, 512)
        for c in range(4):
            raw, rawb = next_scr()
            for half in range(2):
                pt, pb = mm_fm(wv, wb, c * 128, 128, D, hrhs, hbufs, half)
                cx.op("act", lambda e: e.activation(out=raw[:, half * 512:(half + 1) * 512], in_=pt[:, :], func=AF.Copy),
                      reads=[pb], writes=[rawb])

            def extra(tc, ps_ap, pb, c=c):
                import os
                if os.environ.get("SKIP_EXTRA"):
                    return
                cx.op("dve", lambda e: e.tensor_copy(out=vdiff[:, 2 + tc, c * 128:(c + 1) * 128], in_=ps_ap),
                      reads=[pb], writes=[b_vdiff])
            out_transposes(raw, rawb, 128, lambda tc, c=c: o_dv[l, tc * 128:(tc + 1) * 128, c * 128:(c + 1) * 128], extra)
        if stop_after == "diffB":
            dump(lamt[:, 0:264], [b_lam], 264)
            break
        wq_v, wq_b = load_w(w_in[l, :, C_DQ:C_DQ + 512], 16, 512)
        wk_v, wk_b = load_w(w_in[l, :, C_DK:C_DK + 512], 16, 512)
        dscale = 64.0 ** -0.5
        etc = 0
        for h in range(4):
            for s in range(2):
                u = 2 * h + s
                raw, rawb = next_scr()
                for half in range(2):
                    pt, pb = mm_fm(wk_v, wk_b, u * 64, 64, D, hrhs, hbufs, half)
                    cx.op("act", lambda e: e.activation(out=raw[0:64, half * 512:(half + 1) * 512], in_=pt[0:64, :], func=AF.Copy),
                          reads=[pb], writes=[rawb])
                out_transposes(raw, rawb, 64, lambda tc, u=u: o_dk[l, tc * 128:(tc + 1) * 128, u * 64:(u + 1) * 64])
                rope_unit(raw, rawb, lambda sl, s=s: ku[s][0:64, PAST + sl.start:PAST + sl.stop], b_ku[s])
                for tc in range(2):
                    transpose_f32(lambda ps_ap, pb, s=s, tc=tc: cx.op(
                        "dve", lambda e: e.tensor_copy(out=ku[s][0:64, tc * 128:(tc + 1) * 128], in_=ps_ap),
                        reads=[pb], writes=[b_ku[s]]), ctxk[:, tc, u * 64:(u + 1) * 64], 128, 64, [b_ctxk])
                raw, rawb = next_scr()
                for half in range(2):
                    pt, pb = mm_fm(wq_v, wq_b, u * 64, 64, D, hrhs, hbufs, half)
                    cx.op("act", lambda e: e.activation(out=raw[0:64, half * 512:(half + 1) * 512], in_=pt[0:64, :], func=AF.Copy),
                          reads=[pb], writes=[rawb])
                rope_unit(raw, rawb, lambda sl, s=s: qu[s][0:64, sl], b_qu[s])
            conv_step(l, 1)
            for qh in range(2):
                qs = slice(qh * 512, (qh + 1) * 512)
                for s in range(2):
                    et, etb = ET[etc % 2], b_ET[etc % 2]
                    etc += 1
                    o_ps, o_pb, d_ps, d_pb = attention_core(
                        [(lambda kc, s=s: ku[s][0:68, kc * 128:(kc + 1) * 128], [b_ku[s]], 68)],
                        [(qu[s][0:68, qs], [b_qu[s]])],
                        lambda kc: vdiff[:, kc, h * 128:(h + 1) * 128], [b_vdiff], et, etb, qh, dscale)
                    cx.op("dve", lambda e: e.reciprocal(out=asb[2][:, :], in_=d_ps[:, :]), reads=[d_pb], writes=[b_asb[2]])
                    cx.op("dve", lambda e: e.tensor_tensor(out=asb[s][:, :], in0=o_ps[:, :], in1=asb[2][:, :], op=ALU.mult),
                          reads=[o_pb, b_asb[2]], writes=[b_asb[s]])
                cx.op("dve", lambda e: e.scalar_tensor_tensor(out=asb[0][:, :], in0=asb[1][:, :], scalar=lamt[:, 260:261],
                                                              in1=asb[0][:, :], op0=ALU.mult, op1=ALU.add),
                      reads=[b_asb[0], b_asb[1], b_lam], writes=[b_asb[0]])
                cx.op("act", lambda e: e.activation(out=asb[1][:, :], in_=asb[0][:, :], func=AF.Square),
                      reads=[b_asb[0]], writes=[b_asb[1]])
                pt, pb = pr.next()
                cx.op("pe", lambda e: e.matmul(pt[:, :], ones_f[:, :], asb[1][:, :], start=True, stop=True),
                      reads=[b_asb[1], b_const], writes=[pb])
                rstd_from_psum([(pt, pb)], 128, lambda half: asb[2][:, :], b_asb[2])
                cx.op("dve", lambda e: e.tensor_tensor(out=asb[0][:, :], in0=asb[0][:, :], in1=asb[2][:, :], op=ALU.mult),
                      reads=[b_asb[0], b_asb[2]], writes=[b_asb[0]])
                cx.op("dve", lambda e: e.tensor_scalar(out=diff_oT[:, h, qs], in0=asb[0][:, :], scalar1=drv[:, 32:33], scalar2=None, op0=ALU.mult),
                      reads=[b_asb[0], b_drv], writes=[b_diff_o])
        if stop_after == "diff":
            for j in range(4):
                xt, xb = next_scr()
                cx.op("act", lambda e: e.activation(out=xt[:, :], in_=diff_oT[:, j, :], func=AF.Copy), reads=[b_diff_o], writes=[xb])
                cx.dma("sp", dbg_out[:, j * T:(j + 1) * T], xt[:, :], reads=[xb], is_output=True)
            break
        cx.fence()
        ar.off = mark
        zp = ar.take([128, 4, 272], F32); b_zp = Buf("zp")
        pa = ar.take([128, 4, 272], F32); b_pa = Buf("pa")
        pb2 = ar.take([128, 4, 272], F32); b_pb2 = Buf("pb2")
        dTt = ar.take([128, T], BF16); b_dT = Buf("dT")
        wpl = ar.take([128, 4, 128], BF16); b_wpl = Buf("wpl")
        cx.dma("pool", wpl, w_pool[l].rearrange("g c d -> c g d"), writes=[b_wpl])
        cx.op("dve", lambda e: e.memset(zp[:, :, :], 0.0), writes=[b_zp])
        wv, wb = load_w(w_in[l, :, C_PZ:C_PZ + 512], 16, 512)
        for g in range(4):
            for half in range(2):
                pt, pb = mm_fm(wv, wb, g * 128, 128, D, hrhs, hbufs, half)
                cx.op("act", lambda e: e.activation(out=zp[:, 2 * half:2 * half + 2, 8:264],
                                                    in_=pt[:, :].rearrange("p (a b) -> p a b", a=2), func=AF.Copy),
                      reads=[pb], writes=[b_zp])
            cx.op("dve", lambda e: e.tensor_scalar(out=zp[:, 1:4, 0:8], in0=zp[:, 0:3, 256:264], scalar1=flag[:, 0:1], scalar2=None, op0=ALU.mult),
                  reads=[b_zp, b_const], writes=[b_zp])
            cx.op("dve", lambda e: e.tensor_scalar(out=zp[:, 0:3, 264:272], in0=zp[:, 1:4, 8:16], scalar1=flag[:, 0:1], scalar2=None, op0=ALU.mult),
                  reads=[b_zp, b_const], writes=[b_zp])
            cx.op("dve", lambda e: e.tensor_tensor(out=pa[:, :, 1:272], in0=zp[:, :, 1:272], in1=zp[:, :, 0:271], op=ALU.add),
                  reads=[b_zp], writes=[b_pa])
            win, winb = pa, b_pa
            if g >= 1:
                cx.op("dve", lambda e: e.tensor_tensor(out=pb2[:, :, 2:271], in0=pa[:, :, 3:272], in1=pa[:, :, 1:270], op=ALU.add),
                      reads=[b_pa], writes=[b_pb2])
                win, winb = pb2, b_pb2
            if g >= 2:
                cx.op("dve", lambda e: e.tensor_tensor(out=pa[:, :, 4:269], in0=pb2[:, :, 6:271], in1=pb2[:, :, 2:267], op=ALU.add),
                      reads=[b_pb2], writes=[b_pa])
                win, winb = pa, b_pa
            if g >= 3:
                cx.op("dve", lambda e: e.tensor_tensor(out=pb2[:, :, 8:264], in0=pa[:, :, 12:268], in1=pa[:, :, 4:260], op=ALU.add),
                      reads=[b_pa], writes=[b_pb2])
                win, winb = pb2, b_pb2
            for half in range(2):
                pt, pb = pr.next()
                cx.op("pe", lambda e: e.matmul(pt[:, :], sel4[:, g * 128:(g + 1) * 128], invcnt[:, half * 512:(half + 1) * 512],
                                               start=True, stop=True), reads=[b_const], writes=[pb])
                other = pa if win is pb2 else pb2
                otherb = b_pa if win is pb2 else b_pb2
                cx.op("dve", lambda e: e.tensor_tensor(out=other[:, 2 * half:2 * half + 2, 8:264], in0=win[:, 2 * half:2 * half + 2, 8:264],
                                                       in1=pt[:, :].rearrange("p (a b) -> p a b", a=2), op=ALU.mult),
                      reads=[winb, pb], writes=[otherb])
                cx.op("dve", lambda e: e.tensor_tensor(out=dTt[:, half * 512:(half + 1) * 512].rearrange("p (a b) -> p a b", a=2),
                                                       in0=other[:, 2 * half:2 * half + 2, 8:264], in1=zp[:, 2 * half:2 * half + 2, 8:264], op=ALU.subtract),
                      reads=[otherb, b_zp], writes=[b_dT])
            for half in range(2):
                pt, pb = pr.next()
                cx.op("pe", lambda e: e.matmul(pt[:, :], wpl[:, g, :], dTt[:, half * 512:(half + 1) * 512], start=True, stop=True),
                      reads=[b_wpl, b_dT], writes=[pb])
                cx.op("dve", lambda e: e.tensor_scalar(out=pool_oT[:, g, half * 512:(half + 1) * 512], in0=pt[:, :],
                                                       scalar1=vecT[:, 134 + g:135 + g], scalar2=None, op0=ALU.mult),
                      reads=[pb, b_vecT], writes=[b_pool_o])
        if stop_after == "pool":
            for j in range(4):
                xt, xb = next_scr()
                cx.op("act", lambda e: e.activation(out=xt[:, :], in_=pool_oT[:, j, :], func=AF.Copy), reads=[b_pool_o], writes=[xb])
                cx.dma("sp", dbg_out[:, j * T:(j + 1) * T], xt[:, :], reads=[xb], is_output=True)
            break
        cx.fence()
        ar.off = mark

        ckvT = ar.take([128, 2, TK], BF16); b_ckvT = Buf("ckvT")
        krT = ar.take([128, TK], BF16); b_krT = Buf("krT")
        qanT = ar.take([128, 4, T], BF16); b_qan = Buf("qanT")
        ET = [ar.take([128, 10, 512], BF16) for _ in range(2)]; b_ET = [Buf("ET0"), Buf("ET1")]
        qn = ar.take([128, T], BF16); b_qn = Buf("qn")
        qr = ar.take([128, T], BF16); b_qr = Buf("qr")
        kn = ar.take([128, TK], BF16); b_kn = Buf("kn")
        vm = ar.take([128, 10, 128], BF16); b_vm = Buf("vm")
        cst = ar.take([128, 2, 320], F32); b_cst = Buf("cst")
        rsm = ar.take([128, 512], F32); b_rsm = Buf("rsm")
        cx.dma("pool", qr[64:68, :], qaug_d, writes=[b_qr])
        cx.dma("pool", krT[64:68, :], kaug_d, writes=[b_krT])
        cx.dma("sp", cst[:, :, 0:256], ctx_ckv[l].rearrange("(c p) f -> p c f", p=128), writes=[b_cst])
        cx.dma("sp", cst[:, :, 256:320], ctx_krope[l].rearrange("(c p) f -> p c f", p=128), writes=[b_cst])
        for tc in range(2):
            for c in range(2):
                transpose_f32(lambda ps_ap, pb, tc=tc, c=c: cx.op(
                    "act", lambda e: e.activation(out=ckvT[:, c, tc * 128:(tc + 1) * 128], in_=ps_ap, func=AF.Copy),
                    reads=[pb], writes=[b_ckvT]), cst[:, tc, c * 128:(c + 1) * 128], 128, 128, [b_cst])
            transpose_f32(lambda ps_ap, pb, tc=tc: cx.op(
                "act", lambda e: e.activation(out=krT[0:64, tc * 128:(tc + 1) * 128], in_=ps_ap, func=AF.Copy),
                reads=[pb], writes=[b_krT]), cst[:, tc, 256:320], 128, 64, [b_cst])
        wv, wb = load_w(w_in[l, :, 0:512], 16, 512)
        wv2, wb2 = load_w(w_in[l, :, 512:832], 16, 320)
        for (wv_, wb_, nch, gcol, nfeat, is_q) in ((wv, wb, 4, 128, 512, True), (wv2, wb2, 2, 132, 256, False)):
            acc = pr.hold(2)
            for c in range(nch):
                for half in range(2):
                    pt, pb = mm_fm(wv_, wb_, c * 128, 128, D, hrhs, hbufs, half)
                    sq, sqb = next_scr()
                    cx.op("act", lambda e: e.activation(out=sq[:, 0:512], in_=pt[:, :], func=AF.Square), reads=[pb], writes=[sqb])
                    cx.op("pe", lambda e: e.matmul(acc[half][0][:, :], ones_f[:, :], sq[:, 0:512], start=(c == 0), stop=(c == nch - 1)),
                          reads=[sqb, b_const], writes=[acc[half][1]])
            rstd_from_psum(acc, nfeat, lambda half: rs[:, half * 512:(half + 1) * 512], b_rs)
            pr.release(acc)
            for c in range(nch):
                raw, rawb = next_scr()
                for half in range(2):
                    sl = slice(half * 512, (half + 1) * 512)
                    pt, pb = mm_fm(wv_, wb_, c * 128, 128, D, hrhs, hbufs, half)
                    cx.op("dve", lambda e: e.tensor_tensor(out=raw[:, sl], in0=pt[:, :], in1=rs[:, sl], op=ALU.mult),
                          reads=[pb, b_rs], writes=[rawb])
                    if is_q:
                        cx.op("dve", lambda e: e.tensor_scalar(out=qanT[:, c, sl], in0=raw[:, sl], scalar1=vecT[:, gcol + c:gcol + c + 1],
                                                               scalar2=None, op0=ALU.mult), reads=[rawb, b_vecT], writes=[b_qan])
                    else:
                        cx.op("dve", lambda e: e.tensor_scalar(out=raw[:, sl], in0=raw[:, sl], scalar1=vecT[:, gcol + c:gcol + c + 1],
                                                               scalar2=None, op0=ALU.mult), reads=[rawb, b_vecT], writes=[rawb])
                        cx.op("act", lambda e: e.activation(out=ckvT[:, c, PAST + half * 512:PAST + (half + 1) * 512], in_=raw[:, sl], func=AF.Copy),
                              reads=[rawb], writes=[b_ckvT])
                if not is_q:
                    out_transposes(raw, rawb, 128, lambda tc, c=c: o_ckv[l, tc * 128:(tc + 1) * 128, c * 128:(c + 1) * 128])
        raw, rawb = next_scr()
        for half in range(2):
            pt, pb = mm_fm(wv2, wb2, 256, 64, D, hrhs, hbufs, half)
            cx.op("act", lambda e: e.activation(out=raw[0:64, half * 512:(half + 1) * 512], in_=pt[0:64, :], func=AF.Copy),
                  reads=[pb], writes=[rawb])
        out_transposes(raw, rawb, 64, lambda tc: o_krope[l, tc * 128:(tc + 1) * 128, :])
        rope_unit(raw, rawb, lambda sl: krT[0:64, PAST + sl.start:PAST + sl.stop], b_krT)
        wqb_v, wqb_b = load_w(w_qb[l], 4, 1536)
        wkv_v, wkv_b = load_w(w_kvb[l], 2, 2048, small=True)
        qan_rhs = lambda kc, half: qanT[:, kc, half * 512:(half + 1) * 512]
        qan_bufs = lambda kc: [b_qan]
        mscale = 192.0 ** -0.5
        etc = 0
        for h in range(8):
            for half in range(2):
                pt, pb = mm_fm(wqb_v, wqb_b, h * 192, 128, 512, qan_rhs, qan_bufs, half)
                cx.op("act", lambda e: e.activation(out=qn[:, half * 512:(half + 1) * 512], in_=pt[:, :], func=AF.Copy),
                      reads=[pb], writes=[b_qn])
            raw, rawb = next_scr()
            for half in range(2):
                pt, pb = mm_fm(wqb_v, wqb_b, h * 192 + 128, 64, 512, qan_rhs, qan_bufs, half)
                cx.op("act", lambda e: e.activation(out=raw[0:64, half * 512:(half + 1) * 512], in_=pt[0:64, :], func=AF.Copy),
                      reads=[pb], writes=[rawb])
            rope_unit(raw, rawb, lambda sl: qr[0:64, sl], b_qr)
            for (c0, n) in ((0, 512), (512, 512), (1024, 256)):
                pt, pb = pr.next()
                for kc in range(2):
                    cx.op("pe", lambda e: e.matmul(pt[:, 0:n], wkv_v[:, kc, h * 256:h * 256 + 128], ckvT[:, kc, c0:c0 + n],
                                                   start=(kc == 0), stop=(kc == 1)), reads=[wkv_b, b_ckvT], writes=[pb])
                cx.op("act", lambda e: e.activation(out=kn[:, c0:c0 + n], in_=pt[:, 0:n], func=AF.Copy), reads=[pb], writes=[b_kn])
            for g4 in range(3):
                pt, pb = pr.next()
                nb = 4 if g4 < 2 else 2
                for bi in range(nb):
                    tkc = g4 * 4 + bi
                    for kc in range(2):
                        cx.op("pe", lambda e: e.matmul(pt[:, bi * 128:(bi + 1) * 128], ckvT[:, kc, tkc * 128:(tkc + 1) * 128],
                                                       wkv_v[:, kc, h * 256 + 128:h * 256 + 256], start=(kc == 0), stop=(kc == 1)),
                              reads=[wkv_b, b_ckvT], writes=[pb])
                cx.op("act", lambda e: e.activation(out=vm[:, g4 * 4:g4 * 4 + nb, :], in_=pt[:, 0:nb * 128].rearrange("p (a b) -> p a b", a=nb),
                                                    func=AF.Copy), reads=[pb], writes=[b_vm])
            conv_step(l, 1)
            for qh in range(2):
                qs = slice(qh * 512, (qh + 1) * 512)
                et, etb = ET[etc % 2], b_ET[etc % 2]
                etc += 1
                o_ps, o_pb, d_ps, d_pb = attention_core(
                    [(lambda kc: kn[:, kc * 128:(kc + 1) * 128], [b_kn], 128),
                     (lambda kc: krT[0:68, kc * 128:(kc + 1) * 128], [b_krT], 68)],
                    [(qn[:, qs], [b_qn]), (qr[0:68, qs], [b_qr])],
                    lambda kc: vm[:, kc, :], [b_vm], et, etb, qh, mscale)
                cx.op("dve", lambda e: e.reciprocal(out=rsm[:, :], in_=d_ps[:, :]), reads=[d_pb], writes=[b_rsm])
                cx.op("dve", lambda e: e.tensor_tensor(out=mla_oT[:, h, qs], in0=o_ps[:, :], in1=rsm[:, :], op=ALU.mult),
                      reads=[o_pb, b_rsm], writes=[b_mla_o])
        if stop_after == "mla":
            for j in range(8):
                xt, xb = next_scr()
                cx.op("act", lambda e: e.activation(out=xt[:, :], in_=mla_oT[:, j, :], func=AF.Copy), reads=[b_mla_o], writes=[xb])
                cx.dma("sp", dbg_out[:, j * T:(j + 1) * T], xt[:, :], reads=[xb], is_output=True)
            break
        cx.fence()
        ar.off = mark
        mergedT = ar.take([128, 16, T], BF16); b_merged = [Buf("mg%d" % j) for j in range(16)]
        accg = ar.take([128, 8, 512], F32); b_accg = [Buf("accg%d" % i) for i in range(8)]
        sgt = [ar.take([128, 512], F32) for _ in range(2)]; b_sgt = [Buf("sg0"), Buf("sg1")]
        branches = ((w_br_mla[l], 1024, mla_oT, b_mla_o), (w_br_pool[l], 512, pool_oT, b_pool_o),
                    (w_br_diff[l], 512, diff_oT, b_diff_o))
        sgc = 0
        for jg in range(4):
            for b in range(3):
                gv, gb = load_w(w_in[l, :, C_GL + b * 2048 + jg * 512:C_GL + b * 2048 + (jg + 1) * 512], 16, 512)
                wbr, Kb, brT, brb = branches[b]
                bv, bb = load_w(wbr[:, jg * 512:(jg + 1) * 512], Kb // 128, 512, small=True)
                for jj in range(4):
                    j = jg * 4 + jj
                    for half in range(2):
                        sl = slice(half * 512, (half + 1) * 512)
                        ai = jj * 2 + half
                        pg, pgb = mm_fm(gv, gb, jj * 128, 128, D, hrhs, hbufs, half)
                        s_t, s_b = sgt[sgc % 2], b_sgt[sgc % 2]
                        sgc += 1
                        cx.op("act", lambda e: e.activation(out=s_t[:, :], in_=pg[:, :], func=AF.Sigmoid), reads=[pgb], writes=[s_b])
                        pbr, pbrb = mm_fm(bv, bb, jj * 128, 128, Kb, lambda kc, hf: brT[:, kc, hf * 512:(hf + 1) * 512],
                                          lambda kc: [brb], half)
                        if b == 0:
                            cx.op("dve", lambda e: e.tensor_tensor(out=accg[:, ai, :], in0=s_t[:, :], in1=pbr[:, :], op=ALU.mult),
                                  reads=[s_b, pbrb], writes=[b_accg[ai]])
                        else:
                            cx.op("dve", lambda e: e.tensor_tensor(out=s_t[:, :], in0=s_t[:, :], in1=pbr[:, :], op=ALU.mult),
                                  reads=[s_b, pbrb], writes=[s_b])
                            if b == 1:
                                cx.op("dve", lambda e: e.tensor_tensor(out=accg[:, ai, :], in0=accg[:, ai, :], in1=s_t[:, :], op=ALU.add),
                                      reads=[s_b, b_accg[ai]], writes=[b_accg[ai]])
                            else:
                                cx.op("dve", lambda e: e.tensor_tensor(out=mergedT[:, j, sl], in0=accg[:, ai, :], in1=s_t[:, :], op=ALU.add),
                                      reads=[s_b, b_accg[ai]], writes=[b_merged[j]])
        if stop_after == "merge":
            for j in range(16):
                xt, xb = next_scr()
                cx.op("act", lambda e: e.activation(out=xt[:, :], in_=mergedT[:, j, :], func=AF.Copy), reads=[b_merged[j]], writes=[xb])
                cx.dma("sp", dbg_out[:, j * T:(j + 1) * T], xt[:, :], reads=[xb], is_output=True)
            break
        for jg in range(4):
            wv, wb = load_w(w_out[l, :, jg * 512:(jg + 1) * 512], 16, 512)
            for jj in range(4):
                j = jg * 4 + jj
                xt, xb = next_scr()
                cx.dma("sp", xt[:, :], xs[j], reads=[b_xs[j]], writes=[xb])
                for half in range(2):
                    sl = slice(half * 512, (half + 1) * 512)
                    pt, pb = mm_fm(wv, wb, jj * 128, 128, D, lambda kc, hf: mergedT[:, kc, hf * 512:(hf + 1) * 512],
                                   lambda kc: [b_merged[kc]], half)
                    cx.op("dve", lambda e: e.scalar_tensor_tensor(out=xt[:, sl], in0=pt[:, :], scalar=modT[:, 32 + j:33 + j],
                                                                  in1=xt[:, sl], op0=ALU.mult, op1=ALU.add),
                          reads=[pb, b_modT, xb], writes=[xb])
                cx.dma("sp", xs[j], xt[:, :], reads=[xb], writes=[b_xs[j]])
        norm_x(l, 2)
        apply_norm(l, 2)
        cx.fence()
        ar.reset()
        if stop_after == "attn":
            for j in range(16):
                xt, xb = next_scr()
                cx.dma("sp", xt[:, :], xs[j], reads=[b_xs[j]], writes=[xb])
                cx.dma("sp", dbg_out[:, j * T:(j + 1) * T], xt[:, :], reads=[xb], is_output=True)
            break
        skT = ar.take([128, 2, 128], BF16); b_skT = Buf("skT")
        skst = ar.take([128, 2, 128], F32); b_skst = Buf("skst")
        qT = ar.take([128, 16, 512], BF16); b_qT = Buf("qT")
        s1 = ar.take([128, 16, 128], F32); b_s1 = Buf("s1")
        s2 = ar.take([128, 16, 128], F32); b_s2 = Buf("s2")
        vtop = ar.take([128, 16, 16], F32); b_vtop = Buf("vtop")
        ix = ar.take([128, 16, 16], U32); b_ix = Buf("ix")
        ixf = ar.take([128, 16, 16], F32); b_ixf = Buf("ixf")
        best = ar.take([128, 8, 16], F32); b_best = Buf("best")
        sel = ar.take([128, 8, 16], U32); b_sel = Buf("sel")
        ru = ar.take([128, 2, 8, 16], U32); b_ru = Buf("ru")
        rf = ar.take([128, 2, 8, 16], F32); b_rf = Buf("rf")
        ab = ar.take([128, 2, 8, 16], F32); b_ab = Buf("ab")
        idxf = ar.take([128, 128], F32); b_idxf = Buf("idxf")
        idxi = ar.take([128, 128], I32); b_idxi = Buf("idxi")
        gw = ar.take([128, 8, 16], F32); b_gw = Buf("gw")
        sm = ar.take([128, 32], F32); b_sm = Buf("sm")
        h2tok = ar.take([128, 2048], BF16); b_h2tok = Buf("h2tok")
        junk = ar.take([128, 2048], BF16); b_junk = Buf("junk")
        actv = ar.take([128, 128], F32); b_actv = Buf("actv")
        wgt = ar.take([128, 128], F32); b_wgt = Buf("wgt")
        NG = 4
        gbuf = [ar.take([128, 2048], BF16) for _ in range(NG)]; b_gbuf = [Buf("g%d" % i) for i in range(NG)]
        diag = [ar.take([128, 128], BF16) for _ in range(2)]; b_diag = [Buf("dg0"), Buf("dg1")]
        otok = [ar.take([128, 512], F32) for _ in range(2)]; b_otok = [Buf("ot0"), Buf("ot1")]
        xupd = [ar.take([128, 4, 128], F32) for _ in range(2)]; b_xupd = [Buf("xu0"), Buf("xu1")]
        conv_step(l, 16)
        cx.dma("sp", skst, subkeys[l].rearrange("p k c -> k p c"), writes=[b_skst])
        for p in range(2):
            transpose_f32(lambda ps_ap, pb, p=p: cx.op("act", lambda e: e.activation(out=skT[:, p, :], in_=ps_ap, func=AF.Copy),
                                                       reads=[pb], writes=[b_skT]), skst[:, p, :], 128, 128, [b_skst])
        gcnt = 0
        for th in range(2):
            for jg in range(4):
                wv, wb = load_w(w_peer_q[l, :, jg * 512:(jg + 1) * 512], 16, 512)
                for jj in range(4):
                    pt, pb = mm_fm(wv, wb, jj * 128, 128, D, hrhs, hbufs, th)
                    cx.op("act", lambda e: e.activation(out=qT[:, jg * 4 + jj, :], in_=pt[:, :], func=AF.Copy), reads=[pb], writes=[b_qT])
            for tb4 in range(4):
                tb = th * 4 + tb4
                tsl = slice(tb4 * 128, (tb4 + 1) * 128)
                banks = pr.hold(4)
                for hp in range(16):
                    bt, bbf = banks[hp // 4]
                    cx.op("pe", lambda e: e.matmul(bt[:, (hp % 4) * 128:(hp % 4 + 1) * 128], qT[:, hp, tsl], skT[:, hp % 2, :],
                                                   start=True, stop=True), reads=[b_qT, b_skT], writes=[bbf])
                for q4 in range(4):
                    bt, bbf = banks[q4]
                    cx.op("act", lambda e: e.activation(out=s1[:, q4 * 4:(q4 + 1) * 4, :], in_=bt[:, :].rearrange("p (a b) -> p a b", a=4),
                                                        func=AF.Copy), reads=[bbf], writes=[b_s1])
                pr.release(banks)
                for hp in range(16):
                    cx.op("dve", lambda e: e.max(out=vtop[:, hp, 0:8], in_=s1[:, hp, :]), reads=[b_s1], writes=[b_vtop])
                    cx.op("dve", lambda e: e.max_index(out=ix[:, hp, 0:8], in_max=vtop[:, hp, 0:8], in_values=s1[:, hp, :]),
                          reads=[b_s1, b_vtop], writes=[b_ix])
                    cx.op("dve", lambda e: e.match_replace(out=s2[:, hp, :], in_to_replace=vtop[:, hp, 0:8], in_values=s1[:, hp, :],
                                                           imm_value=-1e30), reads=[b_s1, b_vtop], writes=[b_s2])
                    cx.op("dve", lambda e: e.max(out=vtop[:, hp, 8:16], in_=s2[:, hp, :]), reads=[b_s2], writes=[b_vtop])
                    cx.op("dve", lambda e: e.max_index(out=ix[:, hp, 8:16], in_max=vtop[:, hp, 8:16], in_values=s2[:, hp, :]),
                          reads=[b_s2, b_vtop], writes=[b_ix])
                cand = s1.rearrange("p (h a) (b c) -> p h a b c", a=2, b=8)
                cand4 = s1.rearrange("p (h a) (b c) -> p h (a b) c", a=2, b=8)
                cand2_4 = s2.rearrange("p (h a) (b c) -> p h (a b) c", a=2, b=8)
                candf = s1.rearrange("p (h a) k -> p h (a k)", a=2)
                cand2f = s2.rearrange("p (h a) k -> p h (a k)", a=2)
                vt4 = vtop.rearrange("p (h a) k -> p h a k", a=2)
                cx.op("dve", lambda e: e.tensor_tensor(out=cand4, in0=vt4[:, :, 0, :].unsqueeze(3).to_broadcast([128, 8, 16, 16]),
                                                       in1=vt4[:, :, 1, :].unsqueeze(2).to_broadcast([128, 8, 16, 16]), op=ALU.add),
                      reads=[b_vtop], writes=[b_s1])
                for h in range(8):
                    cx.op("dve", lambda e: e.max(out=best[:, h, 0:8], in_=candf[:, h, :]), reads=[b_s1], writes=[b_best])
                    cx.op("dve", lambda e: e.max_index(out=sel[:, h, 0:8], in_max=best[:, h, 0:8], in_values=candf[:, h, :]),
                          reads=[b_s1, b_best], writes=[b_sel])
                    cx.op("dve", lambda e: e.match_replace(out=cand2f[:, h, :], in_to_replace=best[:, h, 0:8], in_values=candf[:, h, :],
                                                           imm_value=-1e30), reads=[b_s1, b_best], writes=[b_s2])
                    cx.op("dve", lambda e: e.max(out=best[:, h, 8:16], in_=cand2f[:, h, :]), reads=[b_s2], writes=[b_best])
                    cx.op("dve", lambda e: e.max_index(out=sel[:, h, 8:16], in_max=best[:, h, 8:16], in_values=cand2f[:, h, :]),
                          reads=[b_s2, b_best], writes=[b_sel])
                cx.op("dve", lambda e: e.tensor_single_scalar(out=ru[:, 0], in_=sel, scalar=4, op=ALU.logical_shift_right),
                      reads=[b_sel], writes=[b_ru])
                cx.op("dve", lambda e: e.tensor_single_scalar(out=ru[:, 1], in_=sel, scalar=15, op=ALU.bitwise_and),
                      reads=[b_sel], writes=[b_ru])
                cx.op("dve", lambda e: e.tensor_copy(out=rf, in_=ru), reads=[b_ru], writes=[b_rf])
                cx.op("dve", lambda e: e.tensor_copy(out=ixf, in_=ix), reads=[b_ix], writes=[b_ixf])
                ixf4 = ixf.rearrange("p (h a) k -> p h a k", a=2)
                for a in range(2):
                    cx.op("dve", lambda e: e.tensor_tensor(out=cand4, in0=iota16[:, :].unsqueeze(1).unsqueeze(1).to_broadcast([128, 8, 16, 16]),
                                                           in1=rf[:, a].unsqueeze(3).to_broadcast([128, 8, 16, 16]), op=ALU.is_equal),
                          reads=[b_rf, b_const], writes=[b_s1])
                    cx.op("dve", lambda e: e.tensor_tensor(out=cand2_4, in0=cand4,
                                                           in1=ixf4[:, :, a, :].unsqueeze(2).to_broadcast([128, 8, 16, 16]), op=ALU.mult),
                          reads=[b_s1, b_ixf], writes=[b_s2])
                    cx.op("dve", lambda e: e.tensor_reduce(out=ab[:, a], in_=cand2_4, axis=AX.X, op=ALU.add),
                          reads=[b_s2], writes=[b_ab])
                cx.op("dve", lambda e: e.scalar_tensor_tensor(out=idxf.rearrange("p (h k) -> p h k", h=8), in0=ab[:, 0], scalar=128.0,
                                                              in1=ab[:, 1], op0=ALU.mult, op1=ALU.add), reads=[b_ab], writes=[b_idxf])
                cx.op("dve", lambda e: e.tensor_scalar(out=idxf, in0=idxf, scalar1=float(l * 16384), scalar2=None, op0=ALU.add),
                      reads=[b_idxf], writes=[b_idxf])
                cx.op("dve", lambda e: e.tensor_copy(out=idxi, in_=idxf), reads=[b_idxf], writes=[b_idxi])
                cx.op("dve", lambda e: e.tensor_scalar(out=sm[:, 0:8], in0=best[:, :, 0], scalar1=-1.0, scalar2=None, op0=ALU.mult),
                      reads=[b_best], writes=[b_sm])
                for h in range(8):
                    cx.op("act", lambda e: e.activation(out=gw[:, h, :], in_=best[:, h, :], func=AF.Exp, bias=sm[:, h:h + 1], scale=1.0,
                                                        accum_out=sm[:, 8 + h:9 + h]), reads=[b_best, b_sm], writes=[b_gw, b_sm])
                cx.op("dve", lambda e: e.reciprocal(out=sm[:, 16:24], in_=sm[:, 8:16]), reads=[b_sm], writes=[b_sm])
                cx.op("dve", lambda e: e.tensor_tensor(out=gw, in0=gw, in1=sm[:, 16:24].unsqueeze(2).to_broadcast([128, 8, 16]), op=ALU.mult),
                      reads=[b_gw, b_sm], writes=[b_gw])
                for j8 in range(2):
                    pt, pb = pr.next()
                    ptb = pt[:, :].bitcast(BF16)
                    for jj in range(8):
                        j = j8 * 8 + jj
                        cx.op("pe", lambda e: e.transpose(ptb[:, jj * 128:(jj + 1) * 128], hT[:, j, tb * 128:(tb + 1) * 128], ident_b[:, :]),
                              reads=[b_hT[j], b_const], writes=[pb])
                    cx.op("act", lambda e: e.activation(out=h2tok[:, j8 * 1024:(j8 + 1) * 1024], in_=ptb, func=AF.Copy),
                          reads=[pb], writes=[b_h2tok])
                for kk in range(128):
                    gi = gcnt % NG
                    gcnt += 1
                    cx.dma("pool", gbuf[gi][:, :], pu16, indirect=idxi[:, kk:kk + 1], reads=[b_idxi] + b_tab[l][0:8], writes=[b_gbuf[gi]])
                    cx.op("dve", lambda e: e.scalar_tensor_tensor(out=junk[:, :], in0=gbuf[gi][:, :], scalar=1.0, in1=h2tok[:, :],
                                                                  op0=ALU.mult, op1=ALU.mult, accum_out=actv[:, kk:kk + 1]),
                          reads=[b_gbuf[gi], b_h2tok], writes=[b_junk, b_actv])
                cx.op("act", lambda e: e.activation(out=wgt[:, :], in_=actv[:, :], func=AF.Gelu), reads=[b_actv], writes=[b_wgt])
                cx.op("dve", lambda e: e.tensor_tensor(out=wgt[:, :], in0=wgt[:, :], in1=gw.rearrange("p h k -> p (h k)"), op=ALU.mult),
                      reads=[b_wgt, b_gw], writes=[b_wgt])
                accb = pr.hold(4)
                for kk in range(128):
                    gi = gcnt % NG
                    gcnt += 1
                    cx.dma("pool", gbuf[gi][:, :], pv16, indirect=idxi[:, kk:kk + 1], reads=[b_idxi] + b_tab[l][8:16], writes=[b_gbuf[gi]])
                    dg, dgb = diag[kk % 2], b_diag[kk % 2]
                    cx.op("dve", lambda e: e.tensor_scalar(out=dg[:, :], in0=ident_b[:, :], scalar1=wgt[:, kk:kk + 1], scalar2=None, op0=ALU.mult),
                          reads=[b_wgt, b_const], writes=[dgb])
                    for dq in range(4):
                        cx.op("pe", lambda e: e.matmul(accb[dq][0][:, :], dg[:, :], gbuf[gi][:, dq * 512:(dq + 1) * 512],
                                                       start=(kk == 0), stop=(kk == 127)), reads=[dgb, b_gbuf[gi]], writes=[accb[dq][1]])
                for dq in range(4):
                    ot, otb = otok[dq % 2], b_otok[dq % 2]
                    xu, xub = xupd[dq % 2], b_xupd[dq % 2]
                    cx.op("act", lambda e: e.activation(out=ot[:, :], in_=accb[dq][0][:, :], func=AF.Copy), reads=[accb[dq][1]], writes=[otb])
                    cx.dma("sp", xu, xs[dq * 4:(dq + 1) * 4, :, tb * 128:(tb + 1) * 128].rearrange("j p t -> p j t"),
                           reads=[b_xs[dq * 4 + i] for i in range(4)], writes=[xub])
                    for jj in range(4):
                        j = dq * 4 + jj
                        transpose_f32(lambda ps_ap, pb, jj=jj, j=j: cx.op(
                            "dve", lambda e: e.scalar_tensor_tensor(out=xu[:, jj, :], in0=ps_ap, scalar=modT[:, 80 + j:81 + j],
                                                                    in1=xu[:, jj, :], op0=ALU.mult, op1=ALU.add),
                            reads=[pb, b_modT, xub], writes=[xub]), ot[:, jj * 128:(jj + 1) * 128], 128, 128, [otb])
                    cx.dma("sp", xs[dq * 4:(dq + 1) * 4, :, tb * 128:(tb + 1) * 128].rearrange("j p t -> p j t"), xu,
                           reads=[xub], writes=[b_xs[dq * 4 + i] for i in range(4)])
                pr.release(accb)
        cx.fence()
        if stop_after == "layer":
            for j in range(16):
                xt, xb = next_scr()
                cx.dma("sp", xt[:, :], xs[j], reads=[b_xs[j]], writes=[xb])
                cx.dma("sp", dbg_out[:, j * T:(j + 1) * T], xt[:, :], reads=[xb], is_output=True)
            break
    else:
        norm_x(L, 1)
        for j in range(16):
            xt, xb = next_scr()
            cx.dma("sp", xt[:, :], xs[j], reads=[b_xs[j]], writes=[xb])
            cx.op("dve", lambda e: e.tensor_tensor(out=xt[:, :], in0=xt[:, :], in1=rs[:, :], op=ALU.mult), reads=[xb, b_rs], writes=[xb])
            cx.op("dve", lambda e: e.tensor_scalar(out=xt[:, :], in0=xt[:, :], scalar1=gvT[:, 16 + j:17 + j], scalar2=None, op0=ALU.mult),
                  reads=[xb, b_gvT], writes=[xb])
            for tc in range(8):
                st, sbf = next_stg()
                transpose_f32(lambda ps_ap, pb, st=st, sbf=sbf: cx.op("act", lambda e: e.activation(out=st[:, :], in_=ps_ap, func=AF.Copy),
                                                                      reads=[pb], writes=[sbf]), xt[:, tc * 128:(tc + 1) * 128], 128, 128, [xb])
                cx.dma("sp", y_out[tc * 128:(tc + 1) * 128, j * 128:(j + 1) * 128], st[:, :], reads=[sbf], is_output=True)

    cx.finish()
    es.close()
    return nc, in_names


def _rope_tables(sample):
    f = np.arange(64)
    axis = f // 32
    jj = f % 16
    hf = (f % 32) // 16
    t = np.arange(T)
    if sample:
        pos = np.where(axis[:, None] == 0, (t // 64)[None, :], (t % 64)[None, :]).astype(np.float32)
        inv = (10000.0 ** (-(jj.astype(np.float32)) / 16.0)).astype(np.float32)
        ang = pos * inv[:, None]
        c = np.cos(ang).astype(np.float32)
        s = np.sin(ang).astype(np.float32)
        s = np.where(hf[:, None] == 0, -s, s).astype(np.float32)
    else:
        c = np.ones((64, T), np.float32)
        s = np.zeros((64, T), np.float32)
    partner = np.where(hf == 0, f + 16, f - 16)
    perm = np.zeros((64, 64), np.float32)
    perm[partner, f] = 1.0
    return c, s, perm


def _core_consts(sample):
    c, s, perm = _rope_tables(sample)
    t = np.arange(T)
    qaug = np.zeros((4, T), np.float32)
    kaug = np.zeros((4, TK), np.float32)
    seglen = T if sample else 256
    if not sample:
        seg = t // 256
        for sp in range(4):
            qaug[sp] = -(seg == sp).astype(np.float32)
            kaug[sp, :PAST] = BIG
            kaug[sp, PAST:] = BIG * (seg != sp).astype(np.float32)
    invcnt = np.zeros((4, T), np.float32)
    tt = t % seglen
    for gi, w in enumerate((2, 4, 8, 16)):
        lo = np.maximum(tt - w // 2, 0)
        hi = np.minimum(tt + (w - 1) // 2, seglen - 1)
        invcnt[gi] = 1.0 / (hi - lo + 1).astype(np.float32)
    sel4 = np.zeros((4, 4, 128), np.float32)
    for g in range(4):
        sel4[g, g, :] = 1.0
    return {
        "ident": np.eye(128, dtype=np.float32), "perm64": perm, "ropec": c, "ropes": s, "qaug": qaug, "kaug": kaug,
        "flag": np.full((128, 1), 1.0 if sample else 0.0, np.float32), "invcnt": invcnt,
        "sel4": sel4.reshape(4, 512), "iota16": np.tile(np.arange(16, dtype=np.float32), (128, 1)),
    }


def make_in_maps(inp, L=4, cores=range(8), names=None):
    f = lambda a: np.ascontiguousarray(np.asarray(a, dtype=np.float32))
    vecs = np.zeros((L, 144, 128), np.float32)
    for l in range(L):
        vecs[l, 0:96] = f(inp["b_mod"])[l].reshape(96, 128)
        vecs[l, 96:112] = f(inp["g_norm1"])[l].reshape(16, 128)
        vecs[l, 112:128] = f(inp["g_norm2"])[l].reshape(16, 128)
        vecs[l, 128:132] = f(inp["g_qnorm"])[l].reshape(4, 128)
        vecs[l, 132:134] = f(inp["g_kvnorm"])[l].reshape(2, 128)
        vecs[l, 134:138] = f(inp["pool_scale"])[l].reshape(4, 128)
        vecs[l, 138] = f(inp["g_diffnorm"])[l]
    shared = {
        "w_mod": f(inp["w_mod"])[:L], "vecs": vecs, "w_in": f(inp["w_in"])[:L], "w_qb": f(inp["w_qb"])[:L],
        "w_kvb": f(inp["w_kvb"])[:L], "w_pool": f(inp["w_pool"])[:L],
        "diff_lambda": f(inp["diff_lambda"])[:L].reshape(L, 256),
        "w_br_mla": f(inp["w_br_mla"])[:L], "w_br_pool": f(inp["w_br_pool"])[:L], "w_br_diff": f(inp["w_br_diff"])[:L],
        "w_out": f(inp["w_out"])[:L], "w_peer_q": f(inp["w_peer_q"])[:L], "peer_subkeys": f(inp["peer_subkeys"])[:L],
    }
    if names is None or "peer_u" in names:
        shared["peer_u"] = f(inp["peer_u"])[:L]
        shared["peer_v"] = f(inp["peer_v"])[:L]
    xp = f(inp["x_prompt"]); xsm = f(inp["x_sample"])
    cs = {True: _core_consts(True), False: _core_consts(False)}
    maps = []
    for c in cores:
        sample = c >= 4
        m = dict(shared)
        m.update(cs[sample])
        gv = np.zeros((32, 128), np.float32)
        gv[16:32] = f(inp["g_final"]).reshape(16, 128)
        if not sample:
            m["x"] = xp[4 * c:4 * c + 4].reshape(T, D)
            gv[0:16] = f(inp["c_ctx"]).reshape(16, 128)
            m["ctx_ckv"] = np.zeros((L, PAST, 256), np.float32)
            m["ctx_krope"] = np.zeros((L, PAST, 64), np.float32)
            m["ctx_dk"] = np.zeros((L, PAST, 512), np.float32)
            m["ctx_dv"] = np.zeros((L, PAST, 512), np.float32)
        else:
            b = (c - 4) % 2
            m["x"] = xsm[b]
            gv[0:16] = f(inp["c"])[b].reshape(16, 128)
            m["ctx_ckv"] = f(inp["cache_mla_ckv"])[b, :L]
            m["ctx_krope"] = f(inp["cache_mla_krope"])[b, :L]
            m["ctx_dk"] = f(inp["cache_diff_k"])[b, :L].reshape(L, PAST, 512)
            m["ctx_dv"] = f(inp["cache_diff_v"])[b, :L].reshape(L, PAST, 512)
        m["gvec"] = gv
        if names is not None:
            m = {k: v for k, v in m.items() if k in names}
        maps.append(m)
    return maps


def kernel(**inp):
    L = 4
    nc, names = build_program(L)
    maps = make_in_maps(inp, L, names=names)
    res = run_bass_kernel_spmd(nc, maps, core_ids=list(range(8)))
    r = res.results
    y_prompt = np.zeros((16, 256, D), np.float32)
    st_ckv = np.zeros((16, L, 256, 256), np.float32)
    st_krope = np.zeros((16, L, 256, 64), np.float32)
    st_k = np.zeros((16, L, 256, 4, 128), np.float32)
    st_v = np.zeros((16, L, 256, 4, 128), np.float32)
    for c in range(4):
        y_prompt[4 * c:4 * c + 4] = r[c]["y"].reshape(4, 256, D)
        for l in range(L):
            st_ckv[4 * c:4 * c + 4, l] = r[c]["o_ckv"][l].reshape(4, 256, 256)
            st_krope[4 * c:4 * c + 4, l] = r[c]["o_krope"][l].reshape(4, 256, 64)
            st_k[4 * c:4 * c + 4, l] = r[c]["o_dk"][l].reshape(4, 256, 4, 128)
            st_v[4 * c:4 * c + 4, l] = r[c]["o_dv"][l].reshape(4, 256, 4, 128)
    y_sample = np.stack([r[4]["y"], r[5]["y"]], axis=0)
    return (y_prompt, y_sample, st_ckv, st_krope, st_k, st_v)
```

```python
import contextlib
import math

import numpy as np
import concourse.bass as bass
import concourse.mybir as mybir
from concourse.bass_utils import run_bass_kernel_spmd

F32 = mybir.dt.float32
BF16 = mybir.dt.bfloat16
I32 = mybir.dt.int32
U32 = mybir.dt.uint32
AF = mybir.ActivationFunctionType
ALU = mybir.AluOpType
AX = mybir.AxisListType

D = 2048
T = 1024
TK = 1280
PAST = 256
NDS = 40
EPS = 1e-6
BIG = 30000.0

C_QA, C_KVA, C_PZ, C_DQ, C_DK, C_DV, C_GL = 0, 512, 832, 1344, 1856, 2368, 2880


class Buf:
    __slots__ = ("name", "w", "r")

    def __init__(self, name):
        self.name = name
        self.w = None
        self.r = {}


class Ctx:
    def __init__(self, nc, es):
        self.nc = nc
        self.es = es
        self.eng = {}
        for name, h in (("pe", nc.tensor), ("dve", nc.vector), ("act", nc.scalar),
                        ("pool", nc.gpsimd), ("sp", nc.sync)):
            self.eng[name] = {"h": h, "sem": es.enter_context(nc.semaphore("e_" + name)), "cnt": 0,
                              "known": {}, "name": name}
        self.dsems = [es.enter_context(nc.semaphore("d%d" % i)) for i in range(NDS)]
        self.dcnt = [0] * NDS
        self.dnext = 0
        self.ntile = 0
        self.out_events = []

    def sb(self, name, shape, dtype):
        self.ntile += 1
        return self.es.enter_context(self.nc.sbuf_tensor("%s_%d" % (name, self.ntile), list(shape), dtype))

    def ps(self, name, shape, dtype):
        self.ntile += 1
        return self.es.enter_context(self.nc.psum_tensor("%s_%d" % (name, self.ntile), list(shape), dtype))

    def _wait(self, e, ev):
        sem, val = ev
        k = id(sem)
        if e["known"].get(k, 0) >= val:
            return
        e["h"].wait_ge(sem, val)
        e["known"][k] = val

    def _collect(self, reads, writes):
        evs = []
        for b in reads:
            if b.w is not None:
                evs.append(b.w)
        for b in writes:
            if b.w is not None:
                evs.append(b.w)
            evs.extend(b.r.values())
        return evs

    def op(self, ename, fn, reads=(), writes=()):
        e = self.eng[ename]
        for ev in self._collect(reads, writes):
            self._wait(e, ev)
        inst = fn(e["h"])
        e["cnt"] += 1
        inst.then_inc(e["sem"], 1)
        ev = (e["sem"], e["cnt"])
        e["known"][id(e["sem"])] = e["cnt"] if ename == "pe" else e["cnt"] - 1
        for b in reads:
            b.r[ename] = ev
        for b in writes:
            b.w = ev
            b.r = {}
        return ev

    def dma(self, qname, out, in_, reads=(), writes=(), indirect=None, is_output=False):
        e = self.eng[qname]
        for ev in self._collect(reads, writes):
            self._wait(e, ev)
        slot = self.dnext
        self.dnext = (slot + 1) % NDS
        if self.dcnt[slot] > 0:
            self._wait(e, (self.dsems[slot], 16 * self.dcnt[slot]))
        self.dcnt[slot] += 1
        if indirect is not None:
            inst = e["h"].indirect_dma_start(out=out, out_offset=None, in_=in_,
                                             in_offset=bass.IndirectOffsetOnAxis(ap=indirect, axis=0))
        else:
            inst = e["h"].dma_start(out=out, in_=in_)
        inst.then_inc(self.dsems[slot], 16)
        ev = (self.dsems[slot], 16 * self.dcnt[slot])
        for b in reads:
            b.r[("d", slot)] = ev
        for b in writes:
            b.w = ev
            b.r = {}
        if is_output:
            self.out_events.append(ev)
        return ev

    def fence(self):
        evs = [(e["sem"], e["cnt"]) for e in self.eng.values() if e["cnt"] > 0]
        evs += [(self.dsems[s], 16 * self.dcnt[s]) for s in range(NDS) if self.dcnt[s] > 0]
        for e in self.eng.values():
            for ev in evs:
                if ev[0] is e["sem"]:
                    continue
                self._wait(e, ev)

    def finish(self):
        e = self.eng["sp"]
        for ev in self.out_events:
            self._wait(e, ev)
        evs = [(x["sem"], x["cnt"]) for x in self.eng.values() if x["cnt"] > 0 and x is not e]
        evs += [(self.dsems[s], 16 * self.dcnt[s]) for s in range(NDS) if self.dcnt[s] > 0]
        for ev in evs:
            self._wait(e, ev)


class PsumRing:
    def __init__(self, ctx, n=8):
        self.tiles = [ctx.ps("bank", [128, 512], F32) for _ in range(n)]
        self.bufs = [Buf("bank%d" % i) for i in range(n)]
        self.i = 0
        self.n = n
        self.held = set()

    def next(self):
        while self.i in self.held:
            self.i = (self.i + 1) % self.n
        i = self.i
        self.i = (i + 1) % self.n
        return self.tiles[i], self.bufs[i]

    def next_n(self, k):
        return [self.next() for _ in range(k)]

    def hold(self, k):
        out = []
        for _ in range(k):
            t, b = self.next()
            self.held.add(self.tiles.index(t))
            out.append((t, b))
        return out

    def release(self, banks):
        for t, b in banks:
            self.held.discard(self.tiles.index(t))


def build_program(L=4, stop_after=None):
    nc = bass.Bass("TRN2", target_bir_lowering=False)
    es = contextlib.ExitStack()
    cx = Ctx(nc, es)
    dbg = stop_after is not None

    in_names = []

    def din(name, shape, dtype=F32):
        in_names.append(name)
        return nc.dram_tensor(name, list(shape), dtype, kind="ExternalInput").ap()

    def dout(name, shape, dtype=F32):
        return nc.dram_tensor(name, list(shape), dtype, kind="ExternalOutput").ap()

    x_in = din("x", [T, D])
    gvec = din("gvec", [32, 128])
    ctx_ckv = din("ctx_ckv", [L, PAST, 256])
    ctx_krope = din("ctx_krope", [L, PAST, 64])
    ctx_dk = din("ctx_dk", [L, PAST, 512])
    ctx_dv = din("ctx_dv", [L, PAST, 512])
    ident_d = din("ident", [128, 128])
    perm_d = din("perm64", [64, 64])
    ropec_d = din("ropec", [64, T])
    ropes_d = din("ropes", [64, T])
    qaug_d = din("qaug", [4, T])
    kaug_d = din("kaug", [4, TK])
    flag_d = din("flag", [128, 1])
    invcnt_d = din("invcnt", [4, T])
    sel4_d = din("sel4", [4, 512])
    iota16_d = din("iota16", [128, 16])
    w_mod = din("w_mod", [L, D, 6 * D])
    vecs = din("vecs", [L, 144, 128])
    w_in = din("w_in", [L, D, 9024])
    w_qb = din("w_qb", [L, 512, 1536])
    w_kvb = din("w_kvb", [L, 256, 2048])
    w_pool = din("w_pool", [L, 4, 128, 128])
    dlam = din("diff_lambda", [L, 256])
    w_br_mla = din("w_br_mla", [L, 1024, D])
    w_br_pool = din("w_br_pool", [L, 512, D])
    w_br_diff = din("w_br_diff", [L, 512, D])
    w_out = din("w_out", [L, D, D])
    w_peer_q = din("w_peer_q", [L, D, D])
    subkeys = din("peer_subkeys", [L, 2, 128, 128])
    need_peer = stop_after is None or stop_after.startswith("peer") or stop_after == "layer"
    if need_peer:
        peer_u = din("peer_u", [L, 16384, D])
        peer_v = din("peer_v", [L, 16384, D])

    y_out = dout("y", [T, D])
    o_ckv = dout("o_ckv", [L, T, 256])
    o_krope = dout("o_krope", [L, T, 64])
    o_dk = dout("o_dk", [L, T, 512])
    o_dv = dout("o_dv", [L, T, 512])
    if dbg:
        dbg_out = dout("dbg", [128, 16 * T])

    xs = nc.dram_tensor("xs", [16, 128, T], F32, kind="Internal").ap()
    if need_peer:
        pu16 = nc.dram_tensor("pu16", [L * 16384, D], BF16, kind="Internal").ap()
        pv16 = nc.dram_tensor("pv16", [L * 16384, D], BF16, kind="Internal").ap()
        pu_flat = peer_u.rearrange("l e d -> (l e) d")
        pv_flat = peer_v.rearrange("l e d -> (l e) d")
    b_tab = [[Buf("tab%d_%d" % (l, i)) for i in range(16)] for l in range(L)]
    conv_pos = [0] * L

    def conv_step(l, n):
        if not need_peer:
            return
        for _ in range(n):
            i = conv_pos[l]
            if i >= 16:
                return
            conv_pos[l] += 1
            src, dst = (pu_flat, pu16) if i < 8 else (pv_flat, pv16)
            r0 = l * 16384 + (i % 8) * 2048
            cx.dma("pool", dst[r0:r0 + 2048, :].rearrange("(p r) d -> p r d", p=128),
                   src[r0:r0 + 2048, :].rearrange("(p r) d -> p r d", p=128), writes=[b_tab[l][i]])

    ident_f = cx.sb("ident_f", [128, 128], F32); b_ident = Buf("ident")
    ident_b = cx.sb("ident_b", [128, 128], BF16)
    ones_f = cx.sb("ones_f", [128, 128], F32)
    ones_b = cx.sb("ones_b", [128, 128], BF16)
    perm_f = cx.sb("perm", [64, 64], F32)
    ropec = cx.sb("ropec", [64, T], F32)
    ropes = cx.sb("ropes", [64, T], F32)
    flag = cx.sb("flag", [128, 1], F32)
    invcnt = cx.sb("invcnt", [4, T], F32)
    sel4 = cx.sb("sel4", [4, 512], F32)
    iota16 = cx.sb("iota16", [128, 16], F32)
    b_const = Buf("consts")
    hT = cx.sb("hT", [128, 16, T], BF16)
    b_hT = [Buf("hT%d" % j) for j in range(16)]
    NW = 2
    wbuf = [cx.sb("wbuf", [128, 8192], BF16) for _ in range(NW)]
    b_wbuf = [Buf("wbuf%d" % i) for i in range(NW)]
    wsm = [cx.sb("wsm", [128, 4096], BF16) for _ in range(2)]
    b_wsm = [Buf("wsm%d" % i) for i in range(2)]
    scr = [cx.sb("scr", [128, T], F32) for _ in range(3)]
    b_scr = [Buf("scr%d" % i) for i in range(3)]
    scr_i = [0]
    rs = cx.sb("rs", [128, T], F32); b_rs = Buf("rs")
    modT = cx.sb("modT", [128, 96], F32); b_modT = Buf("modT")
    vecT = cx.sb("vecT", [128, 144], F32); b_vecT = Buf("vecT")
    gvT = cx.sb("gvT", [128, 32], F32); b_gvT = Buf("gvT")
    scondT = cx.sb("scondT", [128, 16], BF16); b_scond = Buf("scond")
    drv = cx.sb("drv", [128, 64], F32); b_drv = Buf("drv")
    lamt = cx.sb("lamt", [128, 264], F32); b_lam = Buf("lam")
    stg = [cx.sb("stg", [128, 128], F32) for _ in range(4)]
    b_stg = [Buf("stg%d" % i) for i in range(4)]
    stg_i = [0]
    ARENA = 90 * 1024
    arena = cx.sb("arena", [128, ARENA // 2], BF16)

    pr = PsumRing(cx, 8)

    def next_scr():
        i = scr_i[0]
        scr_i[0] = (i + 1) % 3
        return scr[i], b_scr[i]

    def next_stg():
        i = stg_i[0]
        stg_i[0] = (i + 1) % 4
        return stg[i], b_stg[i]

    class Arena:
        def __init__(self):
            self.off = 0

        def reset(self):
            self.off = 0

        def take(self, shape, dtype):
            n = 1
            for s in shape[1:]:
                n *= s
            nbytes = n * (4 if dtype in (F32, I32, U32) else 2)
            nbytes = (nbytes + 63) // 64 * 64
            assert self.off + nbytes <= ARENA, ("arena overflow", self.off, nbytes)
            a = arena[:, self.off // 2:(self.off + nbytes) // 2]
            self.off += nbytes
            if dtype != BF16:
                a = a.bitcast(dtype)
            a = a[:, 0:n]
            if len(shape) == 3:
                a = a.rearrange("p (a b) -> p a b", a=shape[1])
            elif len(shape) == 4:
                a = a.rearrange("p (a b c) -> p a b c", a=shape[1], b=shape[2])
            return a

    ar = Arena()

    for t_sb, t_d in ((ident_f, ident_d), (perm_f, perm_d), (ropec, ropec_d), (ropes, ropes_d), (flag, flag_d),
                      (invcnt, invcnt_d), (sel4, sel4_d), (iota16, iota16_d)):
        cx.dma("sp", t_sb[:], t_d, writes=[b_const])
    cx.dma("pool", ident_b[:], ident_d, writes=[b_const])
    cx.op("dve", lambda e: e.memset(ones_f[:], 1.0), writes=[b_const])
    cx.op("dve", lambda e: e.memset(ones_b[:], 1.0), writes=[b_const])

    def transpose_f32(dst_fn, src_ap, p_in, f_in, reads, n_consume=1):
        pt, pb = pr.next()
        cx.op("pe", lambda e: e.transpose(pt[0:f_in, 0:p_in], src_ap, ident_f[0:p_in, 0:p_in]),
              reads=list(reads) + [b_const], writes=[pb])
        dst_fn(pt[0:f_in, 0:p_in], pb)

    for tc in range(8):
        xt, xb = next_scr()
        for hh in range(2):
            cx.dma("sp", xt[:, 0:T], x_in[tc * 128:(tc + 1) * 128, hh * T:(hh + 1) * T], writes=[xb])
            for jj in range(8):
                j = hh * 8 + jj
                st, sbf = next_stg()

                def ev(ps_ap, pb, st=st, sbf=sbf):
                    cx.op("act", lambda e: e.activation(out=st[:, :], in_=ps_ap, func=AF.Copy),
                          reads=[pb], writes=[sbf])
                transpose_f32(ev, xt[:, jj * 128:(jj + 1) * 128], 128, 128, [xb])
                cx.dma("sp", xs[j, :, tc * 128:(tc + 1) * 128], st[:, :], reads=[sbf], writes=[])
    b_xs = [Buf("xs%d" % j) for j in range(16)]
    cx.fence()

    st, sbf = next_stg()
    cx.dma("sp", st[0:32, :], gvec, writes=[sbf])
    transpose_f32(lambda ps_ap, pb: cx.op("act", lambda e: e.activation(out=gvT[:, :], in_=ps_ap, func=AF.Copy),
                                          reads=[pb], writes=[b_gvT]), st[0:32, :], 32, 128, [sbf])
    cx.op("act", lambda e: e.activation(out=scondT[:, :], in_=gvT[:, 0:16], func=AF.Silu),
          reads=[b_gvT], writes=[b_scond])

    w_i = [0]

    def load_w(dram_ap, kc, ncols, small=False):
        if small:
            i = w_i[0] % 2
            tile, buf = wsm[i], b_wsm[i]
            assert kc * ncols <= 4096
        else:
            i = w_i[0] % NW
            tile, buf = wbuf[i], b_wbuf[i]
            assert kc * ncols <= 8192
        w_i[0] += 1
        view = tile[:, 0:kc * ncols].rearrange("p (k n) -> p k n", k=kc)
        rows = dram_ap.shape[0]
        if rows >= 128:
            cx.dma("pool", view, dram_ap.rearrange("(k p) n -> p k n", p=128), writes=[buf])
        else:
            cx.dma("pool", view[0:rows, 0, :], dram_ap, writes=[buf])
        return view, buf

    def proj_fm(w_dram, K, cols, rhs_fn, rhs_bufs, consume, small=False, group=512, halves=(0, 1)):
        kc_n = max(1, K // 128)
        gi = 0
        while gi < len(cols):
            c_start = cols[gi][0]
            ge = gi
            while ge < len(cols) and cols[ge][0] + cols[ge][1] - c_start <= group:
                ge += 1
            width = cols[ge - 1][0] + cols[ge - 1][1] - c_start
            wv, wb = load_w(w_dram[:, c_start:c_start + width], kc_n, width, small=small)
            for ci in range(gi, ge):
                c0, M = cols[ci]
                for half in halves:
                    pt, pb = pr.next()
                    for kc in range(kc_n):
                        kp = min(128, K - kc * 128)
                        cx.op("pe", lambda e, kc=kc, kp=kp: e.matmul(
                            pt[0:M, :], wv[0:kp, kc, c0 - c_start:c0 - c_start + M], rhs_fn(kc, half),
                            start=(kc == 0), stop=(kc == kc_n - 1)),
                            reads=[wb] + list(rhs_bufs(kc)), writes=[pb])
                    consume(ci, half, pt[0:M, :], pb)
            gi = ge

    def rstd_from_psum(ps_list, nfeat, out_ap_fn, out_buf):
        for half, (pt, pb) in enumerate(ps_list):
            cx.op("act", lambda e: e.activation(out=out_ap_fn(half), in_=pt[:, :], func=AF.Sqrt,
                                                scale=1.0 / nfeat, bias=eps_t[:, 0:1]),
                  reads=[pb, b_const], writes=[out_buf])
        for half in range(len(ps_list)):
            cx.op("dve", lambda e: e.reciprocal(out=out_ap_fn(half), in_=out_ap_fn(half)),
                  reads=[out_buf], writes=[out_buf])

    eps_t = cx.sb("eps", [128, 1], F32)
    cx.op("dve", lambda e: e.memset(eps_t[:], EPS), writes=[b_const])

    def norm_x(l, which, final=False):
        acc = pr.next_n(2)
        for j in range(16):
            xt, xb = next_scr()
            cx.dma("sp", xt[:, :], xs[j], reads=[b_xs[j]], writes=[xb])
            sq, sqb = next_scr()
            cx.op("act", lambda e: e.activation(out=sq[:, :], in_=xt[:, :], func=AF.Square), reads=[xb], writes=[sqb])
            for half in range(2):
                pt, pb = acc[half]
                cx.op("pe", lambda e: e.matmul(pt[:, :], ones_f[:, :], sq[:, half * 512:(half + 1) * 512],
                                               start=(j == 0), stop=(j == 15)), reads=[sqb, b_const], writes=[pb])
        rstd_from_psum(acc, D, lambda half: rs[:, half * 512:(half + 1) * 512], b_rs)

    def apply_norm(l, which):
        gs = drv[:, (0 if which == 1 else 16):]
        sh = modT[:, (0 if which == 1 else 48):]
        for j in range(16):
            xt, xb = next_scr()
            cx.dma("sp", xt[:, :], xs[j], reads=[b_xs[j]], writes=[xb])
            cx.op("dve", lambda e: e.tensor_tensor(out=xt[:, :], in0=xt[:, :], in1=rs[:, :], op=ALU.mult),
                  reads=[xb, b_rs], writes=[xb])
            cx.op("dve", lambda e: e.tensor_scalar(out=hT[:, j, :], in0=xt[:, :], scalar1=gs[:, j:j + 1],
                                                   scalar2=sh[:, j:j + 1], op0=ALU.mult, op1=ALU.add),
                  reads=[xb, b_drv, b_modT], writes=[b_hT[j]])

    def dump(ap_2d, bufs, ncols):
        cx.dma("sp", dbg_out[0:ap_2d.shape[0], 0:ncols], ap_2d, reads=bufs, writes=[], is_output=True)

    for l in range(L):
        st, sbf = next_stg()
        cx.dma("sp", st[:, :], vecs[l, 0:128, :], writes=[sbf])
        transpose_f32(lambda ps_ap, pb: cx.op("act", lambda e: e.activation(out=vecT[:, 0:128], in_=ps_ap, func=AF.Copy),
                                              reads=[pb], writes=[b_vecT]), st[:, :], 128, 128, [sbf])
        st, sbf = next_stg()
        cx.dma("sp", st[0:16, :], vecs[l, 128:144, :], writes=[sbf])
        transpose_f32(lambda ps_ap, pb: cx.op("act", lambda e: e.activation(out=vecT[:, 128:144], in_=ps_ap, func=AF.Copy),
                                              reads=[pb], writes=[b_vecT]), st[0:16, :], 16, 128, [sbf])
        mt, mb = pr.next()
        for g in range(24):
            wv, wb = load_w(w_mod[l, :, g * 512:(g + 1) * 512], 16, 512)
            for cc in range(4):
                m = g * 4 + cc
                for kc in range(16):
                    cx.op("pe", lambda e: e.matmul(mt[:, m:m + 1], wv[:, kc, cc * 128:(cc + 1) * 128],
                                                   scondT[:, kc:kc + 1], start=(kc == 0), stop=(kc == 15)),
                          reads=[wb, b_scond], writes=[mb])
        cx.op("dve", lambda e: e.tensor_tensor(out=modT[:, :], in0=mt[:, 0:96], in1=vecT[:, 0:96], op=ALU.add),
              reads=[mb, b_vecT], writes=[b_modT])
        cx.op("dve", lambda e: e.scalar_tensor_tensor(out=drv[:, 0:16], in0=modT[:, 16:32], scalar=1.0,
                                                      in1=vecT[:, 96:112], op0=ALU.add, op1=ALU.mult),
              reads=[b_modT, b_vecT], writes=[b_drv])
        cx.op("dve", lambda e: e.scalar_tensor_tensor(out=drv[:, 16:32], in0=modT[:, 64:80], scalar=1.0,
                                                      in1=vecT[:, 112:128], op0=ALU.add, op1=ALU.mult),
              reads=[b_modT, b_vecT], writes=[b_drv])
        if stop_after == "mod":
            dump(modT[:, :], [b_modT], 96)
            break
        norm_x(l, 1)
        apply_norm(l, 1)
        conv_step(l, 4)
        if stop_after == "norm1":
            for j in range(16):
                xt, xb = next_scr()
                cx.op("act", lambda e: e.activation(out=xt[:, :], in_=hT[:, j, :], func=AF.Copy),
                      reads=[b_hT[j]], writes=[xb])
                cx.dma("sp", dbg_out[:, j * T:(j + 1) * T], xt[:, :], reads=[xb], is_output=True)
            break
        hrhs = lambda kc, half: hT[:, kc, half * 512:(half + 1) * 512]
        hbufs = lambda kc: [b_hT[kc]]
        lam_init = 0.8 - 0.6 * math.exp(-0.3 * l)
        ar.reset()
        mla_oT = ar.take([128, 8, T], BF16); b_mla_o = Buf("mla_o")
        pool_oT = ar.take([128, 4, T], BF16); b_pool_o = Buf("pool_o")
        diff_oT = ar.take([128, 4, T], BF16); b_diff_o = Buf("diff_o")
        mark = ar.off

        def mm_fm(wv, wb, coff, M, K, rhs_fn, rhs_bufs, half, N=512):
            kc_n = max(1, K // 128)
            pt, pb = pr.next()
            for kc in range(kc_n):
                kp = min(128, K - kc * 128)
                cx.op("pe", lambda e: e.matmul(pt[0:M, 0:N], wv[0:kp, kc, coff:coff + M], rhs_fn(kc, half),
                                               start=(kc == 0), stop=(kc == kc_n - 1)),
                      reads=[wb] + list(rhs_bufs(kc)), writes=[pb])
            return pt, pb

        def rope_unit(raw, rawb, dst_fn, dstb):
            for half in range(2):
                sl = slice(half * 512, (half + 1) * 512)
                pt, pb = pr.next()
                cx.op("pe", lambda e: e.matmul(pt[0:64, :], perm_f[:, :], raw[0:64, sl], start=True, stop=True),
                      reads=[rawb, b_const], writes=[pb])
                t1, t1b = next_scr()
                cx.op("dve", lambda e: e.tensor_tensor(out=t1[0:64, 0:512], in0=pt[0:64, :], in1=ropes[:, sl], op=ALU.mult),
                      reads=[pb, b_const], writes=[t1b])
                cx.op("dve", lambda e: e.tensor_tensor(out=t1[0:64, 512:1024], in0=raw[0:64, sl], in1=ropec[:, sl], op=ALU.mult),
                      reads=[rawb, b_const], writes=[t1b])
                cx.op("dve", lambda e: e.tensor_tensor(out=dst_fn(sl), in0=t1[0:64, 0:512], in1=t1[0:64, 512:1024], op=ALU.add),
                      reads=[t1b], writes=[dstb])

        def out_transposes(raw, rawb, nfeat, out_dram_fn, extra=None):
            for tc in range(8):
                st, sbf = next_stg()

                def ev(ps_ap, pb, st=st, sbf=sbf, tc=tc):
                    cx.op("act", lambda e: e.activation(out=st[:, 0:nfeat], in_=ps_ap, func=AF.Copy),
                          reads=[pb], writes=[sbf])
                    if extra is not None:
                        extra(tc, st[:, 0:nfeat], sbf)
                transpose_f32(ev, raw[0:nfeat, tc * 128:(tc + 1) * 128], nfeat, 128, [rawb])
                import os
                if os.environ.get("SKIP_ODMA"):
                    continue
                cx.dma("sp", out_dram_fn(tc), st[:, 0:nfeat], reads=[sbf], is_output=True)

        def attention_core(kparts, qparts, v_fn, v_bufs, et, etb, qh, scale):
            for kc in range(10):
                pt, pb = pr.next()
                n = len(kparts)
                for i in range(n):
                    kf, kb, K = kparts[i]
                    qa_, qb_ = qparts[i]
                    cx.op("pe", lambda e: e.matmul(pt[:, :], kf(kc), qa_, start=(i == 0), stop=(i == n - 1)),
                          reads=list(kb) + list(qb_), writes=[pb])
                cx.op("act", lambda e: e.activation(out=et[:, kc, :], in_=pt[:, :], func=AF.Exp, scale=scale),
                      reads=[pb], writes=[etb])
            (o_ps, o_pb), (d_ps, d_pb) = pr.next_n(2)
            for kc in range(10):
                cx.op("pe", lambda e: e.matmul(o_ps[:, :], v_fn(kc), et[:, kc, :], start=(kc == 0), stop=(kc == 9)),
                      reads=[etb] + list(v_bufs), writes=[o_pb])
            for kc in range(10):
                cx.op("pe", lambda e: e.matmul(d_ps[:, :], ones_b[:, :], et[:, kc, :], start=(kc == 0), stop=(kc == 9)),
                      reads=[etb, b_const], writes=[d_pb])
            return o_ps, o_pb, d_ps, d_pb

        vdiff = ar.take([128, 10, 512], BF16); b_vdiff = Buf("vdiff")
        ET = [ar.take([128, 10, 512], BF16) for _ in range(2)]; b_ET = [Buf("ET0"), Buf("ET1")]
        qu = [ar.take([128, T], BF16) for _ in range(2)]; b_qu = [Buf("qu0"), Buf("qu1")]
        ku = [ar.take([128, TK], BF16) for _ in range(2)]; b_ku = [Buf("ku0"), Buf("ku1")]
        asb = [ar.take([128, 512], F32) for _ in range(3)]; b_asb = [Buf("as0"), Buf("as1"), Buf("as2")]
        ctxk = ar.take([128, 2, 512], F32); b_ctxk = Buf("ctxk")
        for s in range(2):
            cx.dma("pool", qu[s][64:68, :], qaug_d, writes=[b_qu[s]])
            cx.dma("pool", ku[s][64:68, :], kaug_d, writes=[b_ku[s]])
        cx.dma("sp", ctxk, ctx_dk[l].rearrange("(c p) f -> p c f", p=128), writes=[b_ctxk])
        cx.dma("pool", vdiff[:, 0:2, :], ctx_dv[l].rearrange("(c p) f -> p c f", p=128), writes=[b_vdiff])
        cx.dma("sp", lamt[:, 0:256], dlam[l].partition_broadcast(128), writes=[b_lam])
        for i in range(2):
            cx.op("dve", lambda e: e.scalar_tensor_tensor(out=lamt[:, 128 * i:128 * i + 64], in0=lamt[:, 128 * i:128 * i + 64],
                                                          scalar=1.0, in1=lamt[:, 128 * i + 64:128 * i + 128],
                                                          op0=ALU.mult, op1=ALU.mult, accum_out=lamt[:, 256 + i:257 + i]),
                  reads=[b_lam], writes=[b_lam])
        cx.op("act", lambda e: e.activation(out=lamt[:, 258:260], in_=lamt[:, 256:258], func=AF.Exp), reads=[b_lam], writes=[b_lam])
        cx.op("dve", lambda e: e.tensor_tensor(out=lamt[:, 260:261], in0=lamt[:, 259:260], in1=lamt[:, 258:259], op=ALU.subtract),
              reads=[b_lam], writes=[b_lam])
        cx.op("dve", lambda e: e.tensor_scalar(out=lamt[:, 260:261], in0=lamt[:, 260:261], scalar1=-lam_init, scalar2=None, op0=ALU.add),
              reads=[b_lam], writes=[b_lam])
        cx.op("dve", lambda e: e.tensor_scalar(out=drv[:, 32:33], in0=vecT[:, 138:139], scalar1=(1.0 - lam_init), scalar2=None, op0=ALU.mult),
              reads=[b_vecT], writes=[b_drv])
        if stop_after == "diffA":
            dump(lamt[:, 0:264], [b_lam], 264)
            break
        wv, wb = load_w(w_in[l, :, C_DV:C_DV + 512], 16, 512)
        for c in range(4):
            raw, rawb = next_scr()
            for half in range(2):
                pt, pb = mm_fm(wv, wb, c * 128, 128, D, hrhs, hbufs, half)
                cx.op("act", lambda e: e.activation(out=raw[:, half * 512:(half + 1) * 512], in_=pt[:, :], func=AF.Copy),
                      reads=[pb], writes=[rawb])

            def extra(tc, ps_ap, pb, c=c):
                import os
                if os.environ.get("SKIP_EXTRA"):
                    return
                cx.op("dve", lambda e: e.tensor_copy(out=vdiff[:, 2 + tc, c * 128:(c + 1) * 128], in_=ps_ap),
                      reads=[pb], writes=[b_vdiff])
            out_transposes(raw, rawb, 128, lambda tc, c=c: o_dv[l, tc * 128:(tc + 1) * 128, c * 128:(c + 1) * 128], extra)
        if stop_after == "diffB":
            dump(lamt[:, 0:264], [b_lam], 264)
            break
        wq_v, wq_b = load_w(w_in[l, :, C_DQ:C_DQ + 512], 16, 512)
        wk_v, wk_b = load_w(w_in[l, :, C_DK:C_DK + 512], 16, 512)
        dscale = 64.0 ** -0.5
        etc = 0
        for h in range(4):
            for s in range(2):
                u = 2 * h + s
                raw, rawb = next_scr()
                for half in range(2):
                    pt, pb = mm_fm(wk_v, wk_b, u * 64, 64, D, hrhs, hbufs, half)
                    cx.op("act", lambda e: e.activation(out=raw[0:64, half * 512:(half + 1) * 512], in_=pt[0:64, :], func=AF.Copy),
                          reads=[pb], writes=[rawb])
                out_transposes(raw, rawb, 64, lambda tc, u=u: o_dk[l, tc * 128:(tc + 1) * 128, u * 64:(u + 1) * 64])
                rope_unit(raw, rawb, lambda sl, s=s: ku[s][0:64, PAST + sl.start:PAST + sl.stop], b_ku[s])
                for tc in range(2):
                    transpose_f32(lambda ps_ap, pb, s=s, tc=tc: cx.op(
                        "dve", lambda e: e.tensor_copy(out=ku[s][0:64, tc * 128:(tc + 1) * 128], in_=ps_ap),
                        reads=[pb], writes=[b_ku[s]]), ctxk[:, tc, u * 64:(u + 1) * 64], 128, 64, [b_ctxk])
                raw, rawb = next_scr()
                for half in range(2):
                    pt, pb = mm_fm(wq_v, wq_b, u * 64, 64, D, hrhs, hbufs, half)
                    cx.op("act", lambda e: e.activation(out=raw[0:64, half * 512:(half + 1) * 512], in_=pt[0:64, :], func=AF.Copy),
                          reads=[pb], writes=[rawb])
                rope_unit(raw, rawb, lambda sl, s=s: qu[s][0:64, sl], b_qu[s])
            conv_step(l, 1)
            for qh in range(2):
                qs = slice(qh * 512, (qh + 1) * 512)
                for s in range(2):
                    et, etb = ET[etc % 2], b_ET[etc % 2]
                    etc += 1
                    o_ps, o_pb, d_ps, d_pb = attention_core(
                        [(lambda kc, s=s: ku[s][0:68, kc * 128:(kc + 1) * 128], [b_ku[s]], 68)],
                        [(qu[s][0:68, qs], [b_qu[s]])],
                        lambda kc: vdiff[:, kc, h * 128:(h + 1) * 128], [b_vdiff], et, etb, qh, dscale)
                    cx.op("dve", lambda e: e.reciprocal(out=asb[2][:, :], in_=d_ps[:, :]), reads=[d_pb], writes=[b_asb[2]])
                    cx.op("dve", lambda e: e.tensor_tensor(out=asb[s][:, :], in0=o_ps[:, :], in1=asb[2][:, :], op=ALU.mult),
                          reads=[o_pb, b_asb[2]], writes=[b_asb[s]])
                cx.op("dve", lambda e: e.scalar_tensor_tensor(out=asb[0][:, :], in0=asb[1][:, :], scalar=lamt[:, 260:261],
                                                              in1=asb[0][:, :], op0=ALU.mult, op1=ALU.add),
                      reads=[b_asb[0], b_asb[1], b_lam], writes=[b_asb[0]])
                cx.op("act", lambda e: e.activation(out=asb[1][:, :], in_=asb[0][:, :], func=AF.Square),
                      reads=[b_asb[0]], writes=[b_asb[1]])
                pt, pb = pr.next()
                cx.op("pe", lambda e: e.matmul(pt[:, :], ones_f[:, :], asb[1][:, :], start=True, stop=True),
                      reads=[b_asb[1], b_const], writes=[pb])
                rstd_from_psum([(pt, pb)], 128, lambda half: asb[2][:, :], b_asb[2])
                cx.op("dve", lambda e: e.tensor_tensor(out=asb[0][:, :], in0=asb[0][:, :], in1=asb[2][:, :], op=ALU.mult),
                      reads=[b_asb[0], b_asb[2]], writes=[b_asb[0]])
                cx.op("dve", lambda e: e.tensor_scalar(out=diff_oT[:, h, qs], in0=asb[0][:, :], scalar1=drv[:, 32:33], scalar2=None, op0=ALU.mult),
                      reads=[b_asb[0], b_drv], writes=[b_diff_o])
        if stop_after == "diff":
            for j in range(4):
                xt, xb = next_scr()
                cx.op("act", lambda e: e.activation(out=xt[:, :], in_=diff_oT[:, j, :], func=AF.Copy), reads=[b_diff_o], writes=[xb])
                cx.dma("sp", dbg_out[:, j * T:(j + 1) * T], xt[:, :], reads=[xb], is_output=True)
            break
        cx.fence()
        ar.off = mark
        zp = ar.take([128, 4, 272], F32); b_zp = Buf("zp")
        pa = ar.take([128, 4, 272], F32); b_pa = Buf("pa")
        pb2 = ar.take([128, 4, 272], F32); b_pb2 = Buf("pb2")
        dTt = ar.take([128, T], BF16); b_dT = Buf("dT")
        wpl = ar.take([128, 4, 128], BF16); b_wpl = Buf("wpl")
        cx.dma("pool", wpl, w_pool[l].rearrange("g c d -> c g d"), writes=[b_wpl])
        cx.op("dve", lambda e: e.memset(zp[:, :, :], 0.0), writes=[b_zp])
        wv, wb = load_w(w_in[l, :, C_PZ:C_PZ + 512], 16, 512)
        for g in range(4):
            for half in range(2):
                pt, pb = mm_fm(wv, wb, g * 128, 128, D, hrhs, hbufs, half)
                cx.op("act", lambda e: e.activation(out=zp[:, 2 * half:2 * half + 2, 8:264],
                                                    in_=pt[:, :].rearrange("p (a b) -> p a b", a=2), func=AF.Copy),
                      reads=[pb], writes=[b_zp])
            cx.op("dve", lambda e: e.tensor_scalar(out=zp[:, 1:4, 0:8], in0=zp[:, 0:3, 256:264], scalar1=flag[:, 0:1], scalar2=None, op0=ALU.mult),
                  reads=[b_zp, b_const], writes=[b_zp])
            cx.op("dve", lambda e: e.tensor_scalar(out=zp[:, 0:3, 264:272], in0=zp[:, 1:4, 8:16], scalar1=flag[:, 0:1], scalar2=None, op0=ALU.mult),
                  reads=[b_zp, b_const], writes=[b_zp])
            cx.op("dve", lambda e: e.tensor_tensor(out=pa[:, :, 1:272], in0=zp[:, :, 1:272], in1=zp[:, :, 0:271], op=ALU.add),
                  reads=[b_zp], writes=[b_pa])
            win, winb = pa, b_pa
            if g >= 1:
                cx.op("dve", lambda e: e.tensor_tensor(out=pb2[:, :, 2:271], in0=pa[:, :, 3:272], in1=pa[:, :, 1:270], op=ALU.add),
                      reads=[b_pa], writes=[b_pb2])
                win, winb = pb2, b_pb2
            if g >= 2:
                cx.op("dve", lambda e: e.tensor_tensor(out=pa[:, :, 4:269], in0=pb2[:, :, 6:271], in1=pb2[:, :, 2:267], op=ALU.add),
                      reads=[b_pb2], writes=[b_pa])
                win, winb = pa, b_pa
            if g >= 3:
                cx.op("dve", lambda e: e.tensor_tensor(out=pb2[:, :, 8:264], in0=pa[:, :, 12:268], in1=pa[:, :, 4:260], op=ALU.add),
                      reads=[b_pa], writes=[b_pb2])
                win, winb = pb2, b_pb2
            for half in range(2):
                pt, pb = pr.next()
                cx.op("pe", lambda e: e.matmul(pt[:, :], sel4[:, g * 128:(g + 1) * 128], invcnt[:, half * 512:(half + 1) * 512],
                                               start=True, stop=True), reads=[b_const], writes=[pb])
                other = pa if win is pb2 else pb2
                otherb = b_pa if win is pb2 else b_pb2
                cx.op("dve", lambda e: e.tensor_tensor(out=other[:, 2 * half:2 * half + 2, 8:264], in0=win[:, 2 * half:2 * half + 2, 8:264],
                                                       in1=pt[:, :].rearrange("p (a b) -> p a b", a=2), op=ALU.mult),
                      reads=[winb, pb], writes=[otherb])
                cx.op("dve", lambda e: e.tensor_tensor(out=dTt[:, half * 512:(half + 1) * 512].rearrange("p (a b) -> p a b", a=2),
                                                       in0=other[:, 2 * half:2 * half + 2, 8:264], in1=zp[:, 2 * half:2 * half + 2, 8:264], op=ALU.subtract),
                      reads=[otherb, b_zp], writes=[b_dT])
            for half in range(2):
                pt, pb = pr.next()
                cx.op("pe", lambda e: e.matmul(pt[:, :], wpl[:, g, :], dTt[:, half * 512:(half + 1) * 512], start=True, stop=True),
                      reads=[b_wpl, b_dT], writes=[pb])
                cx.op("dve", lambda e: e.tensor_scalar(out=pool_oT[:, g, half * 512:(half + 1) * 512], in0=pt[:, :],
                                                       scalar1=vecT[:, 134 + g:135 + g], scalar2=None, op0=ALU.mult),
                      reads=[pb, b_vecT], writes=[b_pool_o])
        if stop_after == "pool":
            for j in range(4):
                xt, xb = next_scr()
                cx.op("act", lambda e: e.activation(out=xt[:, :], in_=pool_oT[:, j, :], func=AF.Copy), reads=[b_pool_o], writes=[xb])
                cx.dma("sp", dbg_out[:, j * T:(j + 1) * T], xt[:, :], reads=[xb], is_output=True)
            break
        cx.fence()
        ar.off = mark

        ckvT = ar.take([128, 2, TK], BF16); b_ckvT = Buf("ckvT")
        krT = ar.take([128, TK], BF16); b_krT = Buf("krT")
        qanT = ar.take([128, 4, T], BF16); b_qan = Buf("qanT")
        ET = [ar.take([128, 10, 512], BF16) for _ in range(2)]; b_ET = [Buf("ET0"), Buf("ET1")]
        qn = ar.take([128, T], BF16); b_qn = Buf("qn")
        qr = ar.take([128, T], BF16); b_qr = Buf("qr")
        kn = ar.take([128, TK], BF16); b_kn = Buf("kn")
        vm = ar.take([128, 10, 128], BF16); b_vm = Buf("vm")
        cst = ar.take([128, 2, 320], F32); b_cst = Buf("cst")
        rsm = ar.take([128, 512], F32); b_rsm = Buf("rsm")
        cx.dma("pool", qr[64:68, :], qaug_d, writes=[b_qr])
        cx.dma("pool", krT[64:68, :], kaug_d, writes=[b_krT])
        cx.dma("sp", cst[:, :, 0:256], ctx_ckv[l].rearrange("(c p) f -> p c f", p=128), writes=[b_cst])
        cx.dma("sp", cst[:, :, 256:320], ctx_krope[l].rearrange("(c p) f -> p c f", p=128), writes=[b_cst])
        for tc in range(2):
            for c in range(2):
                transpose_f32(lambda ps_ap, pb, tc=tc, c=c: cx.op(
                    "act", lambda e: e.activation(out=ckvT[:, c, tc * 128:(tc + 1) * 128], in_=ps_ap, func=AF.Copy),
                    reads=[pb], writes=[b_ckvT]), cst[:, tc, c * 128:(c + 1) * 128], 128, 128, [b_cst])
            transpose_f32(lambda ps_ap, pb, tc=tc: cx.op(
                "act", lambda e: e.activation(out=krT[0:64, tc * 128:(tc + 1) * 128], in_=ps_ap, func=AF.Copy),
                reads=[pb], writes=[b_krT]), cst[:, tc, 256:320], 128, 64, [b_cst])
        wv, wb = load_w(w_in[l, :, 0:512], 16, 512)
        wv2, wb2 = load_w(w_in[l, :, 512:832], 16, 320)
        for (wv_, wb_, nch, gcol, nfeat, is_q) in ((wv, wb, 4, 128, 512, True), (wv2, wb2, 2, 132, 256, False)):
            acc = pr.hold(2)
            for c in range(nch):
                for half in range(2):
                    pt, pb = mm_fm(wv_, wb_, c * 128, 128, D, hrhs, hbufs, half)
                    sq, sqb = next_scr()
                    cx.op("act", lambda e: e.activation(out=sq[:, 0:512], in_=pt[:, :], func=AF.Square), reads=[pb], writes=[sqb])
                    cx.op("pe", lambda e: e.matmul(acc[half][0][:, :], ones_f[:, :], sq[:, 0:512], start=(c == 0), stop=(c == nch - 1)),
                          reads=[sqb, b_const], writes=[acc[half][1]])
            rstd_from_psum(acc, nfeat, lambda half: rs[:, half * 512:(half + 1) * 512], b_rs)
            pr.release(acc)
            for c in range(nch):
                raw, rawb = next_scr()
                for half in range(2):
                    sl = slice(half * 512, (half + 1) * 512)
                    pt, pb = mm_fm(wv_, wb_, c * 128, 128, D, hrhs, hbufs, half)
                    cx.op("dve", lambda e: e.tensor_tensor(out=raw[:, sl], in0=pt[:, :], in1=rs[:, sl], op=ALU.mult),
                          reads=[pb, b_rs], writes=[rawb])
                    if is_q:
                        cx.op("dve", lambda e: e.tensor_scalar(out=qanT[:, c, sl], in0=raw[:, sl], scalar1=vecT[:, gcol + c:gcol + c + 1],
                                                               scalar2=None, op0=ALU.mult), reads=[rawb, b_vecT], writes=[b_qan])
                    else:
                        cx.op("dve", lambda e: e.tensor_scalar(out=raw[:, sl], in0=raw[:, sl], scalar1=vecT[:, gcol + c:gcol + c + 1],
                                                               scalar2=None, op0=ALU.mult), reads=[rawb, b_vecT], writes=[rawb])
                        cx.op("act", lambda e: e.activation(out=ckvT[:, c, PAST + half * 512:PAST + (half + 1) * 512], in_=raw[:, sl], func=AF.Copy),
                              reads=[rawb], writes=[b_ckvT])
                if not is_q:
                    out_transposes(raw, rawb, 128, lambda tc, c=c: o_ckv[l, tc * 128:(tc + 1) * 128, c * 128:(c + 1) * 128])
        raw, rawb = next_scr()
        for half in range(2):
            pt, pb = mm_fm(wv2, wb2, 256, 64, D, hrhs, hbufs, half)
            cx.op("act", lambda e: e.activation(out=raw[0:64, half * 512:(half + 1) * 512], in_=pt[0:64, :], func=AF.Copy),
                  reads=[pb], writes=[rawb])
        out_transposes(raw, rawb, 64, lambda tc: o_krope[l, tc * 128:(tc + 1) * 128, :])
        rope_unit(raw, rawb, lambda sl: krT[0:64, PAST + sl.start:PAST + sl.stop], b_krT)
        wqb_v, wqb_b = load_w(w_qb[l], 4, 1536)
        wkv_v, wkv_b = load_w(w_kvb[l], 2, 2048, small=True)
        qan_rhs = lambda kc, half: qanT[:, kc, half * 512:(half + 1) * 512]
        qan_bufs = lambda kc: [b_qan]
        mscale = 192.0 ** -0.5
        etc = 0
        for h in range(8):
            for half in range(2):
                pt, pb = mm_fm(wqb_v, wqb_b, h * 192, 128, 512, qan_rhs, qan_bufs, half)
                cx.op("act", lambda e: e.activation(out=qn[:, half * 512:(half + 1) * 512], in_=pt[:, :], func=AF.Copy),
                      reads=[pb], writes=[b_qn])
            raw, rawb = next_scr()
            for half in range(2):
                pt, pb = mm_fm(wqb_v, wqb_b, h * 192 + 128, 64, 512, qan_rhs, qan_bufs, half)
                cx.op("act", lambda e: e.activation(out=raw[0:64, half * 512:(half + 1) * 512], in_=pt[0:64, :], func=AF.Copy),
                      reads=[pb], writes=[rawb])
            rope_unit(raw, rawb, lambda sl: qr[0:64, sl], b_qr)
            for (c0, n) in ((0, 512), (512, 512), (1024, 256)):
                pt, pb = pr.next()
                for kc in range(2):
                    cx.op("pe", lambda e: e.matmul(pt[:, 0:n], wkv_v[:, kc, h * 256:h * 256 + 128], ckvT[:, kc, c0:c0 + n],
                                                   start=(kc == 0), stop=(kc == 1)), reads=[wkv_b, b_ckvT], writes=[pb])
                cx.op("act", lambda e: e.activation(out=kn[:, c0:c0 + n], in_=pt[:, 0:n], func=AF.Copy), reads=[pb], writes=[b_kn])
            for g4 in range(3):
                pt, pb = pr.next()
                nb = 4 if g4 < 2 else 2
                for bi in range(nb):
                    tkc = g4 * 4 + bi
                    for kc in range(2):
                        cx.op("pe", lambda e: e.matmul(pt[:, bi * 128:(bi + 1) * 128], ckvT[:, kc, tkc * 128:(tkc + 1) * 128],
                                                       wkv_v[:, kc, h * 256 + 128:h * 256 + 256], start=(kc == 0), stop=(kc == 1)),
                              reads=[wkv_b, b_ckvT], writes=[pb])
                cx.op("act", lambda e: e.activation(out=vm[:, g4 * 4:g4 * 4 + nb, :], in_=pt[:, 0:nb * 128].rearrange("p (a b) -> p a b", a=nb),
                                                    func=AF.Copy), reads=[pb], writes=[b_vm])
            conv_step(l, 1)
            for qh in range(2):
                qs = slice(qh * 512, (qh + 1) * 512)
                et, etb = ET[etc % 2], b_ET[etc % 2]
                etc += 1
                o_ps, o_pb, d_ps, d_pb = attention_core(
                    [(lambda kc: kn[:, kc * 128:(kc + 1) * 128], [b_kn], 128),
                     (lambda kc: krT[0:68, kc * 128:(kc + 1) * 128], [b_krT], 68)],
                    [(qn[:, qs], [b_qn]), (qr[0:68, qs], [b_qr])],
                    lambda kc: vm[:, kc, :], [b_vm], et, etb, qh, mscale)
                cx.op("dve", lambda e: e.reciprocal(out=rsm[:, :], in_=d_ps[:, :]), reads=[d_pb], writes=[b_rsm])
                cx.op("dve", lambda e: e.tensor_tensor(out=mla_oT[:, h, qs], in0=o_ps[:, :], in1=rsm[:, :], op=ALU.mult),
                      reads=[o_pb, b_rsm], writes=[b_mla_o])
        if stop_after == "mla":
            for j in range(8):
                xt, xb = next_scr()
                cx.op("act", lambda e: e.activation(out=xt[:, :], in_=mla_oT[:, j, :], func=AF.Copy), reads=[b_mla_o], writes=[xb])
                cx.dma("sp", dbg_out[:, j * T:(j + 1) * T], xt[:, :], reads=[xb], is_output=True)
            break
        cx.fence()
        ar.off = mark
        mergedT = ar.take([128, 16, T], BF16); b_merged = [Buf("mg%d" % j) for j in range(16)]
        accg = ar.take([128, 8, 512], F32); b_accg = [Buf("accg%d" % i) for i in range(8)]
        sgt = [ar.take([128, 512], F32) for _ in range(2)]; b_sgt = [Buf("sg0"), Buf("sg1")]
        branches = ((w_br_mla[l], 1024, mla_oT, b_mla_o), (w_br_pool[l], 512, pool_oT, b_pool_o),
                    (w_br_diff[l], 512, diff_oT, b_diff_o))
        sgc = 0
        for jg in range(4):
            for b in range(3):
                gv, gb = load_w(w_in[l, :, C_GL + b * 2048 + jg * 512:C_GL + b * 2048 + (jg + 1) * 512], 16, 512)
                wbr, Kb, brT, brb = branches[b]
                bv, bb = load_w(wbr[:, jg * 512:(jg + 1) * 512], Kb // 128, 512, small=True)
                for jj in range(4):
                    j = jg * 4 + jj
                    for half in range(2):
                        sl = slice(half * 512, (half + 1) * 512)
                        ai = jj * 2 + half
                        pg, pgb = mm_fm(gv, gb, jj * 128, 128, D, hrhs, hbufs, half)
                        s_t, s_b = sgt[sgc % 2], b_sgt[sgc % 2]
                        sgc += 1
                        cx.op("act", lambda e: e.activation(out=s_t[:, :], in_=pg[:, :], func=AF.Sigmoid), reads=[pgb], writes=[s_b])
                        pbr, pbrb = mm_fm(bv, bb, jj * 128, 128, Kb, lambda kc, hf: brT[:, kc, hf * 512:(hf + 1) * 512],
                                          lambda kc: [brb], half)
                        if b == 0:
                            cx.op("dve", lambda e: e.tensor_tensor(out=accg[:, ai, :], in0=s_t[:, :], in1=pbr[:, :], op=ALU.mult),
                                  reads=[s_b, pbrb], writes=[b_accg[ai]])
                        else:
                            cx.op("dve", lambda e: e.tensor_tensor(out=s_t[:, :], in0=s_t[:, :], in1=pbr[:, :], op=ALU.mult),
                                  reads=[s_b, pbrb], writes=[s_b])
                            if b == 1:
                                cx.op("dve", lambda e: e.tensor_tensor(out=accg[:, ai, :], in0=accg[:, ai, :], in1=s_t[:, :], op=ALU.add),
                                      reads=[s_b, b_accg[ai]], writes=[b_accg[ai]])
                            else:
                                cx.op("dve", lambda e: e.tensor_tensor(out=mergedT[:, j, sl], in0=accg[:, ai, :], in1=s_t[:, :], op=ALU.add),
                                      reads=[s_b, b_accg[ai]], writes=[b_merged[j]])
        if stop_after == "merge":
            for j in range(16):
                xt, xb = next_scr()
                cx.op("act", lambda e: e.activation(out=xt[:, :], in_=mergedT[:, j, :], func=AF.Copy), reads=[b_merged[j]], writes=[xb])
                cx.dma("sp", dbg_out[:, j * T:(j + 1) * T], xt[:, :], reads=[xb], is_output=True)
            break
        for jg in range(4):
            wv, wb = load_w(w_out[l, :, jg * 512:(jg + 1) * 512], 16, 512)
            for jj in range(4):
                j = jg * 4 + jj
                xt, xb = next_scr()
                cx.dma("sp", xt[:, :], xs[j], reads=[b_xs[j]], writes=[xb])
                for half in range(2):
                    sl = slice(half * 512, (half + 1) * 512)
                    pt, pb = mm_fm(wv, wb, jj * 128, 128, D, lambda kc, hf: mergedT[:, kc, hf * 512:(hf + 1) * 512],
                                   lambda kc: [b_merged[kc]], half)
                    cx.op("dve", lambda e: e.scalar_tensor_tensor(out=xt[:, sl], in0=pt[:, :], scalar=modT[:, 32 + j:33 + j],
                                                                  in1=xt[:, sl], op0=ALU.mult, op1=ALU.add),
                          reads=[pb, b_modT, xb], writes=[xb])
                cx.dma("sp", xs[j], xt[:, :], reads=[xb], writes=[b_xs[j]])
        norm_x(l, 2)
        apply_norm(l, 2)
        cx.fence()
        ar.reset()
        if stop_after == "attn":
            for j in range(16):
                xt, xb = next_scr()
                cx.dma("sp", xt[:, :], xs[j], reads=[b_xs[j]], writes=[xb])
                cx.dma("sp", dbg_out[:, j * T:(j + 1) * T], xt[:, :], reads=[xb], is_output=True)
            break
        skT = ar.take([128, 2, 128], BF16); b_skT = Buf("skT")
        skst = ar.take([128, 2, 128], F32); b_skst = Buf("skst")
        qT = ar.take([128, 16, 512], BF16); b_qT = Buf("qT")
        s1 = ar.take([128, 16, 128], F32); b_s1 = Buf("s1")
        s2 = ar.take([128, 16, 128], F32); b_s2 = Buf("s2")
        vtop = ar.take([128, 16, 16], F32); b_vtop = Buf("vtop")
        ix = ar.take([128, 16, 16], U32); b_ix = Buf("ix")
        ixf = ar.take([128, 16, 16], F32); b_ixf = Buf("ixf")
        best = ar.take([128, 8, 16], F32); b_best = Buf("best")
        sel = ar.take([128, 8, 16], U32); b_sel = Buf("sel")
        ru = ar.take([128, 2, 8, 16], U32); b_ru = Buf("ru")
        rf = ar.take([128, 2, 8, 16], F32); b_rf = Buf("rf")
        ab = ar.take([128, 2, 8, 16], F32); b_ab = Buf("ab")
        idxf = ar.take([128, 128], F32); b_idxf = Buf("idxf")
        idxi = ar.take([128, 128], I32); b_idxi = Buf("idxi")
        gw = ar.take([128, 8, 16], F32); b_gw = Buf("gw")
        sm = ar.take([128, 32], F32); b_sm = Buf("sm")
        h2tok = ar.take([128, 2048], BF16); b_h2tok = Buf("h2tok")
        junk = ar.take([128, 2048], BF16); b_junk = Buf("junk")
        actv = ar.take([128, 128], F32); b_actv = Buf("actv")
        wgt = ar.take([128, 128], F32); b_wgt = Buf("wgt")
        NG = 4
        gbuf = [ar.take([128, 2048], BF16) for _ in range(NG)]; b_gbuf = [Buf("g%d" % i) for i in range(NG)]
        diag = [ar.take([128, 128], BF16) for _ in range(2)]; b_diag = [Buf("dg0"), Buf("dg1")]
        otok = [ar.take([128, 512], F32) for _ in range(2)]; b_otok = [Buf("ot0"), Buf("ot1")]
        xupd = [ar.take([128, 4, 128], F32) for _ in range(2)]; b_xupd = [Buf("xu0"), Buf("xu1")]
        conv_step(l, 16)
        cx.dma("sp", skst, subkeys[l].rearrange("p k c -> k p c"), writes=[b_skst])
        for p in range(2):
            transpose_f32(lambda ps_ap, pb, p=p: cx.op("act", lambda e: e.activation(out=skT[:, p, :], in_=ps_ap, func=AF.Copy),
                                                       reads=[pb], writes=[b_skT]), skst[:, p, :], 128, 128, [b_skst])
        gcnt = [0]
        idx2 = [idxi, ar.take([128, 128], I32)]; b_idx2 = [b_idxi, Buf("idxi1")]

        def peer_qproj(th):
            for jg in range(4):
                wv, wb = load_w(w_peer_q[l, :, jg * 512:(jg + 1) * 512], 16, 512)
                for jj in range(4):
                    pt, pb = mm_fm(wv, wb, jj * 128, 128, D, hrhs, hbufs, th)
                    cx.op("act", lambda e: e.activation(out=qT[:, jg * 4 + jj, :], in_=pt[:, :], func=AF.Copy), reads=[pb], writes=[b_qT])

        def peer_A1(tb):
            tb4 = tb % 4
            tsl = slice(tb4 * 128, (tb4 + 1) * 128)
            idxo, b_idxo = idx2[tb % 2], b_idx2[tb % 2]
            banks = pr.hold(4)
            for hp in range(16):
                bt, bbf = banks[hp // 4]
                cx.op("pe", lambda e: e.matmul(bt[:, (hp % 4) * 128:(hp % 4 + 1) * 128], qT[:, hp, tsl], skT[:, hp % 2, :],
                                               start=True, stop=True), reads=[b_qT, b_skT], writes=[bbf])
            for q4 in range(4):
                bt, bbf = banks[q4]
                cx.op("act", lambda e: e.activation(out=s1[:, q4 * 4:(q4 + 1) * 4, :], in_=bt[:, :].rearrange("p (a b) -> p a b", a=4),
                                                    func=AF.Copy), reads=[bbf], writes=[b_s1])
            pr.release(banks)
            for hp in range(16):
                cx.op("dve", lambda e: e.max(out=vtop[:, hp, 0:8], in_=s1[:, hp, :]), reads=[b_s1], writes=[b_vtop])
                cx.op("dve", lambda e: e.max_index(out=ix[:, hp, 0:8], in_max=vtop[:, hp, 0:8], in_values=s1[:, hp, :]),
                      reads=[b_s1, b_vtop], writes=[b_ix])
                cx.op("dve", lambda e: e.match_replace(out=s2[:, hp, :], in_to_replace=vtop[:, hp, 0:8], in_values=s1[:, hp, :],
                                                       imm_value=-1e30), reads=[b_s1, b_vtop], writes=[b_s2])
                cx.op("dve", lambda e: e.max(out=vtop[:, hp, 8:16], in_=s2[:, hp, :]), reads=[b_s2], writes=[b_vtop])
                cx.op("dve", lambda e: e.max_index(out=ix[:, hp, 8:16], in_max=vtop[:, hp, 8:16], in_values=s2[:, hp, :]),
                      reads=[b_s2, b_vtop], writes=[b_ix])
            cand4 = s1.rearrange("p (h a) (b c) -> p h (a b) c", a=2, b=8)
            cand2_4 = s2.rearrange("p (h a) (b c) -> p h (a b) c", a=2, b=8)
            candf = s1.rearrange("p (h a) k -> p h (a k)", a=2)
            cand2f = s2.rearrange("p (h a) k -> p h (a k)", a=2)
            vt4 = vtop.rearrange("p (h a) k -> p h a k", a=2)
            cx.op("dve", lambda e: e.tensor_tensor(out=cand4, in0=vt4[:, :, 0, :].unsqueeze(3).to_broadcast([128, 8, 16, 16]),
                                                   in1=vt4[:, :, 1, :].unsqueeze(2).to_broadcast([128, 8, 16, 16]), op=ALU.add),
                  reads=[b_vtop], writes=[b_s1])
            for h in range(8):
                cx.op("dve", lambda e: e.max(out=best[:, h, 0:8], in_=candf[:, h, :]), reads=[b_s1], writes=[b_best])
                cx.op("dve", lambda e: e.max_index(out=sel[:, h, 0:8], in_max=best[:, h, 0:8], in_values=candf[:, h, :]),
                      reads=[b_s1, b_best], writes=[b_sel])
                cx.op("dve", lambda e: e.match_replace(out=cand2f[:, h, :], in_to_replace=best[:, h, 0:8], in_values=candf[:, h, :],
                                                       imm_value=-1e30), reads=[b_s1, b_best], writes=[b_s2])
                cx.op("dve", lambda e: e.max(out=best[:, h, 8:16], in_=cand2f[:, h, :]), reads=[b_s2], writes=[b_best])
                cx.op("dve", lambda e: e.max_index(out=sel[:, h, 8:16], in_max=best[:, h, 8:16], in_values=cand2f[:, h, :]),
                      reads=[b_s2, b_best], writes=[b_sel])
            cx.op("dve", lambda e: e.tensor_single_scalar(out=ru[:, 0], in_=sel, scalar=4, op=ALU.logical_shift_right),
                  reads=[b_sel], writes=[b_ru])
            cx.op("dve", lambda e: e.tensor_single_scalar(out=ru[:, 1], in_=sel, scalar=15, op=ALU.bitwise_and),
                  reads=[b_sel], writes=[b_ru])
            cx.op("dve", lambda e: e.tensor_copy(out=rf, in_=ru), reads=[b_ru], writes=[b_rf])
            cx.op("dve", lambda e: e.tensor_copy(out=ixf, in_=ix), reads=[b_ix], writes=[b_ixf])
            ixf4 = ixf.rearrange("p (h a) k -> p h a k", a=2)
            for a in range(2):
                cx.op("dve", lambda e: e.tensor_tensor(out=cand4, in0=iota16[:, :].unsqueeze(1).unsqueeze(1).to_broadcast([128, 8, 16, 16]),
                                                       in1=rf[:, a].unsqueeze(3).to_broadcast([128, 8, 16, 16]), op=ALU.is_equal),
                      reads=[b_rf, b_const], writes=[b_s1])
                cx.op("dve", lambda e: e.tensor_tensor(out=cand2_4, in0=cand4,
                                                       in1=ixf4[:, :, a, :].unsqueeze(2).to_broadcast([128, 8, 16, 16]), op=ALU.mult),
                      reads=[b_s1, b_ixf], writes=[b_s2])
                cx.op("dve", lambda e: e.tensor_reduce(out=ab[:, a], in_=cand2_4, axis=AX.X, op=ALU.add),
                      reads=[b_s2], writes=[b_ab])
            cx.op("dve", lambda e: e.scalar_tensor_tensor(out=idxf.rearrange("p (h k) -> p h k", h=8), in0=ab[:, 0], scalar=128.0,
                                                          in1=ab[:, 1], op0=ALU.mult, op1=ALU.add), reads=[b_ab], writes=[b_idxf])
            cx.op("dve", lambda e: e.tensor_scalar(out=idxf, in0=idxf, scalar1=float(l * 16384), scalar2=None, op0=ALU.add),
                  reads=[b_idxf], writes=[b_idxf])
            cx.op("dve", lambda e: e.tensor_copy(out=idxo, in_=idxf), reads=[b_idxf], writes=[b_idxo])

        def peer_A2(tb):
            cx.op("dve", lambda e: e.tensor_scalar(out=sm[:, 0:8], in0=best[:, :, 0], scalar1=-1.0, scalar2=None, op0=ALU.mult),
                  reads=[b_best], writes=[b_sm])
            for h in range(8):
                cx.op("act", lambda e: e.activation(out=gw[:, h, :], in_=best[:, h, :], func=AF.Exp, bias=sm[:, h:h + 1], scale=1.0,
                                                    accum_out=sm[:, 8 + h:9 + h]), reads=[b_best, b_sm], writes=[b_gw, b_sm])
            cx.op("dve", lambda e: e.reciprocal(out=sm[:, 16:24], in_=sm[:, 8:16]), reads=[b_sm], writes=[b_sm])
            cx.op("dve", lambda e: e.tensor_tensor(out=gw, in0=gw, in1=sm[:, 16:24].unsqueeze(2).to_broadcast([128, 8, 16]), op=ALU.mult),
                  reads=[b_gw, b_sm], writes=[b_gw])
            for j8 in range(2):
                pt, pb = pr.next()
                ptb = pt[:, :].bitcast(BF16)
                for jj in range(8):
                    j = j8 * 8 + jj
                    cx.op("pe", lambda e: e.transpose(ptb[:, jj * 128:(jj + 1) * 128], hT[:, j, tb * 128:(tb + 1) * 128], ident_b[:, :]),
                          reads=[b_hT[j], b_const], writes=[pb])
                cx.op("act", lambda e: e.activation(out=h2tok[:, j8 * 1024:(j8 + 1) * 1024], in_=ptb, func=AF.Copy),
                      reads=[pb], writes=[b_h2tok])

        def peer_C(tb):
            idxo, b_idxo = idx2[tb % 2], b_idx2[tb % 2]
            for kk in range(128):
                gi = gcnt[0] % NG
                gcnt[0] += 1
                cx.dma("pool", gbuf[gi][:, :], pu16, indirect=idxo[:, kk:kk + 1], reads=[b_idxo] + b_tab[l][0:8], writes=[b_gbuf[gi]])
                cx.op("dve", lambda e: e.scalar_tensor_tensor(out=junk[:, :], in0=gbuf[gi][:, :], scalar=1.0, in1=h2tok[:, :],
                                                              op0=ALU.mult, op1=ALU.mult, accum_out=actv[:, kk:kk + 1]),
                      reads=[b_gbuf[gi], b_h2tok], writes=[b_junk, b_actv])
            cx.op("act", lambda e: e.activation(out=wgt[:, :], in_=actv[:, :], func=AF.Gelu), reads=[b_actv], writes=[b_wgt])
            cx.op("dve", lambda e: e.tensor_tensor(out=wgt[:, :], in0=wgt[:, :], in1=gw.rearrange("p h k -> p (h k)"), op=ALU.mult),
                  reads=[b_wgt, b_gw], writes=[b_wgt])

        def peer_B(tb):
            idxo, b_idxo = idx2[tb % 2], b_idx2[tb % 2]
            accb = pr.hold(4)
            for kk in range(128):
                gi = gcnt[0] % NG
                gcnt[0] += 1
                cx.dma("pool", gbuf[gi][:, :], pv16, indirect=idxo[:, kk:kk + 1], reads=[b_idxo] + b_tab[l][8:16], writes=[b_gbuf[gi]])
                dg, dgb = diag[kk % 2], b_diag[kk % 2]
                cx.op("act", lambda e: e.activation(out=dg[:, :], in_=ident_b[:, :], func=AF.Copy, scale=wgt[:, kk:kk + 1]),
                      reads=[b_wgt, b_const], writes=[dgb])
                for dq in range(4):
                    cx.op("pe", lambda e: e.matmul(accb[dq][0][:, :], dg[:, :], gbuf[gi][:, dq * 512:(dq + 1) * 512],
                                                   start=(kk == 0), stop=(kk == 127)), reads=[dgb, b_gbuf[gi]], writes=[accb[dq][1]])
            for dq in range(4):
                ot, otb = otok[dq % 2], b_otok[dq % 2]
                xu, xub = xupd[dq % 2], b_xupd[dq % 2]
                cx.op("act", lambda e: e.activation(out=ot[:, :], in_=accb[dq][0][:, :], func=AF.Copy), reads=[accb[dq][1]], writes=[otb])
                cx.dma("sp", xu, xs[dq * 4:(dq + 1) * 4, :, tb * 128:(tb + 1) * 128].rearrange("j p t -> p j t"),
                       reads=[b_xs[dq * 4 + i] for i in range(4)], writes=[xub])
                for jj in range(4):
                    j = dq * 4 + jj
                    transpose_f32(lambda ps_ap, pb, jj=jj, j=j: cx.op(
                        "dve", lambda e: e.scalar_tensor_tensor(out=xu[:, jj, :], in0=ps_ap, scalar=modT[:, 80 + j:81 + j],
                                                                in1=xu[:, jj, :], op0=ALU.mult, op1=ALU.add),
                        reads=[pb, b_modT, xub], writes=[xub]), ot[:, jj * 128:(jj + 1) * 128], 128, 128, [otb])
                cx.dma("sp", xs[dq * 4:(dq + 1) * 4, :, tb * 128:(tb + 1) * 128].rearrange("j p t -> p j t"), xu,
                       reads=[xub], writes=[b_xs[dq * 4 + i] for i in range(4)])
            pr.release(accb)

        peer_qproj(0)
        peer_A1(0)
        peer_A2(0)
        peer_C(0)
        for tb in range(8):
            if tb + 1 < 8:
                if tb + 1 == 4:
                    peer_qproj(1)
                peer_A1(tb + 1)
            peer_B(tb)
            if tb + 1 < 8:
                peer_A2(tb + 1)
                peer_C(tb + 1)
        cx.fence()
        if stop_after == "layer":
            for j in range(16):
                xt, xb = next_scr()
                cx.dma("sp", xt[:, :], xs[j], reads=[b_xs[j]], writes=[xb])
                cx.dma("sp", dbg_out[:, j * T:(j + 1) * T], xt[:, :], reads=[xb], is_output=True)
            break
    else:
        norm_x(L, 1)
        for j in range(16):
            xt, xb = next_scr()
            cx.dma("sp", xt[:, :], xs[j], reads=[b_xs[j]], writes=[xb])
            cx.op("dve", lambda e: e.tensor_tensor(out=xt[:, :], in0=xt[:, :], in1=rs[:, :], op=ALU.mult), reads=[xb, b_rs], writes=[xb])
            cx.op("dve", lambda e: e.tensor_scalar(out=xt[:, :], in0=xt[:, :], scalar1=gvT[:, 16 + j:17 + j], scalar2=None, op0=ALU.mult),
                  reads=[xb, b_gvT], writes=[xb])
            for tc in range(8):
                st, sbf = next_stg()
                transpose_f32(lambda ps_ap, pb, st=st, sbf=sbf: cx.op("act", lambda e: e.activation(out=st[:, :], in_=ps_ap, func=AF.Copy),
                                                                      reads=[pb], writes=[sbf]), xt[:, tc * 128:(tc + 1) * 128], 128, 128, [xb])
                cx.dma("sp", y_out[tc * 128:(tc + 1) * 128, j * 128:(j + 1) * 128], st[:, :], reads=[sbf], is_output=True)

    cx.finish()
    es.close()
    return nc, in_names


def _rope_tables(sample):
    f = np.arange(64)
    axis = f // 32
    jj = f % 16
    hf = (f % 32) // 16
    t = np.arange(T)
    if sample:
        pos = np.where(axis[:, None] == 0, (t // 64)[None, :], (t % 64)[None, :]).astype(np.float32)
        inv = (10000.0 ** (-(jj.astype(np.float32)) / 16.0)).astype(np.float32)
        ang = pos * inv[:, None]
        c = np.cos(ang).astype(np.float32)
        s = np.sin(ang).astype(np.float32)
        s = np.where(hf[:, None] == 0, -s, s).astype(np.float32)
    else:
        c = np.ones((64, T), np.float32)
        s = np.zeros((64, T), np.float32)
    partner = np.where(hf == 0, f + 16, f - 16)
    perm = np.zeros((64, 64), np.float32)
    perm[partner, f] = 1.0
    return c, s, perm


def _core_consts(sample):
    c, s, perm = _rope_tables(sample)
    t = np.arange(T)
    qaug = np.zeros((4, T), np.float32)
    kaug = np.zeros((4, TK), np.float32)
    seglen = T if sample else 256
    if not sample:
        seg = t // 256
        for sp in range(4):
            qaug[sp] = -(seg == sp).astype(np.float32)
            kaug[sp, :PAST] = BIG
            kaug[sp, PAST:] = BIG * (seg != sp).astype(np.float32)
    invcnt = np.zeros((4, T), np.float32)
    tt = t % seglen
    for gi, w in enumerate((2, 4, 8, 16)):
        lo = np.maximum(tt - w // 2, 0)
        hi = np.minimum(tt + (w - 1) // 2, seglen - 1)
        invcnt[gi] = 1.0 / (hi - lo + 1).astype(np.float32)
    sel4 = np.zeros((4, 4, 128), np.float32)
    for g in range(4):
        sel4[g, g, :] = 1.0
    return {
        "ident": np.eye(128, dtype=np.float32), "perm64": perm, "ropec": c, "ropes": s, "qaug": qaug, "kaug": kaug,
        "flag": np.full((128, 1), 1.0 if sample else 0.0, np.float32), "invcnt": invcnt,
        "sel4": sel4.reshape(4, 512), "iota16": np.tile(np.arange(16, dtype=np.float32), (128, 1)),
    }


def make_in_maps(inp, L=4, cores=range(8), names=None):
    f = lambda a: np.ascontiguousarray(np.asarray(a, dtype=np.float32))
    vecs = np.zeros((L, 144, 128), np.float32)
    for l in range(L):
        vecs[l, 0:96] = f(inp["b_mod"])[l].reshape(96, 128)
        vecs[l, 96:112] = f(inp["g_norm1"])[l].reshape(16, 128)
        vecs[l, 112:128] = f(inp["g_norm2"])[l].reshape(16, 128)
        vecs[l, 128:132] = f(inp["g_qnorm"])[l].reshape(4, 128)
        vecs[l, 132:134] = f(inp["g_kvnorm"])[l].reshape(2, 128)
        vecs[l, 134:138] = f(inp["pool_scale"])[l].reshape(4, 128)
        vecs[l, 138] = f(inp["g_diffnorm"])[l]
    shared = {
        "w_mod": f(inp["w_mod"])[:L], "vecs": vecs, "w_in": f(inp["w_in"])[:L], "w_qb": f(inp["w_qb"])[:L],
        "w_kvb": f(inp["w_kvb"])[:L], "w_pool": f(inp["w_pool"])[:L],
        "diff_lambda": f(inp["diff_lambda"])[:L].reshape(L, 256),
        "w_br_mla": f(inp["w_br_mla"])[:L], "w_br_pool": f(inp["w_br_pool"])[:L], "w_br_diff": f(inp["w_br_diff"])[:L],
        "w_out": f(inp["w_out"])[:L], "w_peer_q": f(inp["w_peer_q"])[:L], "peer_subkeys": f(inp["peer_subkeys"])[:L],
    }
    if names is None or "peer_u" in names:
        shared["peer_u"] = f(inp["peer_u"])[:L]
        shared["peer_v"] = f(inp["peer_v"])[:L]
    xp = f(inp["x_prompt"]); xsm = f(inp["x_sample"])
    cs = {True: _core_consts(True), False: _core_consts(False)}
    maps = []
    for c in cores:
        sample = c >= 4
        m = dict(shared)
        m.update(cs[sample])
        gv = np.zeros((32, 128), np.float32)
        gv[16:32] = f(inp["g_final"]).reshape(16, 128)
        if not sample:
            m["x"] = xp[4 * c:4 * c + 4].reshape(T, D)
            gv[0:16] = f(inp["c_ctx"]).reshape(16, 128)
            m["ctx_ckv"] = np.zeros((L, PAST, 256), np.float32)
            m["ctx_krope"] = np.zeros((L, PAST, 64), np.float32)
            m["ctx_dk"] = np.zeros((L, PAST, 512), np.float32)
            m["ctx_dv"] = np.zeros((L, PAST, 512), np.float32)
        else:
            b = (c - 4) % 2
            m["x"] = xsm[b]
            gv[0:16] = f(inp["c"])[b].reshape(16, 128)
            m["ctx_ckv"] = f(inp["cache_mla_ckv"])[b, :L]
            m["ctx_krope"] = f(inp["cache_mla_krope"])[b, :L]
            m["ctx_dk"] = f(inp["cache_diff_k"])[b, :L].reshape(L, PAST, 512)
            m["ctx_dv"] = f(inp["cache_diff_v"])[b, :L].reshape(L, PAST, 512)
        m["gvec"] = gv
        if names is not None:
            m = {k: v for k, v in m.items() if k in names}
        maps.append(m)
    return maps


def kernel(**inp):
    L = 4
    nc, names = build_program(L)
    maps = make_in_maps(inp, L, names=names)
    res = run_bass_kernel_spmd(nc, maps, core_ids=list(range(8)))
    r = res.results
    y_prompt = np.zeros((16, 256, D), np.float32)
    st_ckv = np.zeros((16, L, 256, 256), np.float32)
    st_krope = np.zeros((16, L, 256, 64), np.float32)
    st_k = np.zeros((16, L, 256, 4, 128), np.float32)
    st_v = np.zeros((16, L, 256, 4, 128), np.float32)
    for c in range(4):
        y_prompt[4 * c:4 * c + 4] = r[c]["y"].reshape(4, 256, D)
        for l in range(L):
            st_ckv[4 * c:4 * c + 4, l] = r[c]["o_ckv"][l].reshape(4, 256, 256)
            st_krope[4 * c:4 * c + 4, l] = r[c]["o_krope"][l].reshape(4, 256, 64)
            st_k[4 * c:4 * c + 4, l] = r[c]["o_dk"][l].reshape(4, 256, 4, 128)
            st_v[4 * c:4 * c + 4, l] = r[c]["o_dv"][l].reshape(4, 256, 4, 128)
    y_sample = np.stack([r[4]["y"], r[5]["y"]], axis=0)
    return (y_prompt, y_sample, st_ckv, st_krope, st_k, st_v)
```

```python
import contextlib
import math

import numpy as np
import concourse.bass as bass
import concourse.mybir as mybir
from concourse.bass_utils import run_bass_kernel_spmd

F32 = mybir.dt.float32
BF16 = mybir.dt.bfloat16
I32 = mybir.dt.int32
U32 = mybir.dt.uint32
AF = mybir.ActivationFunctionType
ALU = mybir.AluOpType
AX = mybir.AxisListType

D = 2048
T = 1024
TK = 1280
PAST = 256
NDS = 40
EPS = 1e-6
BIG = 30000.0

C_QA, C_KVA, C_PZ, C_DQ, C_DK, C_DV, C_GL = 0, 512, 832, 1344, 1856, 2368, 2880


class Buf:
    __slots__ = ("name", "w", "r")

    def __init__(self, name):
        self.name = name
        self.w = None
        self.r = {}


class _Proxy:
    def __getattr__(self, name):
        return lambda *a, **k: (name, a, k)


class Ctx:
    def __init__(self, nc, es):
        self.nc = nc
        self.es = es
        self.eng = {}
        for name, h in (("pe", nc.tensor), ("dve", nc.vector), ("act", nc.scalar),
                        ("pool", nc.gpsimd), ("sp", nc.sync)):
            self.eng[name] = {"h": h, "sem": es.enter_context(nc.semaphore("e_" + name)), "cnt": 0,
                              "known": {}, "name": name}
        self.dsems = [es.enter_context(nc.semaphore("d%d" % i)) for i in range(NDS)]
        self.dcnt = [0] * NDS
        self.dnext = 0
        self.ntile = 0
        self.out_events = []

    def sb(self, name, shape, dtype):
        self.ntile += 1
        return self.es.enter_context(self.nc.sbuf_tensor("%s_%d" % (name, self.ntile), list(shape), dtype))

    def ps(self, name, shape, dtype):
        self.ntile += 1
        return self.es.enter_context(self.nc.psum_tensor("%s_%d" % (name, self.ntile), list(shape), dtype))

    def _wait(self, e, ev):
        sem, val = ev
        k = id(sem)
        if e["known"].get(k, 0) >= val:
            return
        e["h"].wait_ge(sem, val)
        e["known"][k] = val

    def _collect(self, reads, writes):
        evs = []
        for b in reads:
            if b.w is not None:
                evs.append(b.w)
        for b in writes:
            if b.w is not None:
                evs.append(b.w)
            evs.extend(b.r.values())
        return evs

    def begin_record(self):
        self.rec = []

    def end_record(self):
        r, self.rec = self.rec, None
        return r

    def replay(self, item):
        if item[0] == "op":
            _, ename, (name, args, kwargs), reads, writes = item
            self.op(ename, lambda h: getattr(h, name)(*args, **kwargs), reads, writes)
        else:
            _, qname, out, in_, reads, writes, indirect, is_output = item
            self.dma(qname, out, in_, reads, writes, indirect, is_output)

    def op(self, ename, fn, reads=(), writes=()):
        if getattr(self, "rec", None) is not None:
            self.rec.append(("op", ename, fn(_Proxy()), list(reads), list(writes)))
            return None
        e = self.eng[ename]
        for ev in self._collect(reads, writes):
            self._wait(e, ev)
        inst = fn(e["h"])
        e["cnt"] += 1
        inst.then_inc(e["sem"], 1)
        ev = (e["sem"], e["cnt"])
        e["known"][id(e["sem"])] = e["cnt"] if ename == "pe" else e["cnt"] - 1
        for b in reads:
            b.r[ename] = ev
        for b in writes:
            b.w = ev
            b.r = {}
        return ev

    def dma(self, qname, out, in_, reads=(), writes=(), indirect=None, is_output=False):
        if getattr(self, "rec", None) is not None:
            self.rec.append(("dma", qname, out, in_, list(reads), list(writes), indirect, is_output))
            return None
        e = self.eng[qname]
        for ev in self._collect(reads, writes):
            self._wait(e, ev)
        slot = self.dnext
        self.dnext = (slot + 1) % NDS
        if self.dcnt[slot] > 0:
            self._wait(e, (self.dsems[slot], 16 * self.dcnt[slot]))
        self.dcnt[slot] += 1
        if indirect is not None:
            inst = e["h"].indirect_dma_start(out=out, out_offset=None, in_=in_,
                                             in_offset=bass.IndirectOffsetOnAxis(ap=indirect, axis=0))
        else:
            inst = e["h"].dma_start(out=out, in_=in_)
        inst.then_inc(self.dsems[slot], 16)
        ev = (self.dsems[slot], 16 * self.dcnt[slot])
        for b in reads:
            b.r[("d", slot)] = ev
        for b in writes:
            b.w = ev
            b.r = {}
        if is_output:
            self.out_events.append(ev)
        return ev

    def fence(self):
        evs = [(e["sem"], e["cnt"]) for e in self.eng.values() if e["cnt"] > 0]
        evs += [(self.dsems[s], 16 * self.dcnt[s]) for s in range(NDS) if self.dcnt[s] > 0]
        for e in self.eng.values():
            for ev in evs:
                if ev[0] is e["sem"]:
                    continue
                self._wait(e, ev)

    def finish(self):
        e = self.eng["sp"]
        for ev in self.out_events:
            self._wait(e, ev)
        evs = [(x["sem"], x["cnt"]) for x in self.eng.values() if x["cnt"] > 0 and x is not e]
        evs += [(self.dsems[s], 16 * self.dcnt[s]) for s in range(NDS) if self.dcnt[s] > 0]
        for ev in evs:
            self._wait(e, ev)


class PsumRing:
    def __init__(self, ctx, n=8):
        self.tiles = [ctx.ps("bank", [128, 512], F32) for _ in range(n)]
        self.bufs = [Buf("bank%d" % i) for i in range(n)]
        self.i = 0
        self.n = n
        self.held = set()

    def next(self):
        while self.i in self.held:
            self.i = (self.i + 1) % self.n
        i = self.i
        self.i = (i + 1) % self.n
        return self.tiles[i], self.bufs[i]

    def next_n(self, k):
        return [self.next() for _ in range(k)]

    def hold(self, k):
        out = []
        for _ in range(k):
            t, b = self.next()
            self.held.add(self.tiles.index(t))
            out.append((t, b))
        return out

    def release(self, banks):
        for t, b in banks:
            self.held.discard(self.tiles.index(t))


def build_program(L=4, stop_after=None):
    nc = bass.Bass("TRN2", target_bir_lowering=False)
    es = contextlib.ExitStack()
    cx = Ctx(nc, es)
    dbg = stop_after is not None

    in_names = []

    def din(name, shape, dtype=F32):
        in_names.append(name)
        return nc.dram_tensor(name, list(shape), dtype, kind="ExternalInput").ap()

    def dout(name, shape, dtype=F32):
        return nc.dram_tensor(name, list(shape), dtype, kind="ExternalOutput").ap()

    x_in = din("x", [T, D])
    gvec = din("gvec", [32, 128])
    ctx_ckv = din("ctx_ckv", [L, PAST, 256])
    ctx_krope = din("ctx_krope", [L, PAST, 64])
    ctx_dk = din("ctx_dk", [L, PAST, 512])
    ctx_dv = din("ctx_dv", [L, PAST, 512])
    ident_d = din("ident", [128, 128])
    perm_d = din("perm64", [64, 64])
    ropec_d = din("ropec", [64, T])
    ropes_d = din("ropes", [64, T])
    qaug_d = din("qaug", [4, T])
    kaug_d = din("kaug", [4, TK])
    flag_d = din("flag", [128, 1])
    invcnt_d = din("invcnt", [4, T])
    sel4_d = din("sel4", [4, 512])
    iota16_d = din("iota16", [128, 16])
    w_mod = din("w_mod", [L, D, 6 * D])
    vecs = din("vecs", [L, 144, 128])
    w_in = din("w_in", [L, D, 9024])
    w_qb = din("w_qb", [L, 512, 1536])
    w_kvb = din("w_kvb", [L, 256, 2048])
    w_pool = din("w_pool", [L, 4, 128, 128])
    dlam = din("diff_lambda", [L, 256])
    w_br_mla = din("w_br_mla", [L, 1024, D])
    w_br_pool = din("w_br_pool", [L, 512, D])
    w_br_diff = din("w_br_diff", [L, 512, D])
    w_out = din("w_out", [L, D, D])
    w_peer_q = din("w_peer_q", [L, D, D])
    subkeys = din("peer_subkeys", [L, 2, 128, 128])
    need_peer = stop_after is None or stop_after.startswith("peer") or stop_after == "layer"
    if need_peer:
        peer_u = din("peer_u", [L, 16384, D])
        peer_v = din("peer_v", [L, 16384, D])

    y_out = dout("y", [T, D])
    o_ckv = dout("o_ckv", [L, T, 256])
    o_krope = dout("o_krope", [L, T, 64])
    o_dk = dout("o_dk", [L, T, 512])
    o_dv = dout("o_dv", [L, T, 512])
    if dbg:
        dbg_out = dout("dbg", [128, 16 * T])

    xs = nc.dram_tensor("xs", [16, 128, T], F32, kind="Internal").ap()
    if need_peer:
        puv16 = [nc.dram_tensor("puv16_%d" % i, [16384, 2, D], BF16, kind="Internal").ap() for i in range(L)]
        puv_rows = [t.rearrange("e a d -> e (a d)") for t in puv16]
        pu_flat = peer_u.rearrange("l e d -> (l e) d")
        pv_flat = peer_v.rearrange("l e d -> (l e) d")
    b_tab = [[Buf("tab%d_%d" % (l, i)) for i in range(16)] for l in range(L)]
    conv_pos = [0] * L

    def conv_step(l, n):
        if not need_peer:
            return
        for _ in range(n):
            i = conv_pos[l]
            if i >= 16:
                return
            conv_pos[l] += 1
            src, which = (pu_flat, 0) if i < 8 else (pv_flat, 1)
            r0 = (i % 8) * 2048
            cx.dma("pool", puv16[l][r0:r0 + 2048, which, :].rearrange("(p r) d -> p r d", p=128),
                   src[l * 16384 + r0:l * 16384 + r0 + 2048, :].rearrange("(p r) d -> p r d", p=128), writes=[b_tab[l][i]])

    ident_f = cx.sb("ident_f", [128, 128], F32); b_ident = Buf("ident")
    ident_b = cx.sb("ident_b", [128, 128], BF16)
    ones_f = cx.sb("ones_f", [128, 128], F32)
    ones_b = cx.sb("ones_b", [128, 128], BF16)
    perm_f = cx.sb("perm", [64, 64], F32)
    ropec = cx.sb("ropec", [64, T], F32)
    ropes = cx.sb("ropes", [64, T], F32)
    flag = cx.sb("flag", [128, 1], F32)
    invcnt = cx.sb("invcnt", [4, T], F32)
    sel4 = cx.sb("sel4", [4, 512], F32)
    iota16 = cx.sb("iota16", [128, 16], F32)
    b_const = Buf("consts")
    hT = cx.sb("hT", [128, 16, T], BF16)
    b_hT = [Buf("hT%d" % j) for j in range(16)]
    NW = 2
    wbuf = [cx.sb("wbuf", [128, 8192], BF16) for _ in range(NW)]
    b_wbuf = [Buf("wbuf%d" % i) for i in range(NW)]
    wsm = [cx.sb("wsm", [128, 4096], BF16) for _ in range(2)]
    b_wsm = [Buf("wsm%d" % i) for i in range(2)]
    scr = [cx.sb("scr", [128, T], F32) for _ in range(3)]
    b_scr = [Buf("scr%d" % i) for i in range(3)]
    scr_i = [0]
    rs = cx.sb("rs", [128, T], F32); b_rs = Buf("rs")
    modT = cx.sb("modT", [128, 96], F32); b_modT = Buf("modT")
    vecT = cx.sb("vecT", [128, 144], F32); b_vecT = Buf("vecT")
    gvT = cx.sb("gvT", [128, 32], F32); b_gvT = Buf("gvT")
    scondT = cx.sb("scondT", [128, 16], BF16); b_scond = Buf("scond")
    drv = cx.sb("drv", [128, 64], F32); b_drv = Buf("drv")
    lamt = cx.sb("lamt", [128, 264], F32); b_lam = Buf("lam")
    stg = [cx.sb("stg", [128, 128], F32) for _ in range(4)]
    b_stg = [Buf("stg%d" % i) for i in range(4)]
    stg_i = [0]
    ARENA = 90 * 1024
    arena = cx.sb("arena", [128, ARENA // 2], BF16)

    pr = PsumRing(cx, 8)

    def next_scr():
        i = scr_i[0]
        scr_i[0] = (i + 1) % 3
        return scr[i], b_scr[i]

    def next_stg():
        i = stg_i[0]
        stg_i[0] = (i + 1) % 4
        return stg[i], b_stg[i]

    class Arena:
        def __init__(self):
            self.off = 0

        def reset(self):
            self.off = 0

        def take(self, shape, dtype):
            n = 1
            for s in shape[1:]:
                n *= s
            nbytes = n * (4 if dtype in (F32, I32, U32) else 2)
            nbytes = (nbytes + 63) // 64 * 64
            assert self.off + nbytes <= ARENA, ("arena overflow", self.off, nbytes)
            a = arena[:, self.off // 2:(self.off + nbytes) // 2]
            self.off += nbytes
            if dtype != BF16:
                a = a.bitcast(dtype)
            a = a[:, 0:n]
            if len(shape) == 3:
                a = a.rearrange("p (a b) -> p a b", a=shape[1])
            elif len(shape) == 4:
                a = a.rearrange("p (a b c) -> p a b c", a=shape[1], b=shape[2])
            return a

    ar = Arena()

    for t_sb, t_d in ((ident_f, ident_d), (perm_f, perm_d), (ropec, ropec_d), (ropes, ropes_d), (flag, flag_d),
                      (invcnt, invcnt_d), (sel4, sel4_d), (iota16, iota16_d)):
        cx.dma("sp", t_sb[:], t_d, writes=[b_const])
    cx.dma("pool", ident_b[:], ident_d, writes=[b_const])
    cx.op("dve", lambda e: e.memset(ones_f[:], 1.0), writes=[b_const])
    cx.op("dve", lambda e: e.memset(ones_b[:], 1.0), writes=[b_const])

    def transpose_f32(dst_fn, src_ap, p_in, f_in, reads, n_consume=1):
        pt, pb = pr.next()
        cx.op("pe", lambda e: e.transpose(pt[0:f_in, 0:p_in], src_ap, ident_f[0:p_in, 0:p_in]),
              reads=list(reads) + [b_const], writes=[pb])
        dst_fn(pt[0:f_in, 0:p_in], pb)

    for tc in range(8):
        xt, xb = next_scr()
        for hh in range(2):
            cx.dma("sp", xt[:, 0:T], x_in[tc * 128:(tc + 1) * 128, hh * T:(hh + 1) * T], writes=[xb])
            for jj in range(8):
                j = hh * 8 + jj
                st, sbf = next_stg()

                def ev(ps_ap, pb, st=st, sbf=sbf):
                    cx.op("act", lambda e: e.activation(out=st[:, :], in_=ps_ap, func=AF.Copy),
                          reads=[pb], writes=[sbf])
                transpose_f32(ev, xt[:, jj * 128:(jj + 1) * 128], 128, 128, [xb])
                cx.dma("sp", xs[j, :, tc * 128:(tc + 1) * 128], st[:, :], reads=[sbf], writes=[])
    b_xs = [Buf("xs%d" % j) for j in range(16)]
    cx.fence()

    st, sbf = next_stg()
    cx.dma("sp", st[0:32, :], gvec, writes=[sbf])
    transpose_f32(lambda ps_ap, pb: cx.op("act", lambda e: e.activation(out=gvT[:, :], in_=ps_ap, func=AF.Copy),
                                          reads=[pb], writes=[b_gvT]), st[0:32, :], 32, 128, [sbf])
    cx.op("act", lambda e: e.activation(out=scondT[:, :], in_=gvT[:, 0:16], func=AF.Silu),
          reads=[b_gvT], writes=[b_scond])

    w_i = [0]

    def load_w(dram_ap, kc, ncols, small=False):
        if small:
            i = w_i[0] % 2
            tile, buf = wsm[i], b_wsm[i]
            assert kc * ncols <= 4096
        else:
            i = w_i[0] % NW
            tile, buf = wbuf[i], b_wbuf[i]
            assert kc * ncols <= 8192
        w_i[0] += 1
        view = tile[:, 0:kc * ncols].rearrange("p (k n) -> p k n", k=kc)
        rows = dram_ap.shape[0]
        if rows >= 128:
            cx.dma("pool", view, dram_ap.rearrange("(k p) n -> p k n", p=128), writes=[buf])
        else:
            cx.dma("pool", view[0:rows, 0, :], dram_ap, writes=[buf])
        return view, buf

    def proj_fm(w_dram, K, cols, rhs_fn, rhs_bufs, consume, small=False, group=512, halves=(0, 1)):
        kc_n = max(1, K // 128)
        gi = 0
        while gi < len(cols):
            c_start = cols[gi][0]
            ge = gi
            while ge < len(cols) and cols[ge][0] + cols[ge][1] - c_start <= group:
                ge += 1
            width = cols[ge - 1][0] + cols[ge - 1][1] - c_start
            wv, wb = load_w(w_dram[:, c_start:c_start + width], kc_n, width, small=small)
            for ci in range(gi, ge):
                c0, M = cols[ci]
                for half in halves:
                    pt, pb = pr.next()
                    for kc in range(kc_n):
                        kp = min(128, K - kc * 128)
                        cx.op("pe", lambda e, kc=kc, kp=kp: e.matmul(
                            pt[0:M, :], wv[0:kp, kc, c0 - c_start:c0 - c_start + M], rhs_fn(kc, half),
                            start=(kc == 0), stop=(kc == kc_n - 1)),
                            reads=[wb] + list(rhs_bufs(kc)), writes=[pb])
                    consume(ci, half, pt[0:M, :], pb)
            gi = ge

    def rstd_from_psum(ps_list, nfeat, out_ap_fn, out_buf):
        for half, (pt, pb) in enumerate(ps_list):
            cx.op("act", lambda e: e.activation(out=out_ap_fn(half), in_=pt[:, :], func=AF.Sqrt,
                                                scale=1.0 / nfeat, bias=eps_t[:, 0:1]),
                  reads=[pb, b_const], writes=[out_buf])
        for half in range(len(ps_list)):
            cx.op("dve", lambda e: e.reciprocal(out=out_ap_fn(half), in_=out_ap_fn(half)),
                  reads=[out_buf], writes=[out_buf])

    eps_t = cx.sb("eps", [128, 1], F32)
    cx.op("dve", lambda e: e.memset(eps_t[:], EPS), writes=[b_const])

    def norm_x(l, which, final=False):
        acc = pr.next_n(2)
        for j in range(16):
            xt, xb = next_scr()
            cx.dma("sp", xt[:, :], xs[j], reads=[b_xs[j]], writes=[xb])
            sq, sqb = next_scr()
            cx.op("act", lambda e: e.activation(out=sq[:, :], in_=xt[:, :], func=AF.Square), reads=[xb], writes=[sqb])
            for half in range(2):
                pt, pb = acc[half]
                cx.op("pe", lambda e: e.matmul(pt[:, :], ones_f[:, :], sq[:, half * 512:(half + 1) * 512],
                                               start=(j == 0), stop=(j == 15)), reads=[sqb, b_const], writes=[pb])
        rstd_from_psum(acc, D, lambda half: rs[:, half * 512:(half + 1) * 512], b_rs)

    def apply_norm(l, which):
        gs = drv[:, (0 if which == 1 else 16):]
        sh = modT[:, (0 if which == 1 else 48):]
        for j in range(16):
            xt, xb = next_scr()
            cx.dma("sp", xt[:, :], xs[j], reads=[b_xs[j]], writes=[xb])
            cx.op("dve", lambda e: e.tensor_tensor(out=xt[:, :], in0=xt[:, :], in1=rs[:, :], op=ALU.mult),
                  reads=[xb, b_rs], writes=[xb])
            cx.op("dve", lambda e: e.tensor_scalar(out=hT[:, j, :], in0=xt[:, :], scalar1=gs[:, j:j + 1],
                                                   scalar2=sh[:, j:j + 1], op0=ALU.mult, op1=ALU.add),
                  reads=[xb, b_drv, b_modT], writes=[b_hT[j]])

    def dump(ap_2d, bufs, ncols):
        cx.dma("sp", dbg_out[0:ap_2d.shape[0], 0:ncols], ap_2d, reads=bufs, writes=[], is_output=True)

    for l in range(L):
        st, sbf = next_stg()
        cx.dma("sp", st[:, :], vecs[l, 0:128, :], writes=[sbf])
        transpose_f32(lambda ps_ap, pb: cx.op("act", lambda e: e.activation(out=vecT[:, 0:128], in_=ps_ap, func=AF.Copy),
                                              reads=[pb], writes=[b_vecT]), st[:, :], 128, 128, [sbf])
        st, sbf = next_stg()
        cx.dma("sp", st[0:16, :], vecs[l, 128:144, :], writes=[sbf])
        transpose_f32(lambda ps_ap, pb: cx.op("act", lambda e: e.activation(out=vecT[:, 128:144], in_=ps_ap, func=AF.Copy),
                                              reads=[pb], writes=[b_vecT]), st[0:16, :], 16, 128, [sbf])
        mt, mb = pr.next()
        for g in range(24):
            wv, wb = load_w(w_mod[l, :, g * 512:(g + 1) * 512], 16, 512)
            for cc in range(4):
                m = g * 4 + cc
                for kc in range(16):
                    cx.op("pe", lambda e: e.matmul(mt[:, m:m + 1], wv[:, kc, cc * 128:(cc + 1) * 128],
                                                   scondT[:, kc:kc + 1], start=(kc == 0), stop=(kc == 15)),
                          reads=[wb, b_scond], writes=[mb])
        cx.op("dve", lambda e: e.tensor_tensor(out=modT[:, :], in0=mt[:, 0:96], in1=vecT[:, 0:96], op=ALU.add),
              reads=[mb, b_vecT], writes=[b_modT])
        cx.op("dve", lambda e: e.scalar_tensor_tensor(out=drv[:, 0:16], in0=modT[:, 16:32], scalar=1.0,
                                                      in1=vecT[:, 96:112], op0=ALU.add, op1=ALU.mult),
              reads=[b_modT, b_vecT], writes=[b_drv])
        cx.op("dve", lambda e: e.scalar_tensor_tensor(out=drv[:, 16:32], in0=modT[:, 64:80], scalar=1.0,
                                                      in1=vecT[:, 112:128], op0=ALU.add, op1=ALU.mult),
              reads=[b_modT, b_vecT], writes=[b_drv])
        if stop_after == "mod":
            dump(modT[:, :], [b_modT], 96)
            break
        norm_x(l, 1)
        apply_norm(l, 1)
        conv_step(l, 4)
        if stop_after == "norm1":
            for j in range(16):
                xt, xb = next_scr()
                cx.op("act", lambda e: e.activation(out=xt[:, :], in_=hT[:, j, :], func=AF.Copy),
                      reads=[b_hT[j]], writes=[xb])
                cx.dma("sp", dbg_out[:, j * T:(j + 1) * T], xt[:, :], reads=[xb], is_output=True)
            break
        hrhs = lambda kc, half: hT[:, kc, half * 512:(half + 1) * 512]
        hbufs = lambda kc: [b_hT[kc]]
        lam_init = 0.8 - 0.6 * math.exp(-0.3 * l)
        ar.reset()
        mla_oT = ar.take([128, 8, T], BF16); b_mla_o = Buf("mla_o")
        pool_oT = ar.take([128, 4, T], BF16); b_pool_o = Buf("pool_o")
        diff_oT = ar.take([128, 4, T], BF16); b_diff_o = Buf("diff_o")
        mark = ar.off

        def mm_fm(wv, wb, coff, M, K, rhs_fn, rhs_bufs, half, N=512):
            kc_n = max(1, K // 128)
            pt, pb = pr.next()
            for kc in range(kc_n):
                kp = min(128, K - kc * 128)
                cx.op("pe", lambda e: e.matmul(pt[0:M, 0:N], wv[0:kp, kc, coff:coff + M], rhs_fn(kc, half),
                                               start=(kc == 0), stop=(kc == kc_n - 1)),
                      reads=[wb] + list(rhs_bufs(kc)), writes=[pb])
            return pt, pb

        def rope_unit(raw, rawb, dst_fn, dstb):
            for half in range(2):
                sl = slice(half * 512, (half + 1) * 512)
                pt, pb = pr.next()
                cx.op("pe", lambda e: e.matmul(pt[0:64, :], perm_f[:, :], raw[0:64, sl], start=True, stop=True),
                      reads=[rawb, b_const], writes=[pb])
                t1, t1b = next_scr()
                cx.op("dve", lambda e: e.tensor_tensor(out=t1[0:64, 0:512], in0=pt[0:64, :], in1=ropes[:, sl], op=ALU.mult),
                      reads=[pb, b_const], writes=[t1b])
                cx.op("dve", lambda e: e.tensor_tensor(out=t1[0:64, 512:1024], in0=raw[0:64, sl], in1=ropec[:, sl], op=ALU.mult),
                      reads=[rawb, b_const], writes=[t1b])
                cx.op("dve", lambda e: e.tensor_tensor(out=dst_fn(sl), in0=t1[0:64, 0:512], in1=t1[0:64, 512:1024], op=ALU.add),
                      reads=[t1b], writes=[dstb])

        def out_transposes(raw, rawb, nfeat, out_dram_fn, extra=None):
            for tc in range(8):
                st, sbf = next_stg()

                def ev(ps_ap, pb, st=st, sbf=sbf, tc=tc):
                    cx.op("act", lambda e: e.activation(out=st[:, 0:nfeat], in_=ps_ap, func=AF.Copy),
                          reads=[pb], writes=[sbf])
                    if extra is not None:
                        extra(tc, st[:, 0:nfeat], sbf)
                transpose_f32(ev, raw[0:nfeat, tc * 128:(tc + 1) * 128], nfeat, 128, [rawb])
                import os
                if os.environ.get("SKIP_ODMA"):
                    continue
                cx.dma("sp", out_dram_fn(tc), st[:, 0:nfeat], reads=[sbf], is_output=True)

        def attention_core(kparts, qparts, v_fn, v_bufs, et, etb, qh, scale):
            for kc in range(10):
                pt, pb = pr.next()
                n = len(kparts)
                for i in range(n):
                    kf, kb, K = kparts[i]
                    qa_, qb_ = qparts[i]
                    cx.op("pe", lambda e: e.matmul(pt[:, :], kf(kc), qa_, start=(i == 0), stop=(i == n - 1)),
                          reads=list(kb) + list(qb_), writes=[pb])
                cx.op("act", lambda e: e.activation(out=et[:, kc, :], in_=pt[:, :], func=AF.Exp, scale=scale),
                      reads=[pb], writes=[etb])
            (o_ps, o_pb), (d_ps, d_pb) = pr.next_n(2)
            for kc in range(10):
                cx.op("pe", lambda e: e.matmul(o_ps[:, :], v_fn(kc), et[:, kc, :], start=(kc == 0), stop=(kc == 9)),
                      reads=[etb] + list(v_bufs), writes=[o_pb])
            for kc in range(10):
                cx.op("pe", lambda e: e.matmul(d_ps[:, :], ones_b[:, :], et[:, kc, :], start=(kc == 0), stop=(kc == 9)),
                      reads=[etb, b_const], writes=[d_pb])
            return o_ps, o_pb, d_ps, d_pb

        vdiff = ar.take([128, 10, 512], BF16); b_vdiff = Buf("vdiff")
        ET = [ar.take([128, 10, 512], BF16) for _ in range(2)]; b_ET = [Buf("ET0"), Buf("ET1")]
        qu = [ar.take([128, T], BF16) for _ in range(2)]; b_qu = [Buf("qu0"), Buf("qu1")]
        ku = [ar.take([128, TK], BF16) for _ in range(2)]; b_ku = [Buf("ku0"), Buf("ku1")]
        asb = [ar.take([128, 512], F32) for _ in range(3)]; b_asb = [Buf("as0"), Buf("as1"), Buf("as2")]
        ctxk = ar.take([128, 2, 512], F32); b_ctxk = Buf("ctxk")
        for s in range(2):
            cx.dma("pool", qu[s][64:68, :], qaug_d, writes=[b_qu[s]])
            cx.dma("pool", ku[s][64:68, :], kaug_d, writes=[b_ku[s]])
        cx.dma("sp", ctxk, ctx_dk[l].rearrange("(c p) f -> p c f", p=128), writes=[b_ctxk])
        cx.dma("pool", vdiff[:, 0:2, :], ctx_dv[l].rearrange("(c p) f -> p c f", p=128), writes=[b_vdiff])
        cx.dma("sp", lamt[:, 0:256], dlam[l].partition_broadcast(128), writes=[b_lam])
        for i in range(2):
            cx.op("dve", lambda e: e.scalar_tensor_tensor(out=lamt[:, 128 * i:128 * i + 64], in0=lamt[:, 128 * i:128 * i + 64],
                                                          scalar=1.0, in1=lamt[:, 128 * i + 64:128 * i + 128],
                                                          op0=ALU.mult, op1=ALU.mult, accum_out=lamt[:, 256 + i:257 + i]),
                  reads=[b_lam], writes=[b_lam])
        cx.op("act", lambda e: e.activation(out=lamt[:, 258:260], in_=lamt[:, 256:258], func=AF.Exp), reads=[b_lam], writes=[b_lam])
        cx.op("dve", lambda e: e.tensor_tensor(out=lamt[:, 260:261], in0=lamt[:, 259:260], in1=lamt[:, 258:259], op=ALU.subtract),
              reads=[b_lam], writes=[b_lam])
        cx.op("dve", lambda e: e.tensor_scalar(out=lamt[:, 260:261], in0=lamt[:, 260:261], scalar1=-lam_init, scalar2=None, op0=ALU.add),
              reads=[b_lam], writes=[b_lam])
        cx.op("dve", lambda e: e.tensor_scalar(out=drv[:, 32:33], in0=vecT[:, 138:139], scalar1=(1.0 - lam_init), scalar2=None, op0=ALU.mult),
              reads=[b_vecT], writes=[b_drv])
        if stop_after == "diffA":
            dump(lamt[:, 0:264], [b_lam], 264)
            break
        wv, wb = load_w(w_in[l, :, C_DV:C_DV + 512], 16, 512)
        for c in range(4):
            raw, rawb = next_scr()
            for half in range(2):
                pt, pb = mm_fm(wv, wb, c * 128, 128, D, hrhs, hbufs, half)
                cx.op("act", lambda e: e.activation(out=raw[:, half * 512:(half + 1) * 512], in_=pt[:, :], func=AF.Copy),
                      reads=[pb], writes=[rawb])

            def extra(tc, ps_ap, pb, c=c):
                import os
                if os.environ.get("SKIP_EXTRA"):
                    return
                cx.op("dve", lambda e: e.tensor_copy(out=vdiff[:, 2 + tc, c * 128:(c + 1) * 128], in_=ps_ap),
                      reads=[pb], writes=[b_vdiff])
            out_transposes(raw, rawb, 128, lambda tc, c=c: o_dv[l, tc * 128:(tc + 1) * 128, c * 128:(c + 1) * 128], extra)
        if stop_after == "diffB":
            dump(lamt[:, 0:264], [b_lam], 264)
            break
        wq_v, wq_b = load_w(w_in[l, :, C_DQ:C_DQ + 512], 16, 512)
        wk_v, wk_b = load_w(w_in[l, :, C_DK:C_DK + 512], 16, 512)
        dscale = 64.0 ** -0.5
        etc = 0
        for h in range(4):
            for s in range(2):
                u = 2 * h + s
                raw, rawb = next_scr()
                for half in range(2):
                    pt, pb = mm_fm(wk_v, wk_b, u * 64, 64, D, hrhs, hbufs, half)
                    cx.op("act", lambda e: e.activation(out=raw[0:64, half * 512:(half + 1) * 512], in_=pt[0:64, :], func=AF.Copy),
                          reads=[pb], writes=[rawb])
                out_transposes(raw, rawb, 64, lambda tc, u=u: o_dk[l, tc * 128:(tc + 1) * 128, u * 64:(u + 1) * 64])
                rope_unit(raw, rawb, lambda sl, s=s: ku[s][0:64, PAST + sl.start:PAST + sl.stop], b_ku[s])
                for tc in range(2):
                    transpose_f32(lambda ps_ap, pb, s=s, tc=tc: cx.op(
                        "dve", lambda e: e.tensor_copy(out=ku[s][0:64, tc * 128:(tc + 1) * 128], in_=ps_ap),
                        reads=[pb], writes=[b_ku[s]]), ctxk[:, tc, u * 64:(u + 1) * 64], 128, 64, [b_ctxk])
                raw, rawb = next_scr()
                for half in range(2):
                    pt, pb = mm_fm(wq_v, wq_b, u * 64, 64, D, hrhs, hbufs, half)
                    cx.op("act", lambda e: e.activation(out=raw[0:64, half * 512:(half + 1) * 512], in_=pt[0:64, :], func=AF.Copy),
                          reads=[pb], writes=[rawb])
                rope_unit(raw, rawb, lambda sl, s=s: qu[s][0:64, sl], b_qu[s])
            conv_step(l, 1)
            for qh in range(2):
                qs = slice(qh * 512, (qh + 1) * 512)
                for s in range(2):
                    et, etb = ET[etc % 2], b_ET[etc % 2]
                    etc += 1
                    o_ps, o_pb, d_ps, d_pb = attention_core(
                        [(lambda kc, s=s: ku[s][0:68, kc * 128:(kc + 1) * 128], [b_ku[s]], 68)],
                        [(qu[s][0:68, qs], [b_qu[s]])],
                        lambda kc: vdiff[:, kc, h * 128:(h + 1) * 128], [b_vdiff], et, etb, qh, dscale)
                    cx.op("dve", lambda e: e.reciprocal(out=asb[2][:, :], in_=d_ps[:, :]), reads=[d_pb], writes=[b_asb[2]])
                    cx.op("dve", lambda e: e.tensor_tensor(out=asb[s][:, :], in0=o_ps[:, :], in1=asb[2][:, :], op=ALU.mult),
                          reads=[o_pb, b_asb[2]], writes=[b_asb[s]])
                cx.op("dve", lambda e: e.scalar_tensor_tensor(out=asb[0][:, :], in0=asb[1][:, :], scalar=lamt[:, 260:261],
                                                              in1=asb[0][:, :], op0=ALU.mult, op1=ALU.add),
                      reads=[b_asb[0], b_asb[1], b_lam], writes=[b_asb[0]])
                cx.op("act", lambda e: e.activation(out=asb[1][:, :], in_=asb[0][:, :], func=AF.Square),
                      reads=[b_asb[0]], writes=[b_asb[1]])
                pt, pb = pr.next()
                cx.op("pe", lambda e: e.matmul(pt[:, :], ones_f[:, :], asb[1][:, :], start=True, stop=True),
                      reads=[b_asb[1], b_const], writes=[pb])
                rstd_from_psum([(pt, pb)], 128, lambda half: asb[2][:, :], b_asb[2])
                cx.op("dve", lambda e: e.tensor_tensor(out=asb[0][:, :], in0=asb[0][:, :], in1=asb[2][:, :], op=ALU.mult),
                      reads=[b_asb[0], b_asb[2]], writes=[b_asb[0]])
                cx.op("dve", lambda e: e.tensor_scalar(out=diff_oT[:, h, qs], in0=asb[0][:, :], scalar1=drv[:, 32:33], scalar2=None, op0=ALU.mult),
                      reads=[b_asb[0], b_drv], writes=[b_diff_o])
        if stop_after == "diff":
            for j in range(4):
                xt, xb = next_scr()
                cx.op("act", lambda e: e.activation(out=xt[:, :], in_=diff_oT[:, j, :], func=AF.Copy), reads=[b_diff_o], writes=[xb])
                cx.dma("sp", dbg_out[:, j * T:(j + 1) * T], xt[:, :], reads=[xb], is_output=True)
            break
        cx.fence()
        ar.off = mark
        zp = ar.take([128, 4, 272], F32); b_zp = Buf("zp")
        pa = ar.take([128, 4, 272], F32); b_pa = Buf("pa")
        pb2 = ar.take([128, 4, 272], F32); b_pb2 = Buf("pb2")
        dTt = ar.take([128, T], BF16); b_dT = Buf("dT")
        wpl = ar.take([128, 4, 128], BF16); b_wpl = Buf("wpl")
        cx.dma("pool", wpl, w_pool[l].rearrange("g c d -> c g d"), writes=[b_wpl])
        cx.op("dve", lambda e: e.memset(zp[:, :, :], 0.0), writes=[b_zp])
        wv, wb = load_w(w_in[l, :, C_PZ:C_PZ + 512], 16, 512)
        for g in range(4):
            for half in range(2):
                pt, pb = mm_fm(wv, wb, g * 128, 128, D, hrhs, hbufs, half)
                cx.op("act", lambda e: e.activation(out=zp[:, 2 * half:2 * half + 2, 8:264],
                                                    in_=pt[:, :].rearrange("p (a b) -> p a b", a=2), func=AF.Copy),
                      reads=[pb], writes=[b_zp])
            cx.op("dve", lambda e: e.tensor_scalar(out=zp[:, 1:4, 0:8], in0=zp[:, 0:3, 256:264], scalar1=flag[:, 0:1], scalar2=None, op0=ALU.mult),
                  reads=[b_zp, b_const], writes=[b_zp])
            cx.op("dve", lambda e: e.tensor_scalar(out=zp[:, 0:3, 264:272], in0=zp[:, 1:4, 8:16], scalar1=flag[:, 0:1], scalar2=None, op0=ALU.mult),
                  reads=[b_zp, b_const], writes=[b_zp])
            cx.op("dve", lambda e: e.tensor_tensor(out=pa[:, :, 1:272], in0=zp[:, :, 1:272], in1=zp[:, :, 0:271], op=ALU.add),
                  reads=[b_zp], writes=[b_pa])
            win, winb = pa, b_pa
            if g >= 1:
                cx.op("dve", lambda e: e.tensor_tensor(out=pb2[:, :, 2:271], in0=pa[:, :, 3:272], in1=pa[:, :, 1:270], op=ALU.add),
                      reads=[b_pa], writes=[b_pb2])
                win, winb = pb2, b_pb2
            if g >= 2:
                cx.op("dve", lambda e: e.tensor_tensor(out=pa[:, :, 4:269], in0=pb2[:, :, 6:271], in1=pb2[:, :, 2:267], op=ALU.add),
                      reads=[b_pb2], writes=[b_pa])
                win, winb = pa, b_pa
            if g >= 3:
                cx.op("dve", lambda e: e.tensor_tensor(out=pb2[:, :, 8:264], in0=pa[:, :, 12:268], in1=pa[:, :, 4:260], op=ALU.add),
                      reads=[b_pa], writes=[b_pb2])
                win, winb = pb2, b_pb2
            for half in range(2):
                pt, pb = pr.next()
                cx.op("pe", lambda e: e.matmul(pt[:, :], sel4[:, g * 128:(g + 1) * 128], invcnt[:, half * 512:(half + 1) * 512],
                                               start=True, stop=True), reads=[b_const], writes=[pb])
                other = pa if win is pb2 else pb2
                otherb = b_pa if win is pb2 else b_pb2
                cx.op("dve", lambda e: e.tensor_tensor(out=other[:, 2 * half:2 * half + 2, 8:264], in0=win[:, 2 * half:2 * half + 2, 8:264],
                                                       in1=pt[:, :].rearrange("p (a b) -> p a b", a=2), op=ALU.mult),
                      reads=[winb, pb], writes=[otherb])
                cx.op("dve", lambda e: e.tensor_tensor(out=dTt[:, half * 512:(half + 1) * 512].rearrange("p (a b) -> p a b", a=2),
                                                       in0=other[:, 2 * half:2 * half + 2, 8:264], in1=zp[:, 2 * half:2 * half + 2, 8:264], op=ALU.subtract),
                      reads=[otherb, b_zp], writes=[b_dT])
            for half in range(2):
                pt, pb = pr.next()
                cx.op("pe", lambda e: e.matmul(pt[:, :], wpl[:, g, :], dTt[:, half * 512:(half + 1) * 512], start=True, stop=True),
                      reads=[b_wpl, b_dT], writes=[pb])
                cx.op("dve", lambda e: e.tensor_scalar(out=pool_oT[:, g, half * 512:(half + 1) * 512], in0=pt[:, :],
                                                       scalar1=vecT[:, 134 + g:135 + g], scalar2=None, op0=ALU.mult),
                      reads=[pb, b_vecT], writes=[b_pool_o])
        if stop_after == "pool":
            for j in range(4):
                xt, xb = next_scr()
                cx.op("act", lambda e: e.activation(out=xt[:, :], in_=pool_oT[:, j, :], func=AF.Copy), reads=[b_pool_o], writes=[xb])
                cx.dma("sp", dbg_out[:, j * T:(j + 1) * T], xt[:, :], reads=[xb], is_output=True)
            break
        cx.fence()
        ar.off = mark

        ckvT = ar.take([128, 2, TK], BF16); b_ckvT = Buf("ckvT")
        krT = ar.take([128, TK], BF16); b_krT = Buf("krT")
        qanT = ar.take([128, 4, T], BF16); b_qan = Buf("qanT")
        ET = [ar.take([128, 10, 512], BF16) for _ in range(2)]; b_ET = [Buf("ET0"), Buf("ET1")]
        qn = ar.take([128, T], BF16); b_qn = Buf("qn")
        qr = ar.take([128, T], BF16); b_qr = Buf("qr")
        kn = ar.take([128, TK], BF16); b_kn = Buf("kn")
        vm = ar.take([128, 10, 128], BF16); b_vm = Buf("vm")
        cst = ar.take([128, 2, 320], F32); b_cst = Buf("cst")
        rsm = ar.take([128, 512], F32); b_rsm = Buf("rsm")
        cx.dma("pool", qr[64:68, :], qaug_d, writes=[b_qr])
        cx.dma("pool", krT[64:68, :], kaug_d, writes=[b_krT])
        cx.dma("sp", cst[:, :, 0:256], ctx_ckv[l].rearrange("(c p) f -> p c f", p=128), writes=[b_cst])
        cx.dma("sp", cst[:, :, 256:320], ctx_krope[l].rearrange("(c p) f -> p c f", p=128), writes=[b_cst])
        for tc in range(2):
            for c in range(2):
                transpose_f32(lambda ps_ap, pb, tc=tc, c=c: cx.op(
                    "act", lambda e: e.activation(out=ckvT[:, c, tc * 128:(tc + 1) * 128], in_=ps_ap, func=AF.Copy),
                    reads=[pb], writes=[b_ckvT]), cst[:, tc, c * 128:(c + 1) * 128], 128, 128, [b_cst])
            transpose_f32(lambda ps_ap, pb, tc=tc: cx.op(
                "act", lambda e: e.activation(out=krT[0:64, tc * 128:(tc + 1) * 128], in_=ps_ap, func=AF.Copy),
                reads=[pb], writes=[b_krT]), cst[:, tc, 256:320], 128, 64, [b_cst])
        wv, wb = load_w(w_in[l, :, 0:512], 16, 512)
        wv2, wb2 = load_w(w_in[l, :, 512:832], 16, 320)
        for (wv_, wb_, nch, gcol, nfeat, is_q) in ((wv, wb, 4, 128, 512, True), (wv2, wb2, 2, 132, 256, False)):
            acc = pr.hold(2)
            for c in range(nch):
                for half in range(2):
                    pt, pb = mm_fm(wv_, wb_, c * 128, 128, D, hrhs, hbufs, half)
                    sq, sqb = next_scr()
                    cx.op("act", lambda e: e.activation(out=sq[:, 0:512], in_=pt[:, :], func=AF.Square), reads=[pb], writes=[sqb])
                    cx.op("pe", lambda e: e.matmul(acc[half][0][:, :], ones_f[:, :], sq[:, 0:512], start=(c == 0), stop=(c == nch - 1)),
                          reads=[sqb, b_const], writes=[acc[half][1]])
            rstd_from_psum(acc, nfeat, lambda half: rs[:, half * 512:(half + 1) * 512], b_rs)
            pr.release(acc)
            for c in range(nch):
                raw, rawb = next_scr()
                for half in range(2):
                    sl = slice(half * 512, (half + 1) * 512)
                    pt, pb = mm_fm(wv_, wb_, c * 128, 128, D, hrhs, hbufs, half)
                    cx.op("dve", lambda e: e.tensor_tensor(out=raw[:, sl], in0=pt[:, :], in1=rs[:, sl], op=ALU.mult),
                          reads=[pb, b_rs], writes=[rawb])
                    if is_q:
                        cx.op("dve", lambda e: e.tensor_scalar(out=qanT[:, c, sl], in0=raw[:, sl], scalar1=vecT[:, gcol + c:gcol + c + 1],
                                                               scalar2=None, op0=ALU.mult), reads=[rawb, b_vecT], writes=[b_qan])
                    else:
                        cx.op("dve", lambda e: e.tensor_scalar(out=raw[:, sl], in0=raw[:, sl], scalar1=vecT[:, gcol + c:gcol + c + 1],
                                                               scalar2=None, op0=ALU.mult), reads=[rawb, b_vecT], writes=[rawb])
                        cx.op("act", lambda e: e.activation(out=ckvT[:, c, PAST + half * 512:PAST + (half + 1) * 512], in_=raw[:, sl], func=AF.Copy),
                              reads=[rawb], writes=[b_ckvT])
                if not is_q:
                    out_transposes(raw, rawb, 128, lambda tc, c=c: o_ckv[l, tc * 128:(tc + 1) * 128, c * 128:(c + 1) * 128])
        raw, rawb = next_scr()
        for half in range(2):
            pt, pb = mm_fm(wv2, wb2, 256, 64, D, hrhs, hbufs, half)
            cx.op("act", lambda e: e.activation(out=raw[0:64, half * 512:(half + 1) * 512], in_=pt[0:64, :], func=AF.Copy),
                  reads=[pb], writes=[rawb])
        out_transposes(raw, rawb, 64, lambda tc: o_krope[l, tc * 128:(tc + 1) * 128, :])
        rope_unit(raw, rawb, lambda sl: krT[0:64, PAST + sl.start:PAST + sl.stop], b_krT)
        wqb_v, wqb_b = load_w(w_qb[l], 4, 1536)
        wkv_v, wkv_b = load_w(w_kvb[l], 2, 2048, small=True)
        qan_rhs = lambda kc, half: qanT[:, kc, half * 512:(half + 1) * 512]
        qan_bufs = lambda kc: [b_qan]
        mscale = 192.0 ** -0.5
        etc = 0
        for h in range(8):
            for half in range(2):
                pt, pb = mm_fm(wqb_v, wqb_b, h * 192, 128, 512, qan_rhs, qan_bufs, half)
                cx.op("act", lambda e: e.activation(out=qn[:, half * 512:(half + 1) * 512], in_=pt[:, :], func=AF.Copy),
                      reads=[pb], writes=[b_qn])
            raw, rawb = next_scr()
            for half in range(2):
                pt, pb = mm_fm(wqb_v, wqb_b, h * 192 + 128, 64, 512, qan_rhs, qan_bufs, half)
                cx.op("act", lambda e: e.activation(out=raw[0:64, half * 512:(half + 1) * 512], in_=pt[0:64, :], func=AF.Copy),
                      reads=[pb], writes=[rawb])
            rope_unit(raw, rawb, lambda sl: qr[0:64, sl], b_qr)
            for (c0, n) in ((0, 512), (512, 512), (1024, 256)):
                pt, pb = pr.next()
                for kc in range(2):
                    cx.op("pe", lambda e: e.matmul(pt[:, 0:n], wkv_v[:, kc, h * 256:h * 256 + 128], ckvT[:, kc, c0:c0 + n],
                                                   start=(kc == 0), stop=(kc == 1)), reads=[wkv_b, b_ckvT], writes=[pb])
                cx.op("act", lambda e: e.activation(out=kn[:, c0:c0 + n], in_=pt[:, 0:n], func=AF.Copy), reads=[pb], writes=[b_kn])
            for g4 in range(3):
                pt, pb = pr.next()
                nb = 4 if g4 < 2 else 2
                for bi in range(nb):
                    tkc = g4 * 4 + bi
                    for kc in range(2):
                        cx.op("pe", lambda e: e.matmul(pt[:, bi * 128:(bi + 1) * 128], ckvT[:, kc, tkc * 128:(tkc + 1) * 128],
                                                       wkv_v[:, kc, h * 256 + 128:h * 256 + 256], start=(kc == 0), stop=(kc == 1)),
                              reads=[wkv_b, b_ckvT], writes=[pb])
                cx.op("act", lambda e: e.activation(out=vm[:, g4 * 4:g4 * 4 + nb, :], in_=pt[:, 0:nb * 128].rearrange("p (a b) -> p a b", a=nb),
                                                    func=AF.Copy), reads=[pb], writes=[b_vm])
            conv_step(l, 1)
            for qh in range(2):
                qs = slice(qh * 512, (qh + 1) * 512)
                et, etb = ET[etc % 2], b_ET[etc % 2]
                etc += 1
                o_ps, o_pb, d_ps, d_pb = attention_core(
                    [(lambda kc: kn[:, kc * 128:(kc + 1) * 128], [b_kn], 128),
                     (lambda kc: krT[0:68, kc * 128:(kc + 1) * 128], [b_krT], 68)],
                    [(qn[:, qs], [b_qn]), (qr[0:68, qs], [b_qr])],
                    lambda kc: vm[:, kc, :], [b_vm], et, etb, qh, mscale)
                cx.op("dve", lambda e: e.reciprocal(out=rsm[:, :], in_=d_ps[:, :]), reads=[d_pb], writes=[b_rsm])
                cx.op("dve", lambda e: e.tensor_tensor(out=mla_oT[:, h, qs], in0=o_ps[:, :], in1=rsm[:, :], op=ALU.mult),
                      reads=[o_pb, b_rsm], writes=[b_mla_o])
        if stop_after == "mla":
            for j in range(8):
                xt, xb = next_scr()
                cx.op("act", lambda e: e.activation(out=xt[:, :], in_=mla_oT[:, j, :], func=AF.Copy), reads=[b_mla_o], writes=[xb])
                cx.dma("sp", dbg_out[:, j * T:(j + 1) * T], xt[:, :], reads=[xb], is_output=True)
            break
        cx.fence()
        ar.off = mark
        mergedT = ar.take([128, 16, T], BF16); b_merged = [Buf("mg%d" % j) for j in range(16)]
        accg = ar.take([128, 8, 512], F32); b_accg = [Buf("accg%d" % i) for i in range(8)]
        sgt = [ar.take([128, 512], F32) for _ in range(2)]; b_sgt = [Buf("sg0"), Buf("sg1")]
        branches = ((w_br_mla[l], 1024, mla_oT, b_mla_o), (w_br_pool[l], 512, pool_oT, b_pool_o),
                    (w_br_diff[l], 512, diff_oT, b_diff_o))
        sgc = 0
        for jg in range(4):
            for b in range(3):
                gv, gb = load_w(w_in[l, :, C_GL + b * 2048 + jg * 512:C_GL + b * 2048 + (jg + 1) * 512], 16, 512)
                wbr, Kb, brT, brb = branches[b]
                bv, bb = load_w(wbr[:, jg * 512:(jg + 1) * 512], Kb // 128, 512, small=True)
                for jj in range(4):
                    j = jg * 4 + jj
                    for half in range(2):
                        sl = slice(half * 512, (half + 1) * 512)
                        ai = jj * 2 + half
                        pg, pgb = mm_fm(gv, gb, jj * 128, 128, D, hrhs, hbufs, half)
                        s_t, s_b = sgt[sgc % 2], b_sgt[sgc % 2]
                        sgc += 1
                        cx.op("act", lambda e: e.activation(out=s_t[:, :], in_=pg[:, :], func=AF.Sigmoid), reads=[pgb], writes=[s_b])
                        pbr, pbrb = mm_fm(bv, bb, jj * 128, 128, Kb, lambda kc, hf: brT[:, kc, hf * 512:(hf + 1) * 512],
                                          lambda kc: [brb], half)
                        if b == 0:
                            cx.op("dve", lambda e: e.tensor_tensor(out=accg[:, ai, :], in0=s_t[:, :], in1=pbr[:, :], op=ALU.mult),
                                  reads=[s_b, pbrb], writes=[b_accg[ai]])
                        else:
                            cx.op("dve", lambda e: e.tensor_tensor(out=s_t[:, :], in0=s_t[:, :], in1=pbr[:, :], op=ALU.mult),
                                  reads=[s_b, pbrb], writes=[s_b])
                            if b == 1:
                                cx.op("dve", lambda e: e.tensor_tensor(out=accg[:, ai, :], in0=accg[:, ai, :], in1=s_t[:, :], op=ALU.add),
                                      reads=[s_b, b_accg[ai]], writes=[b_accg[ai]])
                            else:
                                cx.op("dve", lambda e: e.tensor_tensor(out=mergedT[:, j, sl], in0=accg[:, ai, :], in1=s_t[:, :], op=ALU.add),
                                      reads=[s_b, b_accg[ai]], writes=[b_merged[j]])
        if stop_after == "merge":
            for j in range(16):
                xt, xb = next_scr()
                cx.op("act", lambda e: e.activation(out=xt[:, :], in_=mergedT[:, j, :], func=AF.Copy), reads=[b_merged[j]], writes=[xb])
                cx.dma("sp", dbg_out[:, j * T:(j + 1) * T], xt[:, :], reads=[xb], is_output=True)
            break
        for jg in range(4):
            wv, wb = load_w(w_out[l, :, jg * 512:(jg + 1) * 512], 16, 512)
            for jj in range(4):
                j = jg * 4 + jj
                xt, xb = next_scr()
                cx.dma("sp", xt[:, :], xs[j], reads=[b_xs[j]], writes=[xb])
                for half in range(2):
                    sl = slice(half * 512, (half + 1) * 512)
                    pt, pb = mm_fm(wv, wb, jj * 128, 128, D, lambda kc, hf: mergedT[:, kc, hf * 512:(hf + 1) * 512],
                                   lambda kc: [b_merged[kc]], half)
                    cx.op("dve", lambda e: e.scalar_tensor_tensor(out=xt[:, sl], in0=pt[:, :], scalar=modT[:, 32 + j:33 + j],
                                                                  in1=xt[:, sl], op0=ALU.mult, op1=ALU.add),
                          reads=[pb, b_modT, xb], writes=[xb])
                cx.dma("sp", xs[j], xt[:, :], reads=[xb], writes=[b_xs[j]])
        norm_x(l, 2)
        apply_norm(l, 2)
        cx.fence()
        ar.reset()
        if stop_after == "attn":
            for j in range(16):
                xt, xb = next_scr()
                cx.dma("sp", xt[:, :], xs[j], reads=[b_xs[j]], writes=[xb])
                cx.dma("sp", dbg_out[:, j * T:(j + 1) * T], xt[:, :], reads=[xb], is_output=True)
            break
        skT = ar.take([128, 2, 128], BF16); b_skT = Buf("skT")
        skst = ar.take([128, 2, 128], F32); b_skst = Buf("skst")
        qT = ar.take([128, 16, 512], BF16); b_qT = Buf("qT")
        s1 = ar.take([128, 16, 128], F32); b_s1 = Buf("s1")
        s2 = ar.take([128, 16, 128], F32); b_s2 = Buf("s2")
        vtop = ar.take([128, 16, 16], F32); b_vtop = Buf("vtop")
        ix = ar.take([128, 16, 16], U32); b_ix = Buf("ix")
        ixf = ar.take([128, 16, 16], F32); b_ixf = Buf("ixf")
        best = ar.take([128, 8, 16], F32); b_best = Buf("best")
        sel = ar.take([128, 8, 16], U32); b_sel = Buf("sel")
        ru = ar.take([128, 2, 8, 16], U32); b_ru = Buf("ru")
        rf = ar.take([128, 2, 8, 16], F32); b_rf = Buf("rf")
        ab = ar.take([128, 2, 8, 16], F32); b_ab = Buf("ab")
        idxf = ar.take([128, 128], F32); b_idxf = Buf("idxf")
        idxi = ar.take([128, 128], I32); b_idxi = Buf("idxi")
        gw = ar.take([128, 8, 16], F32); b_gw = Buf("gw")
        sm = ar.take([128, 32], F32); b_sm = Buf("sm")
        h2tok = ar.take([128, 2048], BF16); b_h2tok = Buf("h2tok")
        actv = ar.take([128, 128], F32); b_actv = Buf("actv")
        wgt = ar.take([128, 128], F32); b_wgt = Buf("wgt")
        NG = 5
        gbuf = [ar.take([128, 4096], BF16) for _ in range(3)] + [wsm[0], wsm[1]]
        b_gbuf = [Buf("g%d" % i) for i in range(3)] + [b_wsm[0], b_wsm[1]]
        ND = 6
        diag = [ar.take([128, 128], BF16) for _ in range(ND)]; b_diag = [Buf("dg%d" % i) for i in range(ND)]
        wcol = ar.take([128, 256], F32); b_wcol = Buf("wcol")
        otok = [ar.take([128, 512], F32) for _ in range(2)]; b_otok = [Buf("ot0"), Buf("ot1")]
        xupd = [ar.take([128, 4, 128], F32) for _ in range(2)]; b_xupd = [Buf("xu0"), Buf("xu1")]
        conv_step(l, 16)
        cx.dma("sp", skst, subkeys[l].rearrange("p k c -> k p c"), writes=[b_skst])
        for p in range(2):
            transpose_f32(lambda ps_ap, pb, p=p: cx.op("act", lambda e: e.activation(out=skT[:, p, :], in_=ps_ap, func=AF.Copy),
                                                       reads=[pb], writes=[b_skT]), skst[:, p, :], 128, 128, [b_skst])
        gcnt = [0]
        idx3 = [idxi, ar.take([128, 128], I32), ar.take([128, 128], I32)]; b_idx3 = [b_idxi, Buf("idxi1"), Buf("idxi2")]
        gw2 = [gw, ar.take([128, 8, 16], F32)]; b_gw2 = [b_gw, Buf("gw1")]
        h2t2 = [h2tok, ar.take([128, 2048], BF16)]; b_h2t2 = [b_h2tok, Buf("h2tok1")]
        wgt2 = [wgt, ar.take([128, 128], F32)]; b_wgt2 = [b_wgt, Buf("wgt1")]

        def peer_qproj(th):
            for jg in range(4):
                wv, wb = load_w(w_peer_q[l, :, jg * 512:(jg + 1) * 512], 16, 512)
                for jj in range(4):
                    pt, pb = mm_fm(wv, wb, jj * 128, 128, D, hrhs, hbufs, th)
                    cx.op("act", lambda e: e.activation(out=qT[:, jg * 4 + jj, :], in_=pt[:, :], func=AF.Copy), reads=[pb], writes=[b_qT])

        def peer_A1(tb):
            tb4 = tb % 4
            tsl = slice(tb4 * 128, (tb4 + 1) * 128)
            idxo, b_idxo = idx3[tb % 3], b_idx3[tb % 3]
            banks = pr.hold(4)
            for hp in range(16):
                bt, bbf = banks[hp // 4]
                cx.op("pe", lambda e: e.matmul(bt[:, (hp % 4) * 128:(hp % 4 + 1) * 128], qT[:, hp, tsl], skT[:, hp % 2, :],
                                               start=True, stop=True), reads=[b_qT, b_skT], writes=[bbf])
            for q4 in range(4):
                bt, bbf = banks[q4]
                cx.op("act", lambda e: e.activation(out=s1[:, q4 * 4:(q4 + 1) * 4, :], in_=bt[:, :].rearrange("p (a b) -> p a b", a=4),
                                                    func=AF.Copy), reads=[bbf], writes=[b_s1])
            pr.release(banks)
            for hp in range(16):
                cx.op("dve", lambda e: e.max(out=vtop[:, hp, 0:8], in_=s1[:, hp, :]), reads=[b_s1], writes=[b_vtop])
                cx.op("dve", lambda e: e.max_index(out=ix[:, hp, 0:8], in_max=vtop[:, hp, 0:8], in_values=s1[:, hp, :]),
                      reads=[b_s1, b_vtop], writes=[b_ix])
                cx.op("dve", lambda e: e.match_replace(out=s2[:, hp, :], in_to_replace=vtop[:, hp, 0:8], in_values=s1[:, hp, :],
                                                       imm_value=-1e30), reads=[b_s1, b_vtop], writes=[b_s2])
                cx.op("dve", lambda e: e.max(out=vtop[:, hp, 8:16], in_=s2[:, hp, :]), reads=[b_s2], writes=[b_vtop])
                cx.op("dve", lambda e: e.max_index(out=ix[:, hp, 8:16], in_max=vtop[:, hp, 8:16], in_values=s2[:, hp, :]),
                      reads=[b_s2, b_vtop], writes=[b_ix])
            cand4 = s1.rearrange("p (h a) (b c) -> p h (a b) c", a=2, b=8)
            cand2_4 = s2.rearrange("p (h a) (b c) -> p h (a b) c", a=2, b=8)
            candf = s1.rearrange("p (h a) k -> p h (a k)", a=2)
            cand2f = s2.rearrange("p (h a) k -> p h (a k)", a=2)
            vt4 = vtop.rearrange("p (h a) k -> p h a k", a=2)
            cx.op("dve", lambda e: e.tensor_tensor(out=cand4, in0=vt4[:, :, 0, :].unsqueeze(3).to_broadcast([128, 8, 16, 16]),
                                                   in1=vt4[:, :, 1, :].unsqueeze(2).to_broadcast([128, 8, 16, 16]), op=ALU.add),
                  reads=[b_vtop], writes=[b_s1])
            for h in range(8):
                cx.op("dve", lambda e: e.max(out=best[:, h, 0:8], in_=candf[:, h, :]), reads=[b_s1], writes=[b_best])
                cx.op("dve", lambda e: e.max_index(out=sel[:, h, 0:8], in_max=best[:, h, 0:8], in_values=candf[:, h, :]),
                      reads=[b_s1, b_best], writes=[b_sel])
                cx.op("dve", lambda e: e.match_replace(out=cand2f[:, h, :], in_to_replace=best[:, h, 0:8], in_values=candf[:, h, :],
                                                       imm_value=-1e30), reads=[b_s1, b_best], writes=[b_s2])
                cx.op("dve", lambda e: e.max(out=best[:, h, 8:16], in_=cand2f[:, h, :]), reads=[b_s2], writes=[b_best])
                cx.op("dve", lambda e: e.max_index(out=sel[:, h, 8:16], in_max=best[:, h, 8:16], in_values=cand2f[:, h, :]),
                      reads=[b_s2, b_best], writes=[b_sel])
            cx.op("dve", lambda e: e.tensor_single_scalar(out=ru[:, 0], in_=sel, scalar=4, op=ALU.logical_shift_right),
                  reads=[b_sel], writes=[b_ru])
            cx.op("dve", lambda e: e.tensor_single_scalar(out=ru[:, 1], in_=sel, scalar=15, op=ALU.bitwise_and),
                  reads=[b_sel], writes=[b_ru])
            cx.op("dve", lambda e: e.tensor_copy(out=rf, in_=ru), reads=[b_ru], writes=[b_rf])
            cx.op("dve", lambda e: e.tensor_copy(out=ixf, in_=ix), reads=[b_ix], writes=[b_ixf])
            ixf4 = ixf.rearrange("p (h a) k -> p h a k", a=2)
            for a in range(2):
                cx.op("dve", lambda e: e.tensor_tensor(out=cand4, in0=iota16[:, :].unsqueeze(1).unsqueeze(1).to_broadcast([128, 8, 16, 16]),
                                                       in1=rf[:, a].unsqueeze(3).to_broadcast([128, 8, 16, 16]), op=ALU.is_equal),
                      reads=[b_rf, b_const], writes=[b_s1])
                cx.op("dve", lambda e: e.tensor_tensor(out=cand2_4, in0=cand4,
                                                       in1=ixf4[:, :, a, :].unsqueeze(2).to_broadcast([128, 8, 16, 16]), op=ALU.mult),
                      reads=[b_s1, b_ixf], writes=[b_s2])
                cx.op("dve", lambda e: e.tensor_reduce(out=ab[:, a], in_=cand2_4, axis=AX.X, op=ALU.add),
                      reads=[b_s2], writes=[b_ab])
            cx.op("dve", lambda e: e.scalar_tensor_tensor(out=idxf.rearrange("p (h k) -> p h k", h=8), in0=ab[:, 0], scalar=128.0,
                                                          in1=ab[:, 1], op0=ALU.mult, op1=ALU.add), reads=[b_ab], writes=[b_idxf])
            cx.op("dve", lambda e: e.tensor_copy(out=idxo, in_=idxf), reads=[b_idxf], writes=[b_idxo])

        def peer_A2(tb):
            gw, b_gw = gw2[tb % 2], b_gw2[tb % 2]
            h2tok, b_h2tok = h2t2[tb % 2], b_h2t2[tb % 2]
            cx.op("dve", lambda e: e.tensor_scalar(out=sm[:, 0:8], in0=best[:, :, 0], scalar1=-1.0, scalar2=None, op0=ALU.mult),
                  reads=[b_best], writes=[b_sm])
            for h in range(8):
                cx.op("act", lambda e: e.activation(out=gw[:, h, :], in_=best[:, h, :], func=AF.Exp, bias=sm[:, h:h + 1], scale=1.0,
                                                    accum_out=sm[:, 8 + h:9 + h]), reads=[b_best, b_sm], writes=[b_gw, b_sm])
            cx.op("dve", lambda e: e.reciprocal(out=sm[:, 16:24], in_=sm[:, 8:16]), reads=[b_sm], writes=[b_sm])
            cx.op("dve", lambda e: e.tensor_tensor(out=gw, in0=gw, in1=sm[:, 16:24].unsqueeze(2).to_broadcast([128, 8, 16]), op=ALU.mult),
                  reads=[b_gw, b_sm], writes=[b_gw])
            for j8 in range(2):
                pt, pb = pr.next()
                ptb = pt[:, :].bitcast(BF16)
                for jj in range(8):
                    j = j8 * 8 + jj
                    cx.op("pe", lambda e: e.transpose(ptb[:, jj * 128:(jj + 1) * 128], hT[:, j, tb * 128:(tb + 1) * 128], ident_b[:, :]),
                          reads=[b_hT[j], b_const], writes=[pb])
                cx.op("act", lambda e: e.activation(out=h2tok[:, j8 * 1024:(j8 + 1) * 1024], in_=ptb, func=AF.Copy),
                      reads=[pb], writes=[b_h2tok])

        def peer_step(tb, kk, accb):
            idxo, b_idxo = idx3[tb % 3], b_idx3[tb % 3]
            h2tok, b_h2tok = h2t2[tb % 2], b_h2t2[tb % 2]
            gw, b_gw = gw2[tb % 2], b_gw2[tb % 2]
            gwf = gw.rearrange("p h k -> p (h k)")
            gi = gcnt[0] % NG
            gcnt[0] += 1
            gb, gbb = gbuf[gi], b_gbuf[gi]
            cx.dma("pool", gb[:, :], puv_rows[l], indirect=idxo[:, kk:kk + 1], reads=[b_idxo] + b_tab[l], writes=[gbb])
            cx.op("dve", lambda e: e.scalar_tensor_tensor(out=gb[:, 0:2048], in0=gb[:, 0:2048], scalar=1.0, in1=h2tok[:, :],
                                                          op0=ALU.mult, op1=ALU.mult, accum_out=actv[:, kk:kk + 1]),
                  reads=[gbb, b_h2tok], writes=[gbb, b_actv])
            cx.op("act", lambda e: e.activation(out=wcol[:, kk:kk + 1], in_=actv[:, kk:kk + 1], func=AF.Gelu),
                  reads=[b_actv], writes=[b_wcol])
            cx.op("act", lambda e: e.activation(out=wcol[:, 128 + kk:129 + kk], in_=wcol[:, kk:kk + 1], func=AF.Copy,
                                                scale=gwf[:, kk:kk + 1]), reads=[b_wcol, b_gw], writes=[b_wcol])
            dg, dgb = diag[kk % ND], b_diag[kk % ND]
            cx.op("act", lambda e: e.activation(out=dg[:, :], in_=ident_b[:, :], func=AF.Copy, scale=wcol[:, 128 + kk:129 + kk]),
                  reads=[b_wcol, b_const], writes=[dgb])
            for dq in range(4):
                cx.op("pe", lambda e: e.matmul(accb[dq][0][:, :], dg[:, :], gb[:, 2048 + dq * 512:2048 + (dq + 1) * 512],
                                               start=(kk == 0), stop=(kk == 127)), reads=[dgb, gbb], writes=[accb[dq][1]])

        def peer_B_tail(tb, accb):
            for dq in range(4):
                ot, otb = otok[dq % 2], b_otok[dq % 2]
                xu, xub = xupd[dq % 2], b_xupd[dq % 2]
                cx.op("act", lambda e: e.activation(out=ot[:, :], in_=accb[dq][0][:, :], func=AF.Copy), reads=[accb[dq][1]], writes=[otb])
                cx.dma("sp", xu, xs[dq * 4:(dq + 1) * 4, :, tb * 128:(tb + 1) * 128].rearrange("j p t -> p j t"),
                       reads=[b_xs[dq * 4 + i] for i in range(4)], writes=[xub])
                for jj in range(4):
                    j = dq * 4 + jj
                    transpose_f32(lambda ps_ap, pb, jj=jj, j=j: cx.op(
                        "dve", lambda e: e.scalar_tensor_tensor(out=xu[:, jj, :], in0=ps_ap, scalar=modT[:, 80 + j:81 + j],
                                                                in1=xu[:, jj, :], op0=ALU.mult, op1=ALU.add),
                        reads=[pb, b_modT, xub], writes=[xub]), ot[:, jj * 128:(jj + 1) * 128], 128, 128, [otb])
                cx.dma("sp", xs[dq * 4:(dq + 1) * 4, :, tb * 128:(tb + 1) * 128].rearrange("j p t -> p j t"), xu,
                       reads=[xub], writes=[b_xs[dq * 4 + i] for i in range(4)])

        def peer_A(tb):
            if tb == 4:
                peer_qproj(1)
            peer_A1(tb)
            peer_A2(tb)

        peer_qproj(0)
        peer_A(0)
        for tb in range(8):
            accb = pr.hold(4)
            items = []
            if tb + 1 < 8:
                cx.begin_record()
                peer_A(tb + 1)
                items = cx.end_record()
            per = (len(items) + 119) // 120
            pos = 0
            for kk in range(128):
                peer_step(tb, kk, accb)
                for _ in range(per):
                    if pos < len(items):
                        cx.replay(items[pos])
                        pos += 1
            while pos < len(items):
                cx.replay(items[pos])
                pos += 1
            peer_B_tail(tb, accb)
            pr.release(accb)
        cx.fence()
        if stop_after == "layer":
            for j in range(16):
                xt, xb = next_scr()
                cx.dma("sp", xt[:, :], xs[j], reads=[b_xs[j]], writes=[xb])
                cx.dma("sp", dbg_out[:, j * T:(j + 1) * T], xt[:, :], reads=[xb], is_output=True)
            break
    else:
        norm_x(L, 1)
        for j in range(16):
            xt, xb = next_scr()
            cx.dma("sp", xt[:, :], xs[j], reads=[b_xs[j]], writes=[xb])
            cx.op("dve", lambda e: e.tensor_tensor(out=xt[:, :], in0=xt[:, :], in1=rs[:, :], op=ALU.mult), reads=[xb, b_rs], writes=[xb])
            cx.op("dve", lambda e: e.tensor_scalar(out=xt[:, :], in0=xt[:, :], scalar1=gvT[:, 16 + j:17 + j], scalar2=None, op0=ALU.mult),
                  reads=[xb, b_gvT], writes=[xb])
            for tc in range(8):
                st, sbf = next_stg()
                transpose_f32(lambda ps_ap, pb, st=st, sbf=sbf: cx.op("act", lambda e: e.activation(out=st[:, :], in_=ps_ap, func=AF.Copy),
                                                                      reads=[pb], writes=[sbf]), xt[:, tc * 128:(tc + 1) * 128], 128, 128, [xb])
                cx.dma("sp", y_out[tc * 128:(tc + 1) * 128, j * 128:(j + 1) * 128], st[:, :], reads=[sbf], is_output=True)

    cx.finish()
    es.close()
    return nc, in_names


def _rope_tables(sample):
    f = np.arange(64)
    axis = f // 32
    jj = f % 16
    hf = (f % 32) // 16
    t = np.arange(T)
    if sample:
        pos = np.where(axis[:, None] == 0, (t // 64)[None, :], (t % 64)[None, :]).astype(np.float32)
        inv = (10000.0 ** (-(jj.astype(np.float32)) / 16.0)).astype(np.float32)
        ang = pos * inv[:, None]
        c = np.cos(ang).astype(np.float32)
        s = np.sin(ang).astype(np.float32)
        s = np.where(hf[:, None] == 0, -s, s).astype(np.float32)
    else:
        c = np.ones((64, T), np.float32)
        s = np.zeros((64, T), np.float32)
    partner = np.where(hf == 0, f + 16, f - 16)
    perm = np.zeros((64, 64), np.float32)
    perm[partner, f] = 1.0
    return c, s, perm


def _core_consts(sample):
    c, s, perm = _rope_tables(sample)
    t = np.arange(T)
    qaug = np.zeros((4, T), np.float32)
    kaug = np.zeros((4, TK), np.float32)
    seglen = T if sample else 256
    if not sample:
        seg = t // 256
        for sp in range(4):
            qaug[sp] = -(seg == sp).astype(np.float32)
            kaug[sp, :PAST] = BIG
            kaug[sp, PAST:] = BIG * (seg != sp).astype(np.float32)
    invcnt = np.zeros((4, T), np.float32)
    tt = t % seglen
    for gi, w in enumerate((2, 4, 8, 16)):
        lo = np.maximum(tt - w // 2, 0)
        hi = np.minimum(tt + (w - 1) // 2, seglen - 1)
        invcnt[gi] = 1.0 / (hi - lo + 1).astype(np.float32)
    sel4 = np.zeros((4, 4, 128), np.float32)
    for g in range(4):
        sel4[g, g, :] = 1.0
    return {
        "ident": np.eye(128, dtype=np.float32), "perm64": perm, "ropec": c, "ropes": s, "qaug": qaug, "kaug": kaug,
        "flag": np.full((128, 1), 1.0 if sample else 0.0, np.float32), "invcnt": invcnt,
        "sel4": sel4.reshape(4, 512), "iota16": np.tile(np.arange(16, dtype=np.float32), (128, 1)),
    }


def make_in_maps(inp, L=4, cores=range(8), names=None):
    f = lambda a: np.ascontiguousarray(np.asarray(a, dtype=np.float32))
    vecs = np.zeros((L, 144, 128), np.float32)
    for l in range(L):
        vecs[l, 0:96] = f(inp["b_mod"])[l].reshape(96, 128)
        vecs[l, 96:112] = f(inp["g_norm1"])[l].reshape(16, 128)
        vecs[l, 112:128] = f(inp["g_norm2"])[l].reshape(16, 128)
        vecs[l, 128:132] = f(inp["g_qnorm"])[l].reshape(4, 128)
        vecs[l, 132:134] = f(inp["g_kvnorm"])[l].reshape(2, 128)
        vecs[l, 134:138] = f(inp["pool_scale"])[l].reshape(4, 128)
        vecs[l, 138] = f(inp["g_diffnorm"])[l]
    shared = {
        "w_mod": f(inp["w_mod"])[:L], "vecs": vecs, "w_in": f(inp["w_in"])[:L], "w_qb": f(inp["w_qb"])[:L],
        "w_kvb": f(inp["w_kvb"])[:L], "w_pool": f(inp["w_pool"])[:L],
        "diff_lambda": f(inp["diff_lambda"])[:L].reshape(L, 256),
        "w_br_mla": f(inp["w_br_mla"])[:L], "w_br_pool": f(inp["w_br_pool"])[:L], "w_br_diff": f(inp["w_br_diff"])[:L],
        "w_out": f(inp["w_out"])[:L], "w_peer_q": f(inp["w_peer_q"])[:L], "peer_subkeys": f(inp["peer_subkeys"])[:L],
    }
    if names is None or "peer_u" in names:
        shared["peer_u"] = f(inp["peer_u"])[:L]
        shared["peer_v"] = f(inp["peer_v"])[:L]
    xp = f(inp["x_prompt"]); xsm = f(inp["x_sample"])
    cs = {True: _core_consts(True), False: _core_consts(False)}
    maps = []
    for c in cores:
        sample = c >= 4
        m = dict(shared)
        m.update(cs[sample])
        gv = np.zeros((32, 128), np.float32)
        gv[16:32] = f(inp["g_final"]).reshape(16, 128)
        if not sample:
            m["x"] = xp[4 * c:4 * c + 4].reshape(T, D)
            gv[0:16] = f(inp["c_ctx"]).reshape(16, 128)
            m["ctx_ckv"] = np.zeros((L, PAST, 256), np.float32)
            m["ctx_krope"] = np.zeros((L, PAST, 64), np.float32)
            m["ctx_dk"] = np.zeros((L, PAST, 512), np.float32)
            m["ctx_dv"] = np.zeros((L, PAST, 512), np.float32)
        else:
            b = (c - 4) % 2
            m["x"] = xsm[b]
            gv[0:16] = f(inp["c"])[b].reshape(16, 128)
            m["ctx_ckv"] = f(inp["cache_mla_ckv"])[b, :L]
            m["ctx_krope"] = f(inp["cache_mla_krope"])[b, :L]
            m["ctx_dk"] = f(inp["cache_diff_k"])[b, :L].reshape(L, PAST, 512)
            m["ctx_dv"] = f(inp["cache_diff_v"])[b, :L].reshape(L, PAST, 512)
        m["gvec"] = gv
        if names is not None:
            m = {k: v for k, v in m.items() if k in names}
        maps.append(m)
    return maps


def kernel(**inp):
    L = 4
    nc, names = build_program(L)
    maps = make_in_maps(inp, L, names=names)
    res = run_bass_kernel_spmd(nc, maps, core_ids=list(range(8)))
    r = res.results
    y_prompt = np.zeros((16, 256, D), np.float32)
    st_ckv = np.zeros((16, L, 256, 256), np.float32)
    st_krope = np.zeros((16, L, 256, 64), np.float32)
    st_k = np.zeros((16, L, 256, 4, 128), np.float32)
    st_v = np.zeros((16, L, 256, 4, 128), np.float32)
    for c in range(4):
        y_prompt[4 * c:4 * c + 4] = r[c]["y"].reshape(4, 256, D)
        for l in range(L):
            st_ckv[4 * c:4 * c + 4, l] = r[c]["o_ckv"][l].reshape(4, 256, 256)
            st_krope[4 * c:4 * c + 4, l] = r[c]["o_krope"][l].reshape(4, 256, 64)
            st_k[4 * c:4 * c + 4, l] = r[c]["o_dk"][l].reshape(4, 256, 4, 128)
            st_v[4 * c:4 * c + 4, l] = r[c]["o_dv"][l].reshape(4, 256, 4, 128)
    y_sample = np.stack([r[4]["y"], r[5]["y"]], axis=0)
    return (y_prompt, y_sample, st_ckv, st_krope, st_k, st_v)
```
